# Optimizing a Trainium2 kernel written in Bass

```python
import jax, jax.numpy as jnp
from jax import lax
import numpy as np

D_MODEL = 4096
BATCH = 2
SEQ = 8192
DEPTH = 1

GRID_W = 64
CTX_LEN = 256
HEAD_DIM = 64
ATTN_WIDTH = D_MODEL // 2
N_Q_HEADS = ATTN_WIDTH // HEAD_DIM
N_KV_HEADS = max(1, N_Q_HEADS // 8)
KV_GROUP = N_Q_HEADS // N_KV_HEADS
KV_WIDTH = N_KV_HEADS * HEAD_DIM
POOL_WIDTH = D_MODEL - ATTN_WIDTH
POOL_WINDOWS = (2, 4, 8, 16)
N_POOL_GROUPS = len(POOL_WINDOWS)
POOL_GROUP_WIDTH = POOL_WIDTH // N_POOL_GROUPS
MIX_WIDTH = ATTN_WIDTH + POOL_WIDTH
IN_WIDTH = ATTN_WIDTH + 2 * KV_WIDTH + POOL_WIDTH
WINDOW = 128
BLOCK = 128
ROPE_HALF = HEAD_DIM // 2
ROPE_BASE = 10000.0
N_EXPERTS = 32
TOP_K = 4
EXPERT_FF = D_MODEL // 4
SWIGLU_LIMIT = 7.0
SWIGLU_ALPHA = 1.702
N_MOD = 6
NORM_EPS = 1e-6
NEG_INF = -1e30

kernel_name = 'hybrid_window_gqa_pool_moe_dit_layer'


def rms_norm(x, g):
    xf = x.astype(jnp.float32)
    y = xf * lax.rsqrt(jnp.mean(xf * xf, axis=-1, keepdims=True) + NORM_EPS)
    return (y * g.astype(jnp.float32)).astype(x.dtype)


def modulate(h, shift, scale):
    return h * (1.0 + scale) + shift


def _rotate(x, ang):
    n = x.shape[-1] // 2
    cos = jnp.cos(ang)[None, :, None, :]
    sin = jnp.sin(ang)[None, :, None, :]
    x1, x2 = x[..., :n], x[..., n:]
    return jnp.concatenate([x1 * cos - x2 * sin, x2 * cos + x1 * sin], axis=-1)


def axial_rope(x, ang_row, ang_col):
    xf = x.astype(jnp.float32)
    out = jnp.concatenate([_rotate(xf[..., :ROPE_HALF], ang_row),
                           _rotate(xf[..., ROPE_HALF:], ang_col)], axis=-1)
    return out.astype(x.dtype)


def project(h, w_in, q_norm_g, k_norm_g):
    z = h @ w_in
    q, k, v, u = jnp.split(z, [ATTN_WIDTH, ATTN_WIDTH + KV_WIDTH, ATTN_WIDTH + 2 * KV_WIDTH], axis=-1)
    lead = h.shape[:-1]
    q = rms_norm(q.reshape(*lead, N_Q_HEADS, HEAD_DIM), q_norm_g)
    k = rms_norm(k.reshape(*lead, N_KV_HEADS, HEAD_DIM), k_norm_g)
    v = v.reshape(*lead, N_KV_HEADS, HEAD_DIM)
    return q, k, v, u


def latent_window_attention(q, k, v, k_ctx, v_ctx, sinks):
    b, s = q.shape[0], q.shape[1]
    nb = s // BLOCK
    scale = HEAD_DIM ** -0.5
    qb = q.reshape(b, nb, BLOCK, N_KV_HEADS, KV_GROUP, HEAD_DIM)

    def bands(t):
        tp = jnp.pad(t, ((0, 0), (BLOCK, BLOCK), (0, 0), (0, 0))).reshape(b, nb + 2, BLOCK, N_KV_HEADS, HEAD_DIM)
        return jnp.concatenate([tp[:, :-2], tp[:, 1:-1], tp[:, 2:]], axis=2)

    kb, vb = bands(k), bands(v)
    s_loc = jnp.einsum('bnqhgd,bnkhd->bnhgqk', qb, kb).astype(jnp.float32) * scale
    s_ctx = jnp.einsum('bnqhgd,bchd->bnhgqc', qb, k_ctx).astype(jnp.float32) * scale
    blk = jnp.arange(nb)[:, None] * BLOCK
    qpos = blk + jnp.arange(BLOCK)[None, :]
    kpos = blk - BLOCK + jnp.arange(3 * BLOCK)[None, :]
    kp = kpos[:, None, :]
    valid = (kp >= 0) & (kp < s) & (jnp.abs(qpos[:, :, None] - kp) <= WINDOW)
    s_loc = jnp.where(valid[None, :, None, None], s_loc, NEG_INF)
    sink = sinks.astype(jnp.float32).reshape(N_KV_HEADS, KV_GROUP)[None, None, :, :, None, None]
    m = jnp.maximum(jnp.maximum(s_loc.max(-1, keepdims=True), s_ctx.max(-1, keepdims=True)), sink)
    e_loc = jnp.exp(s_loc - m)
    e_ctx = jnp.exp(s_ctx - m)
    denom = e_loc.sum(-1, keepdims=True) + e_ctx.sum(-1, keepdims=True) + jnp.exp(sink - m)
    p_loc = (e_loc / denom).astype(v.dtype)
    p_ctx = (e_ctx / denom).astype(v.dtype)
    o = (jnp.einsum('bnhgqk,bnkhd->bnqhgd', p_loc, vb)
         + jnp.einsum('bnhgqc,bchd->bnqhgd', p_ctx, v_ctx))
    return o.reshape(b, s, ATTN_WIDTH)


def context_attention(q, k, v, sinks):
    b, n = q.shape[0], q.shape[1]
    qg = q.reshape(b, n, N_KV_HEADS, KV_GROUP, HEAD_DIM)
    sc = jnp.einsum('bqhgd,bkhd->bhgqk', qg, k).astype(jnp.float32) * HEAD_DIM ** -0.5
    sink = sinks.astype(jnp.float32).reshape(N_KV_HEADS, KV_GROUP)[None, :, :, None, None]
    m = jnp.maximum(sc.max(-1, keepdims=True), sink)
    e = jnp.exp(sc - m)
    p = (e / (e.sum(-1, keepdims=True) + jnp.exp(sink - m))).astype(v.dtype)
    o = jnp.einsum('bhgqk,bkhd->bqhgd', p, v)
    return o.reshape(b, n, ATTN_WIDTH)


def multiscale_pool(u, w_pool, pool_scale):
    b, n, _ = u.shape
    uf = u.astype(jnp.float32)
    cs = jnp.pad(jnp.cumsum(uf, axis=1), ((0, 0), (1, 0), (0, 0)))
    t = jnp.arange(n)
    diffs = []
    for g, w in enumerate(POOL_WINDOWS):
        lo = jnp.clip(t - w // 2, 0, n)
        hi = jnp.clip(t + w // 2, 0, n)
        csg = cs[..., g * POOL_GROUP_WIDTH:(g + 1) * POOL_GROUP_WIDTH]
        mean = (jnp.take(csg, hi, axis=1) - jnp.take(csg, lo, axis=1)) / (hi - lo).astype(jnp.float32)[None, :, None]
        diffs.append(mean - uf[..., g * POOL_GROUP_WIDTH:(g + 1) * POOL_GROUP_WIDTH])
    d = jnp.stack(diffs, axis=2).astype(u.dtype)
    y = jnp.einsum('blgc,gcd->blgd', d, w_pool).reshape(b, n, POOL_WIDTH)
    return y * pool_scale


def moe_ffn(h, w_router, b_router, w_gate, b_gate, w_up, b_up, w_down, b_down):
    hf = h.reshape(-1, h.shape[-1])
    logits = (hf @ w_router + b_router).astype(jnp.float32)
    top_logit, top_idx = lax.top_k(logits, TOP_K)
    top_w = jax.nn.softmax(top_logit, axis=-1)
    combine = jnp.einsum('nk,nke->en', top_w, jax.nn.one_hot(top_idx, N_EXPERTS, dtype=jnp.float32)).astype(h.dtype)
    out = jnp.zeros_like(hf)
    for e in range(N_EXPERTS):
        gate = jnp.minimum(hf @ w_gate[e] + b_gate[e], SWIGLU_LIMIT)
        lin = jnp.clip(hf @ w_up[e] + b_up[e], -SWIGLU_LIMIT, SWIGLU_LIMIT)
        act = gate * jax.nn.sigmoid(SWIGLU_ALPHA * gate) * (lin + 1.0)
        out = out + combine[e][:, None] * (act @ w_down[e] + b_down[e])
    return out.reshape(h.shape)


def setup_inputs(seed: int = 0) -> dict:
    key = jax.random.key(seed)
    ks = jax.random.split(key, 23)

    def nrm(k, shape, scale):
        return jax.random.normal(k, shape, jnp.float32) * scale

    return {
        'x': nrm(ks[0], (BATCH, SEQ, D_MODEL), 1.0),
        'c': nrm(ks[1], (BATCH, D_MODEL), 1.0),
        'ctx': nrm(ks[2], (BATCH, CTX_LEN, D_MODEL), 1.0),
        'c_ctx': nrm(ks[3], (D_MODEL,), 1.0),
        'w_ada': nrm(ks[4], (DEPTH, D_MODEL, N_MOD * D_MODEL), 0.5 * D_MODEL ** -0.5),
        'b_ada': nrm(ks[5], (DEPTH, N_MOD * D_MODEL), 0.02),
        'norm1_g': 1.0 + nrm(ks[6], (DEPTH, D_MODEL), 0.02),
        'w_in': nrm(ks[7], (DEPTH, D_MODEL, IN_WIDTH), D_MODEL ** -0.5),
        'q_norm_g': 1.0 + nrm(ks[8], (DEPTH, HEAD_DIM), 0.02),
        'k_norm_g': 1.0 + nrm(ks[9], (DEPTH, HEAD_DIM), 0.02),
        'sinks': nrm(ks[10], (DEPTH, N_Q_HEADS), 0.5),
        'w_pool': nrm(ks[11], (DEPTH, N_POOL_GROUPS, POOL_GROUP_WIDTH, POOL_GROUP_WIDTH), POOL_GROUP_WIDTH ** -0.5),
        'pool_scale': 1.0 + nrm(ks[12], (DEPTH, POOL_WIDTH), 0.1),
        'w_out': nrm(ks[13], (DEPTH, MIX_WIDTH, D_MODEL), MIX_WIDTH ** -0.5),
        'norm2_g': 1.0 + nrm(ks[14], (DEPTH, D_MODEL), 0.02),
        'w_router': nrm(ks[15], (DEPTH, D_MODEL, N_EXPERTS), D_MODEL ** -0.5),
        'b_router': nrm(ks[16], (DEPTH, N_EXPERTS), 0.01),
        'w_gate': nrm(ks[17], (DEPTH, N_EXPERTS, D_MODEL, EXPERT_FF), D_MODEL ** -0.5),
        'b_gate': nrm(ks[18], (DEPTH, N_EXPERTS, EXPERT_FF), 0.01),
        'w_up': nrm(ks[19], (DEPTH, N_EXPERTS, D_MODEL, EXPERT_FF), D_MODEL ** -0.5),
        'b_up': nrm(ks[20], (DEPTH, N_EXPERTS, EXPERT_FF), 0.01),
        'w_down': nrm(ks[21], (DEPTH, N_EXPERTS, EXPERT_FF, D_MODEL), EXPERT_FF ** -0.5),
        'b_down': nrm(ks[22], (DEPTH, N_EXPERTS, D_MODEL), 0.01),
    }


def reference(x, c, ctx, c_ctx, w_ada, b_ada, norm1_g, w_in, q_norm_g, k_norm_g, sinks,
              w_pool, pool_scale, w_out, norm2_g, w_router, b_router,
              w_gate, b_gate, w_up, b_up, w_down, b_down):
    n_tok = x.shape[1]
    rows = n_tok // GRID_W
    row = jnp.repeat(jnp.arange(rows, dtype=jnp.float32), GRID_W)
    col = jnp.tile(jnp.arange(GRID_W, dtype=jnp.float32), rows)
    inv_freq = ROPE_BASE ** (-jnp.arange(0, ROPE_HALF, 2, dtype=jnp.float32) / ROPE_HALF)
    ang_row = row[:, None] * inv_freq[None, :]
    ang_col = col[:, None] * inv_freq[None, :]

    for layer in range(DEPTH):
        last = layer == DEPTH - 1
        mod = (jax.nn.silu(c) @ w_ada[layer] + b_ada[layer]).reshape(c.shape[0], N_MOD, 1, D_MODEL)
        mod_c = (jax.nn.silu(c_ctx) @ w_ada[layer] + b_ada[layer]).reshape(N_MOD, D_MODEL)

        h_lat = modulate(rms_norm(x, norm1_g[layer]), mod[:, 0], mod[:, 1])
        h_ctx = modulate(rms_norm(ctx, norm1_g[layer]), mod_c[0], mod_c[1])
        q, k, v, u = project(h_lat, w_in[layer], q_norm_g[layer], k_norm_g[layer])
        q_c, k_c, v_c, u_c = project(h_ctx, w_in[layer], q_norm_g[layer], k_norm_g[layer])
        q = axial_rope(q, ang_row, ang_col)
        k = axial_rope(k, ang_row, ang_col)
        attn = latent_window_attention(q, k, v, k_c, v_c, sinks[layer])
        pool = multiscale_pool(u, w_pool[layer], pool_scale[layer])
        x = x + mod[:, 2] * (jnp.concatenate([attn, pool], axis=-1) @ w_out[layer])
        if not last:
            attn_c = context_attention(q_c, k_c, v_c, sinks[layer])
            pool_c = multiscale_pool(u_c, w_pool[layer], pool_scale[layer])
            ctx = ctx + mod_c[2] * (jnp.concatenate([attn_c, pool_c], axis=-1) @ w_out[layer])

        h2 = modulate(rms_norm(x, norm2_g[layer]), mod[:, 3], mod[:, 4])
        x = x + mod[:, 5] * moe_ffn(h2, w_router[layer], b_router[layer], w_gate[layer], b_gate[layer],
                                    w_up[layer], b_up[layer], w_down[layer], b_down[layer])
        if not last:
            h2_c = modulate(rms_norm(ctx, norm2_g[layer]), mod_c[3], mod_c[4])
            ctx = ctx + mod_c[5] * moe_ffn(h2_c, w_router[layer], b_router[layer], w_gate[layer], b_gate[layer],
                                           w_up[layer], b_up[layer], w_down[layer], b_down[layer])
    return x
```

```python
import numpy as np
from contextlib import ExitStack
import concourse.bass as bass
import concourse.mybir as mybir
from concourse.bass_utils import run_bass_kernel_spmd

F32 = mybir.dt.float32
BF16 = mybir.dt.bfloat16
I32 = mybir.dt.int32
ALU = mybir.AluOpType
AF = mybir.ActivationFunctionType
AX = mybir.AxisListType

NCORES = 8
NORM_EPS = 1e-6
BIGSLOT = 1.0e6


class Cfg:
    def __init__(self, D=4096, BATCH=2, SEQ=8192, CTX=256, FF=1024, NE=32, CAP=1024, GRID_W=64):
        self.D = D
        self.BATCH = BATCH
        self.SEQ = SEQ
        self.CTX = CTX
        self.FF = FF
        self.NE = NE
        self.CAP = CAP
        self.GRID_W = GRID_W
        self.HD = 64
        self.AW = D // 2
        self.NQ = self.AW // 64
        self.NKV = max(1, self.NQ // 8)
        self.GQ = self.NQ // self.NKV
        self.KVW = self.NKV * 64
        self.PW = D - self.AW
        self.NPG = 4
        self.PGW = self.PW // 4
        self.INW = self.AW + 2 * self.KVW + self.PW
        self.T = BATCH * SEQ // NCORES
        self.NT = self.T // 128
        self.KC = D // 128
        self.CPB = NCORES // BATCH
        self.NMODC = 6 * D // 128
        self.WCH = 256
        self.PIECE = max(self.KC * 256, (FF // 128) * 512)
        assert self.NT % 2 == 0 and self.NT >= 4
        assert CAP % 128 == 0


class Sem:
    def __init__(self, h):
        self.h = h
        self.count = 0
        self.sw = False


class Buf:
    __slots__ = ("name", "last_w", "readers")

    def __init__(self, name):
        self.name = name
        self.last_w = None
        self.readers = []


class Tl:
    def __init__(self, P, t, name, is_dram=False):
        self.t = t
        self.b = Buf(name)
        self.P = P
        self._ds = None
        self._dsw = None
        self.name = name
        self.is_dram = is_dram

    @property
    def ds(self):
        if self._ds is None:
            self._ds = self.P.sem("d_" + self.name)
        return self._ds

    @property
    def dsw(self):
        if self._dsw is None:
            self._dsw = self.P.sem("w_" + self.name)
            self._dsw.sw = True
        return self._dsw


class Prog:
    ENGS = ("pe", "act", "dve", "pool", "sp")

    def __init__(self, nc, stack):
        self.nc = nc
        self.stack = stack
        self.scope = stack
        self.q = {k: [] for k in self.ENGS}
        self.all_sems = []
        self.esem = {k: self.sem("e_" + k) for k in ("pe", "act", "dve", "pool")}
        self.seen = {k: {} for k in self.ENGS}
        self.n = 0
        self.bc_val = None
        self.bc_reg = None
        self.nblk = 0
        self.cond = None
        self.dummy = None

    def sem(self, name):
        s = Sem(self.stack.enter_context(self.nc.semaphore(name)))
        self.all_sems.append(s)
        return s

    def sb(self, name, shape, dtype):
        return Tl(self, self.scope.enter_context(self.nc.sbuf_tensor("s_" + name, list(shape), dtype)), name)

    def ps(self, name, shape, dtype):
        return Tl(self, self.scope.enter_context(self.nc.psum_tensor("p_" + name, list(shape), dtype)), name)

    def dram(self, t, name):
        return Tl(self, t, name, is_dram=True)

    def op(self, eng, fn, reads=(), writes=(), dsem=None):
        deps = []
        for tl in reads:
            b = tl.b
            if b.last_w is not None:
                deps.append(b.last_w)
        for tl in writes:
            b = tl.b
            if b.last_w is not None:
                deps.append(b.last_w)
            deps.extend(b.readers)
        waits = {}
        mysem = self.esem.get(eng)
        seen = self.seen[eng]
        for (s, v) in deps:
            if v is None:
                v = s.count
            if s is mysem and eng == "pe":
                continue
            if v <= seen.get(id(s), 0):
                continue
            if id(s) in waits and waits[id(s)][1] >= v:
                continue
            waits[id(s)] = (s, v)
        for s, v in waits.values():
            seen[id(s)] = v
        if dsem is not None:
            dsem.count += 16
            tok = (dsem, None)
            inc = (dsem, 16)
        else:
            s = self.esem[eng]
            s.count += 1
            tok = (s, s.count)
            inc = (s, 1)
        self.q[eng].append((list(waits.values()), fn, inc))
        if self.cond is not None:
            d = self.cond["comp"][eng]
            prev = d.get(id(inc[0]), (inc[0], 0))[1]
            d[id(inc[0])] = (inc[0], prev + inc[1])
        self.n += 1
        for tl in reads:
            tl.b.readers.append(tok)
        for tl in writes:
            tl.b.last_w = tok
            tl.b.readers = []

    def wait_only(self, eng, reads):
        waits = {}
        seen = self.seen[eng]
        for tl in reads:
            lw = tl.b.last_w
            if lw is None:
                continue
            s, v = lw
            if v is None:
                v = s.count
            if v <= seen.get(id(s), 0):
                continue
            waits[id(s)] = (s, v)
            seen[id(s)] = v
        self.q[eng].append((list(waits.values()), None, None))

    def cond_begin(self, flag_tl, flag_ap):
        assert self.cond is None
        for k in self.ENGS:
            self.wait_only(k, [flag_tl])
        self.cond = dict(flag_ap=flag_ap, outer_q=self.q, seen={k: dict(v) for k, v in self.seen.items()},
                         start={id(s): s.count for s in self.all_sems}, comp={k: {} for k in self.ENGS})
        self.q = {k: [] for k in self.ENGS}

    def cond_end(self):
        c = self.cond
        sub = self.q
        self.q = c["outer_q"]
        for k in self.ENGS:
            if sub[k]:
                comp = [(sm, c["start"].get(id(sm), 0), d) for (sm, d) in c["comp"][k].values()]
                self.q[k].append(("cond", c["flag_ap"], sub[k], comp))
        self.seen = c["seen"]
        self.cond = None

    def barrier(self):
        for k in self.ENGS:
            waits = []
            for s in self.all_sems:
                if s.count > self.seen[k].get(id(s), 0):
                    waits.append((s, s.count))
                    self.seen[k][id(s)] = s.count
            self.q[k].append((waits, None, None))

    def emit(self):
        nc = self.nc
        q = self.q
        if not any(q.values()):
            return

        self.nblk += 1
        nb = self.nblk

        def run_list(items, e, reg):
            for it in items:
                if it[0] == "cond":
                    _, flag_ap, sub, comp = it
                    e.reg_load(reg, flag_ap)
                    with e.If_ne(reg, 0):
                        run_list(sub, e, reg)
                    with e.Else():
                        prev = None
                        for sm, start, d in comp:
                            e.wait_ge(sm.h, start)
                            if sm.sw:
                                if prev is not None:
                                    e.wait_ge(prev[0].h, prev[1])
                                e.dma_start(out=self.dummy[0], in_=self.dummy[1]).then_inc(sm.h, d)
                                prev = (sm, start + d)
                            else:
                                e.sem_inc(sm.h, d)
                        if prev is not None:
                            e.wait_ge(prev[0].h, prev[1])
                    continue
                waits, fn, inc = it
                for sm, v in waits:
                    e.wait_ge(sm.h, v)
                if fn is None:
                    continue
                ins = fn(e)
                ins.then_inc(inc[0].h, inc[1])

        def run(k, e):
            with e.register(f"fl_{k}_{nb}") as reg:
                run_list(q[k], e, reg)

        with nc.Block() as block:
            @block.tensor
            def _(e):
                run("pe", e)

            @block.scalar
            def _(e):
                run("act", e)

            @block.vector
            def _(e):
                run("dve", e)

            @block.gpsimd
            def _(e):
                if self.bc_val is not None:
                    with e.register(f"bc{nb}") as bc:
                        e.reg_mov(bc, self.bc_val)
                        self.bc_reg = bc
                        run("pool", e)
                else:
                    run("pool", e)

            @block.sync
            def _(e):
                run("sp", e)
        self.q = {k: [] for k in self.ENGS}


STOP = None


def build_program(cfg, debug=False):
    c = cfg
    D, T, NT, KC, NE, FF, CAP = c.D, c.T, c.NT, c.KC, c.NE, c.FF, c.CAP
    NKV, AW, KVW, PW, PGW, NPG, INW = c.NKV, c.AW, c.KVW, c.PW, c.PGW, c.NPG, c.INW
    WCH = c.WCH
    NXT = NT + 2
    nc = bass.Bass("TRN2", target_bir_lowering=False)

    def din(name, shape, dt=F32):
        return nc.dram_tensor(name, list(shape), dt, kind="ExternalInput").ap()

    x_ext = din("x_ext", [NXT * 128, D])
    ctxb = din("ctxb", [c.CTX, D])
    cT_d = din("cT", [128, KC * 2])
    w_ada = din("w_ada", [D, 6 * D])
    b_adaT = din("b_adaT", [128, c.NMODC])
    n1T_d = din("n1T", [128, KC])
    n2T_d = din("n2T", [128, KC])
    w_in = din("w_in", [D, INW])
    gvec_d = din("gvec", [4 * 64])
    sinks_d = din("sinks", [c.NQ])
    w_pool = din("w_pool", [NPG, PGW, PGW])
    pscT_d = din("pscT", [128, PW // 128])
    w_out = din("w_out", [D, D])
    w_router = din("w_router", [D, NE])
    b_router = din("b_router", [NE])
    w_gate = din("w_gate", [NE, D, FF])
    w_up = din("w_up", [NE, D, FF])
    w_down = din("w_down", [NE, FF, D])
    bgT_d = din("bgT", [128, NE * (FF // 128)])
    buT_d = din("buT", [128, NE * (FF // 128)])
    b_down = din("b_down", [NE, D])
    rope_c = din("rope_c", [NXT * 128 + c.CTX, 64])
    rope_s = din("rope_s", [NXT * 128 + c.CTX, 64])
    masks_d = din("masks", [128, 4 * 128])
    poolB_d = din("poolB", [128, NPG * 9 * 128])
    cmat_d = din("cmat", [128, 3 * 128])
    ecap_d = din("ecap", [NE])
    y_out = nc.dram_tensor("y", [T, D], F32, kind="ExternalOutput").ap()
    x1kind = dict(kind="ExternalOutput") if debug else {}
    X1d = nc.dram_tensor("x1s", [T, D], F32, **x1kind).ap()
    HROWS = NE * T
    YW = max(512, D // 4)
    NYC = D // YW
    XEh = [nc.dram_tensor(f"xe_s{i}", [HROWS, D // 2], BF16).ap() for i in range(2)]
    YEq = [nc.dram_tensor(f"ye_s{i}", [HROWS, YW], F32).ap() for i in range(NYC)]
    G1d = nc.dram_tensor("g1b_s", [128, D], F32).ap()
    G2d = nc.dram_tensor("g2b_s", [128, D], F32).ap()
    if debug:
        LGd = nc.dram_tensor("lg_s", [T, NE], F32, kind="ExternalOutput").ap()

    top = ExitStack()
    with top:
        P = Prog(nc, top)
        X1 = P.dram(X1d, "X1")
        XE = P.dram(None, "XE")
        YE = P.dram(None, "YE")
        G1 = P.dram(G1d, "G1")
        G2 = P.dram(G2d, "G2")
        YO = P.dram(y_out, "YO")
        LGo = P.dram(None, "LGo")

        def ckpt(name):
            if STOP == name:
                P.barrier()
                P.emit()
                return True
            return False

        def dma(q, out_tl, out_ap, in_tl, in_ap, sem_tl=None, track_w=True, **kw):
            if sem_tl is None:
                sem_tl = out_tl if (out_tl is not None and not out_tl.is_dram) else in_tl
            reads = [in_tl] if in_tl is not None else []
            writes = [out_tl] if (out_tl is not None and track_w) else []
            sem = sem_tl.dsw if q == "pool" else sem_tl.ds
            P.op(q, lambda e: e.dma_start(out=out_ap, in_=in_ap, **kw), reads=reads, writes=writes,
                 dsem=sem)
            if out_tl is not None and not track_w:
                out_tl.b.last_w = (sem, None)

        A2 = P.sb("A2", [128, KC], F32)
        S2 = P.sb("S2", [128, KC], F32)
        with ExitStack() as scA:
            P.scope = scA
            NG = NT // 2
            cmat = P.sb("cmat", [128, 3, 128], BF16)
            identf = P.sb("identf", [128, 128], F32)
            onesf = P.sb("onesf", [128, 128], F32)
            masks = P.sb("masks", [128, 4, 128], BF16)
            poolB = P.sb("poolB", [128, NPG * 9, 128], BF16)
            gvec = P.sb("gvec", [128, 4, 64], F32)
            esink = P.sb("esink", [128, c.NQ], F32)
            cT = P.sb("cT", [128, KC, 2], F32)
            sig = P.sb("sig", [128, KC, 2], F32)
            scb = P.sb("scb", [128, KC, 2], BF16)
            badaT = P.sb("badaT", [128, c.NMODC], F32)
            MOD = P.sb("MOD", [128, c.NMODC, 2], F32)
            n1T = P.sb("n1T", [128, KC], F32)
            n2T = P.sb("n2T", [128, KC], F32)
            A1 = P.sb("A1", [128, KC], F32)
            A1c = P.sb("A1c", [128, KC], F32)
            pscT = P.sb("pscT", [128, PW // 128], F32)
            cst = P.sb("cst", [128, 1], F32)
            ident = cmat.t[:, 0, :]

            dma("pool", cmat, cmat.t[:], None, cmat_d.rearrange("p (a b) -> p a b", a=3), sem_tl=cst)
            dma("sp", identf, identf.t[:], None, cmat_d[:, 0:128], sem_tl=cst)
            dma("pool", masks, masks.t[:], None, masks_d.rearrange("p (a b) -> p a b", a=4), sem_tl=cst)
            dma("pool", poolB, poolB.t[:], None, poolB_d.rearrange("p (a b) -> p a b", b=128), sem_tl=cst)
            dma("sp", gvec, gvec.t[:], None,
                gvec_d.partition_broadcast(128).rearrange("p (a b) -> p a b", a=4), sem_tl=cst)
            dma("sp", esink, esink.t[:], None, sinks_d.partition_broadcast(128), sem_tl=cst)
            dma("sp", cT, cT.t[:], None, cT_d.rearrange("p (a b) -> p a b", b=2), sem_tl=cst)
            dma("sp", badaT, badaT.t[:], None, b_adaT, sem_tl=cst)
            dma("sp", n1T, n1T.t[:], None, n1T_d, sem_tl=cst)
            dma("sp", n2T, n2T.t[:], None, n2T_d, sem_tl=cst)
            dma("sp", pscT, pscT.t[:], None, pscT_d, sem_tl=cst)
            P.op("dve", lambda e: e.memset(onesf.t[:], 1.0), writes=[onesf])
            P.op("act", lambda e: e.activation(out=esink.t[:], in_=esink.t[:], func=AF.Exp), reads=[esink], writes=[esink])
            P.op("act", lambda e: e.activation(out=sig.t[:], in_=cT.t[:], func=AF.Sigmoid), reads=[cT], writes=[sig])
            P.op("dve", lambda e: e.tensor_tensor(out=scb.t[:], in0=cT.t[:], in1=sig.t[:], op=ALU.mult),
                 reads=[cT, sig], writes=[scb])

            TP = [P.ps(f"TP{i}", [128, 512], BF16) for i in range(2)]
            ACC = [P.ps(f"ACC{i}", [128, 512], F32) for i in range(2)]
            STp = [P.ps(f"ST{i}", [128, 512], F32) for i in range(2)]
            OAC = [P.ps(f"OAC{i}", [128, 4, 128], F32) for i in range(2)]
            rr = {"tp": 0, "acc": 0, "st": 0, "oac": 0, "w": 0, "ev": 0, "z": 0}

            def nxt(key, n):
                v = rr[key]
                rr[key] = (v + 1) % n
                return v

            NW = 3
            wring = [P.sb(f"wr{i}", [128, KC * WCH], BF16) for i in range(NW)]

            def load_w(src_ap, ncols):
                i = nxt("w", NW)
                sl = wring[i]
                view = sl.t[:, 0:KC * ncols].rearrange("p (k n) -> p k n", n=ncols)
                dma("pool", sl, view, None, src_ap.rearrange("(k p) n -> p k n", p=128))
                return sl, view

            modps = ACC[0]
            npc = WCH // 128
            for pi in range(6 * D // WCH):
                sl, wv = load_w(w_ada[:, pi * WCH:(pi + 1) * WCH], WCH)

                def f(e, wv=wv, pi=pi):
                    ins = None
                    for cc in range(npc):
                        j = pi * npc + cc
                        for k in range(KC):
                            ins = e.matmul(modps.t[:, 2 * j:2 * j + 2], lhsT=wv[:, k, cc * 128:(cc + 1) * 128],
                                           rhs=scb.t[:, k, :], start=(k == 0), stop=(k == KC - 1))
                    return ins
                P.op("pe", f, reads=[sl, scb], writes=[modps])
            P.op("dve", lambda e: e.tensor_tensor(
                out=MOD.t[:], in0=modps.t[:, 0:2 * c.NMODC].rearrange("p (j r) -> p j r", r=2),
                in1=badaT.t[:, :].unsqueeze(2).to_broadcast([128, c.NMODC, 2]), op=ALU.add),
                reads=[modps, badaT], writes=[MOD])

            def modv(idx, r=0):
                return MOD.t[:, idx * KC:(idx + 1) * KC, r]
            P.op("dve", lambda e: e.scalar_tensor_tensor(out=A1.t[:], in0=modv(1, 0), scalar=1.0, in1=n1T.t[:],
                                                         op0=ALU.add, op1=ALU.mult), reads=[MOD, n1T], writes=[A1])
            P.op("dve", lambda e: e.scalar_tensor_tensor(out=A1c.t[:], in0=modv(1, 1), scalar=1.0, in1=n1T.t[:],
                                                         op0=ALU.add, op1=ALU.mult), reads=[MOD, n1T], writes=[A1c])

            diag = [P.sb(f"diag{i}", [128, 128], F32) for i in range(2)]
            gpc = [P.sb(f"gpc{i}", [128, 512], F32) for i in range(2)]
            for gi, (midx, Gd, Gt) in enumerate(((2, G1d, G1), (5, G2d, G2))):
                for n4 in range(D // 512):
                    acc = ACC[1]
                    for q4 in range(4):
                        ch = n4 * 4 + q4
                        dg = diag[ch % 2]
                        P.op("dve", lambda e, dg=dg, ch=ch, midx=midx: e.tensor_scalar(
                            out=dg.t[:], in0=identf.t[:], scalar1=MOD.t[:, midx * KC + ch, 0:1], scalar2=0.0,
                            op0=ALU.mult, op1=ALU.add), reads=[identf, MOD], writes=[dg])
                        P.op("pe", lambda e, dg=dg, q4=q4, acc=acc: e.matmul(
                            acc.t[:, q4 * 128:(q4 + 1) * 128], lhsT=onesf.t[:], rhs=dg.t[:], start=True, stop=True),
                            reads=[onesf, dg], writes=[acc])
                    gp = gpc[n4 % 2]
                    P.op("act", lambda e, gp=gp, acc=acc: e.activation(out=gp.t[:], in_=acc.t[:], func=AF.Copy),
                         reads=[acc], writes=[gp])
                    dma("sp", Gt, Gd[:, n4 * 512:(n4 + 1) * 512], gp, gp.t[:], sem_tl=Gt)

            if ckpt("setup"):
                return nc
            NL = 4
            xt = P.sb("xt", [128, D], F32)
            xs = P.sb("xs", [128, D], BF16)
            ss = P.sb("ss", [128, 2], F32)
            hT = P.sb("hT", [128, NL, KC, 128], BF16)
            mixT = P.sb("mixT", [128, KC, 256], BF16)
            kT = P.sb("kT", [64, NL + 2, NKV, 128], BF16)
            Vg = P.sb("Vg", [128, NL + 2, NKV, 65], BF16)
            rc = P.sb("rc", [128, NL, 64], F32)
            rs = P.sb("rs", [128, NL, 64], F32)
            tabs = P.sb("tabs", [128, 4, NL, 64], F32)
            zs = [P.sb(f"zs{i}", [128, 256], F32) for i in range(2)]
            sq = P.sb("sq", [128, 256], F32)
            t1 = P.sb("t1", [128, 256], F32)
            t2 = P.sb("t2", [128, 256], F32)
            hs = P.sb("hs", [128, 8], F32)
            qrot = [P.sb(f"qrot{i}", [128, 256], BF16) for i in range(2)]
            qT4 = [P.sb(f"qT4{i}", [64, 4, 128], BF16) for i in range(2)]
            Pt = [P.sb(f"Pt{i}", [128, 512], BF16) for i in range(3)]
            den = P.sb("den", [128, 8], F32)
            attn = [P.sb(f"attn{i}", [128, 256], BF16) for i in range(2)]
            Ubuf = P.sb("Ubuf", [128, NL, PGW], BF16)
            dT = P.sb("dT", [128, PGW // 128, 256], BF16)
            wpl = [P.sb(f"wpl{i}", [128, PGW // 128, PGW], BF16) for i in range(2)]
            xp = [P.sb(f"xp{i}", [128, 2, WCH], F32) for i in range(2)]
            g1p = [P.sb(f"g1p{i}", [128, WCH], F32) for i in range(2)]
            ytmp = [P.sb(f"ytmp{i}", [128, WCH], F32) for i in range(2)]
            x1p = [P.sb(f"x1p{i}", [128, 2, WCH], F32) for i in range(2)]
            P.op("dve", lambda e: e.memset(Vg.t[:], 1.0), writes=[Vg])
            P.op("dve", lambda e: e.memset(xs.t[:], 0.0), writes=[xs])
            if debug:
                P.op("dve", lambda e: e.memset(xt.t[:], 0.0), writes=[xt])
                for r in range(HROWS // 128):
                    for hh in range(2):
                        dma("sp", XE, XEh[hh][r * 128:(r + 1) * 128, :], xs, xs.t[:, 0:D // 2], sem_tl=xs, track_w=False)
                    for hc in range(NYC):
                        dma("sp", YE, YEq[hc][r * 128:(r + 1) * 128, :], xt, xt.t[:, 0:YW], sem_tl=xt, track_w=False)

            def evac_engine():
                return ("act", "dve")[nxt("ev", 2)]

            def copy_op(eng, out_ap, in_ap, reads, writes):
                if eng == "act":
                    P.op("act", lambda e: e.activation(out=out_ap, in_=in_ap, func=AF.Copy), reads=reads, writes=writes)
                else:
                    P.op(eng, lambda e: e.tensor_copy(out_ap, in_ap), reads=reads, writes=writes)

            def affine_op(eng, out_ap, in_ap, sc_ap, bi_ap, reads, writes):
                if eng == "act":
                    P.op("act", lambda e: e.activation(out=out_ap, in_=in_ap, func=AF.Identity, bias=bi_ap, scale=sc_ap),
                         reads=reads, writes=writes)
                else:
                    P.op(eng, lambda e: e.tensor_scalar(out=out_ap, in0=in_ap, scalar1=sc_ap, scalar2=bi_ap,
                                                        op0=ALU.mult, op1=ALU.add), reads=reads, writes=writes)

            def norm_tile(src_tl, dst_tl, ss_col):
                P.op("dve", lambda e: e.memset(ss.t[:, ss_col:ss_col + 1], 0.0), writes=[ss])
                P.op("act", lambda e: e.activation(out=dst_tl.t[:], in_=src_tl.t[:], func=AF.Square,
                                                   accum_out=ss.t[:, ss_col:ss_col + 1]),
                     reads=[src_tl], writes=[dst_tl, ss])
                P.op("act", lambda e: e.activation(out=ss.t[:, ss_col:ss_col + 1], in_=ss.t[:, ss_col:ss_col + 1],
                                                   func=AF.Sqrt, bias=cst_eps.t[:, 0:1], scale=1.0 / D),
                     reads=[ss, cst_eps], writes=[ss])
                P.op("dve", lambda e: e.reciprocal(out=ss.t[:, ss_col:ss_col + 1], in_=ss.t[:, ss_col:ss_col + 1]),
                     reads=[ss], writes=[ss])
                P.op("dve", lambda e: e.tensor_scalar(out=dst_tl.t[:], in0=src_tl.t[:], scalar1=ss.t[:, ss_col:ss_col + 1],
                                                      scalar2=0.0, op0=ALU.mult, op1=ALU.add),
                     reads=[src_tl, ss], writes=[dst_tl])

            cst_eps = P.sb("cst_eps", [128, 2], F32)
            P.op("dve", lambda e: e.memset(cst_eps.t[:, 0:1], NORM_EPS), writes=[cst_eps])
            P.op("dve", lambda e: e.memset(cst_eps.t[:, 1:2], NORM_EPS), writes=[cst_eps])

            def transposes_to(src_tl, nchunks, dst_fn, sc_fn, bi_fn, dst_tl, extra_reads=()):
                for c0 in range(0, nchunks, 4):
                    tp = TP[nxt("tp", 2)]
                    nn = min(4, nchunks - c0)

                    def f(e, c0=c0, nn=nn, tp=tp):
                        ins = None
                        for i in range(nn):
                            ins = e.transpose(tp.t[:, i * 128:(i + 1) * 128], src_tl.t[:, (c0 + i) * 128:(c0 + i + 1) * 128], ident)
                        return ins
                    P.op("pe", f, reads=[src_tl, cmat], writes=[tp])
                    for i in range(nn):
                        cc = c0 + i
                        eng = evac_engine()
                        if sc_fn is None:
                            copy_op(eng, dst_fn(cc), tp.t[:, i * 128:(i + 1) * 128], [tp], [dst_tl])
                        else:
                            affine_op(eng, dst_fn(cc), tp.t[:, i * 128:(i + 1) * 128], sc_fn(cc), bi_fn(cc),
                                      [tp, MOD, *extra_reads], [dst_tl])

            def stage1(row0, l, Avec, shift_r):
                src = x_ext if row0 >= 0 else ctxb
                r0 = row0 if row0 >= 0 else (-row0 - 1)
                dma("sp", xt, xt.t[:], None, src[r0:r0 + 128, :])
                norm_tile(xt, xs, 0)
                transposes_to(xs, KC, lambda cc: hT.t[:, l, cc, :],
                              lambda cc: Avec.t[:, cc:cc + 1], lambda cc: MOD.t[:, 0 * KC + cc, shift_r:shift_r + 1],
                              hT, extra_reads=[Avec])

            def project(l, wsl, wv, ncols):
                acc = ACC[nxt("acc", 2)]

                def f(e):
                    ins = None
                    for k in range(KC):
                        ins = e.matmul(acc.t[:, 0:ncols], lhsT=hT.t[:, l, k, :], rhs=wv[:, k, :],
                                       start=(k == 0), stop=(k == KC - 1))
                    return ins
                P.op("pe", f, reads=[hT, wsl], writes=[acc])
                return acc

            def rms_rope(acc, nh, l, tA, tB, out_aps):
                w = nh * 64
                z = zs[nxt("z", 2)]
                P.op("act", lambda e: e.activation(out=z.t[:, 0:w], in_=acc.t[:, 0:w], func=AF.Copy), reads=[acc], writes=[z])
                P.op("act", lambda e: e.activation(out=sq.t[:, 0:w], in_=z.t[:, 0:w], func=AF.Square), reads=[z], writes=[sq])
                P.op("dve", lambda e: e.tensor_reduce(out=hs.t[:, 0:nh], in_=sq.t[:, 0:w].rearrange("p (h d) -> p h d", d=64),
                                                      axis=AX.X, op=ALU.add), reads=[sq], writes=[hs])
                P.op("act", lambda e: e.activation(out=hs.t[:, 0:nh], in_=hs.t[:, 0:nh], func=AF.Sqrt,
                                                   bias=cst_eps.t[:, 1:2], scale=1.0 / 64), reads=[hs, cst_eps], writes=[hs])
                P.op("dve", lambda e: e.reciprocal(out=hs.t[:, 0:nh], in_=hs.t[:, 0:nh]), reads=[hs], writes=[hs])
                z3 = z.t[:, 0:w].rearrange("p (h d) -> p h d", d=64)
                z5 = z.t[:, 0:w].rearrange("p (h a b d) -> p h a b d", a=2, b=2, d=16)
                t13 = t1.t[:, 0:w].rearrange("p (h d) -> p h d", d=64)
                t25 = t2.t[:, 0:w].rearrange("p (h a b d) -> p h a b d", a=2, b=2, d=16)
                A_b = tabs.t[:, tA, l, :].unsqueeze(1).to_broadcast([128, nh, 64])
                B5 = tabs.t[:, tB, l, :].rearrange("p (a b d) -> p a b d", a=2, b=2)
                P.op("dve", lambda e: e.tensor_tensor(out=t13, in0=z3, in1=A_b, op=ALU.mult), reads=[z, tabs], writes=[t1])
                for b in range(2):
                    P.op("dve", lambda e, b=b: e.tensor_tensor(
                        out=t25[:, :, :, b, :], in0=z5[:, :, :, 1 - b, :],
                        in1=B5[:, :, b, :].unsqueeze(1).to_broadcast([128, nh, 2, 16]), op=ALU.mult),
                        reads=[z, tabs], writes=[t2])
                P.op("dve", lambda e: e.tensor_tensor(out=t1.t[:, 0:w], in0=t1.t[:, 0:w], in1=t2.t[:, 0:w], op=ALU.add),
                     reads=[t1, t2], writes=[t1])
                for (otl, oap) in out_aps:
                    P.op("dve", lambda e, oap=oap: e.tensor_tensor(
                        out=oap, in0=t13, in1=hs.t[:, 0:nh].unsqueeze(2).to_broadcast([128, nh, 64]), op=ALU.mult),
                        reads=[t1, hs], writes=[otl])

            def load_tables(row0, nl, l0):
                dma("sp", rc, rc.t[:, 0:nl, :], None, rope_c[row0:row0 + nl * 128, :].rearrange("(l p) d -> p l d", p=128))
                dma("sp", rs, rs.t[:, 0:nl, :], None, rope_s[row0:row0 + nl * 128, :].rearrange("(l p) d -> p l d", p=128))
                for ti, (src, gi) in enumerate(((rc, 0), (rs, 1), (rc, 2), (rs, 3))):
                    P.op("dve", lambda e, ti=ti, src=src, gi=gi: e.tensor_tensor(
                        out=tabs.t[:, ti, l0:l0 + nl, :], in0=src.t[:, 0:nl, :],
                        in1=gvec.t[:, gi, :].unsqueeze(1).to_broadcast([128, nl, 64]), op=ALU.mult),
                        reads=[src, gvec], writes=[tabs])

            def do_k(acc, l, slot):
                kr = qrot[nxt("oac", 2)]
                rms_rope(acc, NKV, l, 2, 3, [(kr, kr.t[:, 0:NKV * 64].rearrange("p (h d) -> p h d", d=64))])
                tp = TP[nxt("tp", 2)]

                def f(e):
                    ins = None
                    for h in range(NKV):
                        ins = e.transpose(tp.t[0:64, h * 128:(h + 1) * 128], kr.t[:, h * 64:(h + 1) * 64], ident)
                    return ins
                P.op("pe", f, reads=[kr, cmat], writes=[tp])
                copy_op(evac_engine(), kT.t[0:64, slot, :, :], tp.t[0:64, 0:NKV * 128].rearrange("p (h t) -> p h t", t=128),
                        [tp], [kT])

            def do_v(acc, slot):
                copy_op(evac_engine(), Vg.t[:, slot, :, 0:64], acc.t[:, 0:NKV * 64].rearrange("p (h d) -> p h d", d=64),
                        [acc], [Vg])

            load_tables(NXT * 128, 2, 0)
            kwid = min(WCH, KVW)
            for ci in range(c.CTX // 128):
                stage1(-(ci * 128) - 1, ci, A1c, 1)
            for kc0 in range(0, KVW, kwid):
                assert kwid == KVW, "k/v chunking assumes KVW <= 256"
            wsl, wv = load_w(w_in[:, AW:AW + KVW], KVW)
            for ci in range(c.CTX // 128):
                acc = project(ci, wsl, wv, KVW)
                do_k(acc, ci, NL + ci)
            wsl, wv = load_w(w_in[:, AW + KVW:AW + 2 * KVW], KVW)
            for ci in range(c.CTX // 128):
                acc = project(ci, wsl, wv, KVW)
                do_v(acc, NL + ci)

            if ckpt("ctx"):
                return nc
            scale = float(c.HD) ** -0.5
            nqc = AW // WCH
            nuc = max(1, PGW // WCH)
            ucw = min(WCH, PGW)
            for g in range(NG):
                own0 = 2 * g
                load_tables((own0) * 128, NL, 0)
                for l in range(NL):
                    stage1((own0 + l) * 128, l, A1, 0)
                wsl, wv = load_w(w_in[:, AW:AW + KVW], KVW)
                for l in range(NL):
                    acc = project(l, wsl, wv, KVW)
                    do_k(acc, l, l)
                wsl, wv = load_w(w_in[:, AW + KVW:AW + 2 * KVW], KVW)
                for l in range(NL):
                    acc = project(l, wsl, wv, KVW)
                    do_v(acc, l)
                if g == 0 and ckpt("g0kv"):
                    return nc
                for pg in range(NPG):
                    wp = wpl[pg % 2]
                    dma("pool", wp, wp.t[:], None, w_pool[pg].rearrange("(k p) n -> p k n", p=128))
                    for uc in range(nuc):
                        col0 = AW + 2 * KVW + pg * PGW + uc * ucw
                        wsl, wv = load_w(w_in[:, col0:col0 + ucw], ucw)
                        for l in range(NL):
                            acc = project(l, wsl, wv, ucw)
                            copy_op(evac_engine(), Ubuf.t[:, l, uc * ucw:(uc + 1) * ucw], acc.t[:, 0:ucw], [acc], [Ubuf])
                    for lo in range(2):
                        l = 1 + lo
                        own = own0 + lo
                        var = 0 if own == 0 else (2 if own == NT - 1 else 1)
                        for cc in range(PGW // 128):
                            acc = ACC[nxt("acc", 2)]

                            def f(e, acc=acc, l=l, cc=cc, var=var, pg=pg):
                                ins = None
                                for kd in range(3):
                                    ins = e.matmul(acc.t[:, 0:128], lhsT=Ubuf.t[:, l - 1 + kd, cc * 128:(cc + 1) * 128],
                                                   rhs=poolB.t[:, pg * 9 + var * 3 + kd, :], start=(kd == 0), stop=(kd == 2))
                                return ins
                            P.op("pe", f, reads=[Ubuf, poolB], writes=[acc])
                            copy_op(evac_engine(), dT.t[:, cc, lo * 128:(lo + 1) * 128], acc.t[:, 0:128], [acc], [dT])
                    for co in range(PGW // 128):
                        acc = ACC[nxt("acc", 2)]

                        def f(e, acc=acc, co=co, wp=wp):
                            ins = None
                            nk = PGW // 128
                            for ci in range(nk):
                                ins = e.matmul(acc.t[:, 0:256], lhsT=wp.t[:, ci, co * 128:(co + 1) * 128], rhs=dT.t[:, ci, :],
                                               start=(ci == 0), stop=(ci == nk - 1))
                            return ins
                        P.op("pe", f, reads=[wp, dT], writes=[acc])
                        pj = pg * (PGW // 128) + co
                        P.op("act", lambda e, acc=acc, pj=pj: e.activation(
                            out=mixT.t[:, AW // 128 + pj, :], in_=acc.t[:, 0:256], func=AF.Identity, scale=pscT.t[:, pj:pj + 1]),
                            reads=[acc, pscT], writes=[mixT])
                if g == 0 and ckpt("g0pool"):
                    return nc
                for qc in range(nqc):
                    wsl, wv = load_w(w_in[:, qc * WCH:(qc + 1) * WCH], WCH)
                    kvh = (qc * 4) // c.GQ
                    for lo in range(2):
                        l = 1 + lo
                        own = own0 + lo
                        acc = project(l, wsl, wv, WCH)
                        qr = qrot[nxt("oac", 2)]
                        rms_rope(acc, 4, l, 0, 1, [(qr, qr.t[:].rearrange("p (h d) -> p h d", d=64))])
                        tp = TP[nxt("tp", 2)]

                        def f(e, tp=tp, qr=qr):
                            ins = None
                            for h in range(4):
                                ins = e.transpose(tp.t[0:64, h * 128:(h + 1) * 128], qr.t[:, h * 64:(h + 1) * 64], ident)
                            return ins
                        P.op("pe", f, reads=[qr, cmat], writes=[tp])
                        q4 = qT4[nxt("st", 2)]
                        copy_op(evac_engine(), q4.t[:], tp.t[0:64, :].rearrange("p (h t) -> p h t", t=128), [tp], [q4])
                        oac = OAC[lo]
                        blocks = [(l - 1, 0 if own == 0 else 1), (l, None), (l + 1, 3 if own == NT - 1 else 2),
                                  (NL, None), (NL + 1, None)][:3 + c.CTX // 128]
                        nb = len(blocks)
                        for bi, (slot, mk) in enumerate(blocks):
                            st = STp[bi % 2]
                            P.op("pe", lambda e, st=st, slot=slot, q4=q4, kvh=kvh: e.matmul(
                                st.t[:], lhsT=kT.t[0:64, slot, kvh, :], rhs=q4.t[:].rearrange("p h t -> p (h t)"),
                                start=True, stop=True), reads=[kT, q4], writes=[st])
                            pt = Pt[bi % 3]
                            P.op("act", lambda e, st=st, pt=pt: e.activation(out=pt.t[:], in_=st.t[:], func=AF.Exp, scale=scale),
                                 reads=[st], writes=[pt])
                            if mk is not None:
                                P.op("dve", lambda e, pt=pt, mk=mk: e.tensor_tensor(
                                    out=pt.t[:].rearrange("p (h t) -> p h t", t=128),
                                    in0=pt.t[:].rearrange("p (h t) -> p h t", t=128),
                                    in1=masks.t[:, mk, :].unsqueeze(1).to_broadcast([128, 4, 128]), op=ALU.mult),
                                    reads=[pt, masks], writes=[pt])

                            def f(e, pt=pt, slot=slot, bi=bi, oac=oac, kvh=kvh, nb=nb):
                                ins = None
                                for h in range(4):
                                    ins = e.matmul(oac.t[:, h, 0:65], lhsT=pt.t[:, h * 128:(h + 1) * 128],
                                                   rhs=Vg.t[:, slot, kvh, :], start=(bi == 0 and h == 0),
                                                   stop=(bi == nb - 1 and h == 3), skip_group_check=True)
                                return ins
                            P.op("pe", f, reads=[pt, Vg], writes=[oac])
                        P.op("dve", lambda e, oac=oac, qc=qc: e.tensor_tensor(
                            out=den.t[:, 0:4], in0=oac.t[:, :, 64], in1=esink.t[:, qc * 4:qc * 4 + 4], op=ALU.add),
                            reads=[oac, esink], writes=[den])
                        P.op("dve", lambda e: e.reciprocal(out=den.t[:, 0:4], in_=den.t[:, 0:4]), reads=[den], writes=[den])
                        at = attn[lo]
                        P.op("dve", lambda e, oac=oac, at=at: e.tensor_tensor(
                            out=at.t[:].rearrange("p (h d) -> p h d", d=64), in0=oac.t[:, :, 0:64],
                            in1=den.t[:, 0:4].unsqueeze(2).to_broadcast([128, 4, 64]), op=ALU.mult),
                            reads=[oac, den], writes=[at])
                        tp = TP[nxt("tp", 2)]

                        def f(e, tp=tp, at=at):
                            ins = None
                            for i in range(2):
                                ins = e.transpose(tp.t[:, i * 128:(i + 1) * 128], at.t[:, i * 128:(i + 1) * 128], ident)
                            return ins
                        P.op("pe", f, reads=[at, cmat], writes=[tp])
                        copy_op(evac_engine(), mixT.t[:, 2 * qc:2 * qc + 2, lo * 128:(lo + 1) * 128],
                                tp.t[:, 0:256].rearrange("p (c t) -> p c t", t=128), [tp], [mixT])
                if g == 0 and ckpt("g0attn"):
                    return nc
                for n in range(D // WCH):
                    wsl, wv = load_w(w_out[:, n * WCH:(n + 1) * WCH], WCH)
                    xq = xp[n % 2]
                    gq = g1p[n % 2]
                    xo = x1p[n % 2]
                    r0 = (own0 + 1) * 128
                    dma("sp", xq, xq.t[:], None, x_ext[r0:r0 + 256, n * WCH:(n + 1) * WCH].rearrange("(l p) n -> p l n", p=128))
                    dma("sp", gq, gq.t[:], G1, G1d[:, n * WCH:(n + 1) * WCH])
                    for lo in range(2):
                        acc = ACC[nxt("acc", 2)]

                        def f(e, acc=acc, lo=lo, wv=wv):
                            ins = None
                            for k in range(KC):
                                ins = e.matmul(acc.t[:, 0:WCH], lhsT=mixT.t[:, k, lo * 128:(lo + 1) * 128], rhs=wv[:, k, :],
                                               start=(k == 0), stop=(k == KC - 1))
                            return ins
                        P.op("pe", f, reads=[mixT, wsl], writes=[acc])
                        yt = ytmp[lo]
                        P.op("dve", lambda e, acc=acc, yt=yt, gq=gq: e.tensor_tensor(out=yt.t[:], in0=acc.t[:, 0:WCH], in1=gq.t[:], op=ALU.mult),
                             reads=[acc, gq], writes=[yt])
                        P.op("dve", lambda e, yt=yt, xq=xq, xo=xo, lo=lo: e.tensor_tensor(out=xo.t[:, lo, :], in0=yt.t[:], in1=xq.t[:, lo, :], op=ALU.add),
                             reads=[yt, xq], writes=[xo])
                    dma("sp", X1, X1d[own0 * 128:own0 * 128 + 256, n * WCH:(n + 1) * WCH].rearrange("(l p) n -> p l n", p=128),
                        xo, xo.t[:], sem_tl=xo, track_w=False)

            P.op("dve", lambda e: e.scalar_tensor_tensor(out=A2.t[:], in0=modv(4, 0), scalar=1.0, in1=n2T.t[:],
                                                         op0=ALU.add, op1=ALU.mult), reads=[MOD, n2T], writes=[A2])
            P.op("dve", lambda e: e.tensor_copy(S2.t[:], modv(3, 0)), reads=[MOD], writes=[S2])
            P.barrier()
            P.emit()

        if STOP != "phaseA":
            phaseB(P, c, nc, locals())
        P.emit()
    return nc


def phaseB(P, c, nc, L):
    D, T, NT, KC, NE, FF, CAP = c.D, c.T, c.NT, c.KC, c.NE, c.FF, c.CAP
    X1, XE, YE, G2, YO = L["X1"], L["XE"], L["YE"], L["G2"], L["YO"]
    X1d, XEh, YEq, G2d, y_out = L["X1d"], L["XEh"], L["YEq"], L["G2d"], L["y_out"]
    HROWS, YW, NYC = L["HROWS"], L["YW"], L["NYC"]
    CH = T // 4
    NS = CH // 128
    A2, S2 = L["A2"], L["S2"]
    dma = L["dma"]
    debug = L["debug"]
    FK = FF // 128
    P.bc_val = HROWS - 1
    with ExitStack() as scP:
        P.scope = scP
        SLi = P.sb("SLi", [128, NT, 4], I32)
        SLi1 = P.sb("SLi1", [128, NT, 4], I32)
        FLi = P.sb("FLi", [1, 3 * NE], I32)
        dmy = P.sb("dmy", [1, NE], F32)
        P.dummy = (dmy.t[0:1, :], L["ecap_d"].rearrange("(a n) -> a n", a=1))
        WT = P.sb("WT", [128, NT, 4], F32)
        combT = P.sb("combT", [32, NT, 128], BF16)
        cmat = P.sb("cmatB", [128, 3, 128], BF16)
        cstB = P.sb("cstB", [128, 2], F32)
        ident = cmat.t[:, 0, :]
        dma("pool", cmat, cmat.t[:], None, L["cmat_d"].rearrange("p (a b) -> p a b", a=3), sem_tl=cstB)
        P.op("dve", lambda e: e.memset(cstB.t[:, 0:1], NORM_EPS), writes=[cstB])

        with ExitStack() as scB:
            P.scope = scB
            TP = [P.ps(f"TPb{i}", [128, 512], BF16) for i in range(2)]
            GU = [P.ps(f"GU{i}", [128, 512], F32) for i in range(4)]
            DN = [P.ps(f"DN{i}", [128, 512], F32) for i in range(2)]
            RT = DN[0]
            rr = {"tp": 0, "ev": 0, "w": 0, "dn": 0, "ys": 0, "gu": 0}

            def nxt(key, n):
                v = rr[key]
                rr[key] = (v + 1) % n
                return v

            def evac_engine():
                return ("act", "dve")[nxt("ev", 2)]

            wr_bf = P.sb("wr_bf", [128, KC, NE], BF16)
            brt = P.sb("brt", [128, NE], F32)
            ecap = P.sb("ecap", [128, NE], F32)
            bgT = P.sb("bgT", [128, NE * FK], F32)
            buT = P.sb("buT", [128, NE * FK], F32)
            Rf = P.sb("Rf", [128, NE], F32)
            Rb = P.sb("Rb", [128, NE], BF16)
            dma("pool", wr_bf, wr_bf.t[:], None, L["w_router"].rearrange("(k p) n -> p k n", p=128), sem_tl=cstB)
            dma("sp", brt, brt.t[:], None, L["b_router"].partition_broadcast(128), sem_tl=cstB)
            dma("sp", ecap, ecap.t[:], None, L["ecap_d"].partition_broadcast(128), sem_tl=cstB)
            dma("sp", bgT, bgT.t[:], None, L["bgT_d"], sem_tl=cstB)
            dma("sp", buT, buT.t[:], None, L["buT_d"], sem_tl=cstB)
            P.op("dve", lambda e: e.memset(Rf.t[:], 0.0), writes=[Rf])

            scB1 = ExitStack()
            P.scope = scB1
            x1t = P.sb("x1t", [128, D], F32)
            xs2 = [P.sb(f"xs2{i}", [128, D], BF16) for i in range(2)]
            ss = P.sb("ssB", [128, 1], F32)
            h2T = P.sb("h2T", [128, KC, 128], BF16)
            lg = P.sb("lg", [128, NE], F32)
            top8 = P.sb("top8", [128, 8], F32)
            mask = P.sb("mask", [128, NE], F32)
            maskb = P.sb("maskb", [128, NE], BF16)
            slotf = P.sb("slotf", [128, NE], F32)
            ovf = P.sb("ovf", [128, NE], F32)
            eq = P.sb("eq", [128, NE], F32)
            SLf = P.sb("SLf", [128, 4], F32)
            wex = P.sb("wex", [128, 4], F32)
            wsum = P.sb("wsum", [128, 1], F32)
            ntop = P.sb("ntop", [128, 1], F32)
            comb = P.sb("comb", [128, NE], F32)
            combb = P.sb("combb", [128, NE], BF16)

            for j in range(NT):
                xs_ = xs2[j % 2]
                dma("sp", x1t, x1t.t[:], X1, X1d[j * 128:(j + 1) * 128, :])
                P.op("dve", lambda e: e.memset(ss.t[:], 0.0), writes=[ss])
                P.op("act", lambda e, xs_=xs_: e.activation(out=xs_.t[:], in_=x1t.t[:], func=AF.Square, accum_out=ss.t[:, 0:1]),
                     reads=[x1t], writes=[xs_, ss])
                P.op("act", lambda e: e.activation(out=ss.t[:], in_=ss.t[:], func=AF.Sqrt, bias=cstB.t[:, 0:1], scale=1.0 / D),
                     reads=[ss, cstB], writes=[ss])
                P.op("dve", lambda e: e.reciprocal(out=ss.t[:], in_=ss.t[:]), reads=[ss], writes=[ss])
                P.op("dve", lambda e, xs_=xs_: e.tensor_scalar(out=xs_.t[:], in0=x1t.t[:], scalar1=ss.t[:, 0:1], scalar2=0.0,
                                                              op0=ALU.mult, op1=ALU.add), reads=[x1t, ss], writes=[xs_])
                for c0 in range(0, KC, 4):
                    tp = TP[nxt("tp", 2)]

                    def f(e, c0=c0, tp=tp, xs_=xs_):
                        ins = None
                        for i in range(4):
                            ins = e.transpose(tp.t[:, i * 128:(i + 1) * 128], xs_.t[:, (c0 + i) * 128:(c0 + i + 1) * 128], ident)
                        return ins
                    P.op("pe", f, reads=[xs_, cmat], writes=[tp])
                    for i in range(4):
                        cc = c0 + i
                        if evac_engine() == "act":
                            P.op("act", lambda e, tp=tp, i=i, cc=cc: e.activation(
                                out=h2T.t[:, cc, :], in_=tp.t[:, i * 128:(i + 1) * 128], func=AF.Identity,
                                bias=S2.t[:, cc:cc + 1], scale=A2.t[:, cc:cc + 1]), reads=[tp, A2, S2], writes=[h2T])
                        else:
                            P.op("dve", lambda e, tp=tp, i=i, cc=cc: e.tensor_scalar(
                                out=h2T.t[:, cc, :], in0=tp.t[:, i * 128:(i + 1) * 128], scalar1=A2.t[:, cc:cc + 1],
                                scalar2=S2.t[:, cc:cc + 1], op0=ALU.mult, op1=ALU.add), reads=[tp, A2, S2], writes=[h2T])

                def f(e):
                    ins = None
                    for k in range(KC):
                        ins = e.matmul(RT.t[:, 0:NE], lhsT=h2T.t[:, k, :], rhs=wr_bf.t[:, k, :], start=(k == 0), stop=(k == KC - 1))
                    return ins
                P.op("pe", f, reads=[h2T, wr_bf], writes=[RT])
                P.op("dve", lambda e: e.tensor_tensor(out=lg.t[:], in0=RT.t[:, 0:NE], in1=brt.t[:], op=ALU.add),
                     reads=[RT, brt], writes=[lg])
                if debug:
                    dma("sp", L["LGo"], L["LGd"][j * 128:(j + 1) * 128, :], lg, lg.t[:], sem_tl=lg)
                P.op("dve", lambda e: e.max(out=top8.t[:], in_=lg.t[:]), reads=[lg], writes=[top8])
                P.op("dve", lambda e: e.tensor_scalar(out=mask.t[:], in0=lg.t[:], scalar1=top8.t[:, 3:4], scalar2=0.0,
                                                      op0=ALU.is_ge, op1=ALU.add), reads=[lg, top8], writes=[mask])
                P.op("dve", lambda e: e.tensor_copy(maskb.t[:], mask.t[:]), reads=[mask], writes=[maskb])
                P.op("dve", lambda e: e.tensor_copy(Rb.t[:], Rf.t[:]), reads=[Rf], writes=[Rb])

                def f(e):
                    e.matmul(RT.t[:, 64:64 + NE], lhsT=cmat.t[:, 1, :], rhs=maskb.t[:], start=True, stop=False)
                    return e.matmul(RT.t[:, 64:64 + NE], lhsT=cmat.t[:, 2, :], rhs=Rb.t[:], start=False, stop=True)
                P.op("pe", f, reads=[cmat, maskb, Rb], writes=[RT])
                P.op("dve", lambda e: e.tensor_tensor(out=Rf.t[:], in0=Rf.t[:], in1=mask.t[:], op=ALU.add),
                     reads=[Rf, mask], writes=[Rf])
                P.op("dve", lambda e: e.tensor_tensor(out=slotf.t[:], in0=RT.t[:, 64:64 + NE], in1=ecap.t[:], op=ALU.add),
                     reads=[RT, ecap], writes=[slotf])
                for k in range(4):
                    P.op("dve", lambda e, k=k: e.tensor_scalar(out=eq.t[:], in0=lg.t[:], scalar1=top8.t[:, k:k + 1], scalar2=0.0,
                                                               op0=ALU.is_equal, op1=ALU.add), reads=[lg, top8], writes=[eq])
                    P.op("dve", lambda e: e.tensor_tensor(out=eq.t[:], in0=eq.t[:], in1=slotf.t[:], op=ALU.mult),
                         reads=[eq, slotf], writes=[eq])
                    P.op("dve", lambda e, k=k: e.tensor_reduce(out=SLf.t[:, k:k + 1], in_=eq.t[:], axis=AX.X, op=ALU.add),
                         reads=[eq], writes=[SLf])
                P.op("dve", lambda e, j=j: e.tensor_copy(SLi.t[:, j, :], SLf.t[:]), reads=[SLf], writes=[SLi])
                P.op("dve", lambda e: e.tensor_scalar(out=ntop.t[:], in0=top8.t[:, 0:1], scalar1=-1.0, scalar2=0.0,
                                                      op0=ALU.mult, op1=ALU.add), reads=[top8], writes=[ntop])
                P.op("act", lambda e: e.activation(out=wex.t[:], in_=top8.t[:, 0:4], func=AF.Exp, bias=ntop.t[:, 0:1], scale=1.0),
                     reads=[top8, ntop], writes=[wex])
                P.op("dve", lambda e: e.tensor_reduce(out=wsum.t[:], in_=wex.t[:], axis=AX.X, op=ALU.add), reads=[wex], writes=[wsum])
                P.op("dve", lambda e: e.reciprocal(out=wsum.t[:], in_=wsum.t[:]), reads=[wsum], writes=[wsum])
                P.op("dve", lambda e, j=j: e.tensor_scalar(out=WT.t[:, j, :], in0=wex.t[:], scalar1=wsum.t[:, 0:1], scalar2=0.0,
                                                           op0=ALU.mult, op1=ALU.add), reads=[wex, wsum], writes=[WT])
                P.op("act", lambda e: e.activation(out=comb.t[:], in_=lg.t[:], func=AF.Exp, bias=ntop.t[:, 0:1], scale=1.0),
                     reads=[lg, ntop], writes=[comb])
                P.op("dve", lambda e: e.tensor_tensor(out=comb.t[:], in0=comb.t[:], in1=mask.t[:], op=ALU.mult),
                     reads=[comb, mask], writes=[comb])
                P.op("dve", lambda e: e.tensor_scalar(out=combb.t[:], in0=comb.t[:], scalar1=wsum.t[:, 0:1], scalar2=0.0,
                                                      op0=ALU.mult, op1=ALU.add), reads=[comb, wsum], writes=[combb])
                tp = TP[nxt("tp", 2)]
                P.op("pe", lambda e, tp=tp: e.transpose(tp.t[0:NE, 0:128], combb.t[:, :], ident), reads=[combb, cmat], writes=[tp])
                P.op("act", lambda e, tp=tp, j=j: e.activation(out=combT.t[0:NE, j, :], in_=tp.t[0:NE, 0:128], func=AF.Copy),
                     reads=[tp], writes=[combT])
                for k in range(4):
                    for hh in range(2):
                        P.op("pool", lambda e, j=j, k=k, xs_=xs_, hh=hh: e.indirect_dma_start(
                            out=XEh[hh][:, :], out_offset=bass.IndirectOffsetOnAxis(ap=SLi.t[:, j, k:k + 1], axis=0),
                            in_=xs_.t[:, hh * (D // 2):(hh + 1) * (D // 2)], in_offset=None, bounds_check=P.bc_reg, oob_is_err=False),
                            reads=[xs_, SLi], writes=[], dsem=XE.dsw)
                        XE.b.last_w = (XE.dsw, None)
            P.op("dve", lambda e: e.tensor_copy(Rb.t[:], Rf.t[:]), reads=[Rf], writes=[Rb])
            P.op("pe", lambda e: e.matmul(RT.t[:, 0:NE], lhsT=cmat.t[:, 2, :], rhs=Rb.t[:], start=True, stop=True),
                 reads=[cmat, Rb], writes=[RT])
            for cp in range(1, 4):
                P.op("dve", lambda e, cp=cp: e.tensor_scalar(out=comb.t[0:1, :], in0=RT.t[0:1, 0:NE], scalar1=float(cp * CH) + 0.5, scalar2=0.0,
                                                             op0=ALU.is_gt, op1=ALU.add), reads=[RT], writes=[comb])
                P.op("dve", lambda e, cp=cp: e.tensor_copy(FLi.t[0:1, (cp - 1) * NE:cp * NE], comb.t[0:1, :]), reads=[comb], writes=[FLi])

            P.barrier()
            P.emit()
            scB1.close()
            P.scope = scB
            if STOP == "B1":
                return
            NWB = 6
            wring = [P.sb(f"wrb{i}", [128, KC * 128], BF16) for i in range(NWB)]
            xet = [P.sb(f"xet{i}", [128, D], BF16) for i in range(2)]
            XeT = [P.sb(f"XeT{i}", [128, KC, CH], BF16) for i in range(2)]
            actT = [P.sb(f"actT{i}", [128, FK, CH], BF16) for i in range(2)]
            gs = [P.sb(f"gs{i}", [128, CH], F32) for i in range(2)]
            sg = [P.sb(f"sg{i}", [128, CH], F32) for i in range(2)]
            us = [P.sb(f"us{i}", [128, CH], F32) for i in range(2)]
            ga = [P.sb(f"ga{i}", [128, CH], F32) for i in range(2)]
            ysb = [P.sb(f"ysb{i}", [128, 512], F32) for i in range(4)]
            assert KC * 128 == FK * 512, "weight ring slot size assumption (D == 4*FF)"
            rr.update({"xe": 0, "xt": 0, "at": 0})

            def load_piece(view_fn, src_ap):
                i = nxt("w", NWB)
                sl = wring[i]
                v = view_fn(sl.t)
                dma("pool", sl, v, None, src_ap)
                return sl, v

            def load_xe(e_, c_, xT):
                r0 = e_ * T + c_ * CH
                for s_i in range(NS):
                    xb = xet[nxt("xe", 2)]
                    for hh in range(2):
                        P.op("sp", lambda e, xb=xb, s_i=s_i, hh=hh, r0=r0: e.dma_start(
                            out=xb.t[:, hh * (D // 2):(hh + 1) * (D // 2)], in_=XEh[hh][r0 + s_i * 128:r0 + (s_i + 1) * 128, :]),
                            reads=[XE], writes=([xb] if hh == 0 else []), dsem=xb.ds)
                        xb.b.last_w = (xb.ds, None)
                    for c0 in range(0, KC, 4):
                        tp = TP[nxt("tp", 2)]

                        def f(e, c0=c0, tp=tp, xb=xb):
                            ins = None
                            for i in range(4):
                                ins = e.transpose(tp.t[:, i * 128:(i + 1) * 128], xb.t[:, (c0 + i) * 128:(c0 + i + 1) * 128], ident)
                            return ins
                        P.op("pe", f, reads=[xb, cmat], writes=[tp])
                        for i in range(4):
                            cc = c0 + i
                            if evac_engine() == "act":
                                P.op("act", lambda e, tp=tp, i=i, cc=cc, s_i=s_i, xT=xT: e.activation(
                                    out=xT.t[:, cc, s_i * 128:(s_i + 1) * 128], in_=tp.t[:, i * 128:(i + 1) * 128], func=AF.Identity,
                                    bias=S2.t[:, cc:cc + 1], scale=A2.t[:, cc:cc + 1]), reads=[tp, A2, S2], writes=[xT])
                            else:
                                P.op("dve", lambda e, tp=tp, i=i, cc=cc, s_i=s_i, xT=xT: e.tensor_scalar(
                                    out=xT.t[:, cc, s_i * 128:(s_i + 1) * 128], in0=tp.t[:, i * 128:(i + 1) * 128],
                                    scalar1=A2.t[:, cc:cc + 1], scalar2=S2.t[:, cc:cc + 1], op0=ALU.mult, op1=ALU.add),
                                    reads=[tp, A2, S2], writes=[xT])

            def expert_pass(e_, c_, xT, prefetch=None):
                aT = actT[nxt("at", 2)]
                r0 = e_ * T + c_ * CH
                for m in range(FK):
                    slg, vg = load_piece(lambda t: t[:, 0:KC * 128].rearrange("p (k n) -> p k n", n=128),
                                         L["w_gate"][e_, :, m * 128:(m + 1) * 128].rearrange("(k p) n -> p k n", p=128))
                    slu, vu = load_piece(lambda t: t[:, 0:KC * 128].rearrange("p (k n) -> p k n", n=128),
                                         L["w_up"][e_, :, m * 128:(m + 1) * 128].rearrange("(k p) n -> p k n", p=128))
                    bj = e_ * FK + m
                    pi = nxt("gu", 2)
                    G = GU[2 * pi]
                    U = GU[2 * pi + 1]
                    for (acc, sl, v) in ((G, slg, vg), (U, slu, vu)):
                        def f(e, acc=acc, v=v, xT=xT):
                            ins = None
                            for k in range(KC):
                                ins = e.matmul(acc.t[:, 0:CH], lhsT=v[:, k, :], rhs=xT.t[:, k, :], start=(k == 0), stop=(k == KC - 1))
                            return ins
                        P.op("pe", f, reads=[sl, xT], writes=[acc])
                    g_, s_, u_, a_ = gs[pi], sg[pi], us[pi], ga[pi]
                    P.op("dve", lambda e, G=G, bj=bj, g_=g_: e.tensor_scalar(out=g_.t[:], in0=G.t[:, 0:CH], scalar1=bgT.t[:, bj:bj + 1], scalar2=7.0,
                                                                         op0=ALU.add, op1=ALU.min), reads=[G, bgT], writes=[g_])
                    P.op("act", lambda e, g_=g_, s_=s_: e.activation(out=s_.t[:], in_=g_.t[:], func=AF.Sigmoid, scale=1.702), reads=[g_], writes=[s_])
                    P.op("dve", lambda e, U=U, bj=bj, u_=u_: e.tensor_scalar(out=u_.t[:], in0=U.t[:, 0:CH], scalar1=buT.t[:, bj:bj + 1], scalar2=7.0,
                                                                         op0=ALU.add, op1=ALU.min), reads=[U, buT], writes=[u_])
                    P.op("dve", lambda e, u_=u_: e.tensor_scalar(out=u_.t[:], in0=u_.t[:], scalar1=-7.0, scalar2=1.0, op0=ALU.max, op1=ALU.add),
                         reads=[u_], writes=[u_])
                    P.op("dve", lambda e, g_=g_, s_=s_, a_=a_: e.tensor_tensor(out=a_.t[:], in0=g_.t[:], in1=s_.t[:], op=ALU.mult), reads=[g_, s_], writes=[a_])
                    P.op("dve", lambda e, aT=aT, m=m, a_=a_, u_=u_: e.tensor_tensor(out=aT.t[:, m, :], in0=a_.t[:], in1=u_.t[:], op=ALU.mult),
                         reads=[a_, u_], writes=[aT])
                if prefetch is not None:
                    prefetch()
                for n in range(D // 512):
                    sld, vd = load_piece(lambda t: t[:, 0:FK * 512].rearrange("p (k n) -> p k n", n=512),
                                         L["w_down"][e_, :, n * 512:(n + 1) * 512].rearrange("(k p) n -> p k n", p=128))
                    for s_i in range(NS):
                        dn = DN[nxt("dn", 2)]

                        def f(e, dn=dn, s_i=s_i, vd=vd, aT=aT):
                            ins = None
                            for k in range(FK):
                                ins = e.matmul(dn.t[:], lhsT=aT.t[:, k, s_i * 128:(s_i + 1) * 128], rhs=vd[:, k, :], start=(k == 0), stop=(k == FK - 1))
                            return ins
                        P.op("pe", f, reads=[aT, sld], writes=[dn])
                        yb = ysb[nxt("ys", 4)]
                        if evac_engine() == "act":
                            P.op("act", lambda e, dn=dn, yb=yb: e.activation(out=yb.t[:], in_=dn.t[:], func=AF.Copy), reads=[dn], writes=[yb])
                        else:
                            P.op("dve", lambda e, dn=dn, yb=yb: e.tensor_copy(yb.t[:], dn.t[:]), reads=[dn], writes=[yb])
                        hc = (n * 512) // YW
                        c0_ = n * 512 - hc * YW
                        dma("sp", YE, YEq[hc][r0 + s_i * 128:r0 + (s_i + 1) * 128, c0_:c0_ + 512], yb, yb.t[:], sem_tl=yb, track_w=False)

            load_xe(0, 0, XeT[0])
            for e_ in range(NE):
                cur = XeT[e_ % 2]
                nxt_ = XeT[(e_ + 1) % 2]
                pf = (lambda e_=e_, nxt_=nxt_: load_xe(e_ + 1, 0, nxt_)) if e_ + 1 < NE else None
                expert_pass(e_, 0, cur, prefetch=pf)
                for c_ in range(1, 4):
                    fi = (c_ - 1) * NE + e_
                    P.cond_begin(FLi, FLi.t[0:1, fi:fi + 1])
                    load_xe(e_, c_, cur)
                    expert_pass(e_, c_, cur)
                    P.cond_end()
            P.barrier()
            P.emit()
        if STOP == "B2":
            return

        with ExitStack() as scC:
            P.scope = scC
            BT = [P.ps(f"BT{i}", [128, 512], F32) for i in range(2)]
            NY = 6
            Yk = [P.sb(f"Yk{i}", [128, D], F32) for i in range(NY)]
            x1b = [P.sb(f"x1b{i}", [128, D], F32) for i in range(2)]
            ob = [P.sb(f"ob{i}", [128, D], F32) for i in range(2)]
            G2B = P.sb("G2B", [128, D], F32)
            bdn = P.sb("bdn", [NE, D], BF16)
            dma("sp", G2B, G2B.t[:], G2, G2d)
            dma("pool", bdn, bdn.t[:], None, L["b_down"])
            for i in range(NY):
                P.op("dve", lambda e, i=i: e.memset(Yk[i].t[:], 0.0), writes=[Yk[i]])
            yi = 0
            for j in range(NT):
                ys = []
                for k in range(4):
                    yt = Yk[yi % NY]
                    yi += 1
                    for hc in range(NYC):
                        P.op("pool", lambda e, yt=yt, j=j, k=k, hc=hc: e.indirect_dma_start(
                            out=yt.t[:, hc * YW:(hc + 1) * YW], out_offset=None, in_=YEq[hc][:, :],
                            in_offset=bass.IndirectOffsetOnAxis(ap=SLi.t[:, j, k:k + 1], axis=0),
                            bounds_check=P.bc_reg, oob_is_err=False), reads=[YE, SLi], writes=([yt] if hc == 0 else []), dsem=yt.dsw)
                        yt.b.last_w = (yt.dsw, None)
                    ys.append(yt)
                xb = x1b[j % 2]
                o = ob[j % 2]
                dma("sp", xb, xb.t[:], X1, X1d[j * 128:(j + 1) * 128, :])
                for n in range(D // 512):
                    bt = BT[n % 2]
                    cs = slice(n * 512, (n + 1) * 512)
                    P.op("pe", lambda e, bt=bt, j=j, cs=cs: e.matmul(bt.t[:], lhsT=combT.t[0:NE, j, :], rhs=bdn.t[0:NE, cs], start=True, stop=True),
                         reads=[combT, bdn], writes=[bt])
                    eng = "dve"
                    P.op("dve", lambda e, bt=bt, j=j, cs=cs, o=o, y0=ys[0]: e.scalar_tensor_tensor(
                        out=o.t[:, cs], in0=y0.t[:, cs], scalar=WT.t[:, j, 0:1], in1=bt.t[:], op0=ALU.mult, op1=ALU.add),
                        reads=[ys[0], WT, bt], writes=[o])
                    for k in range(1, 4):
                        P.op("dve", lambda e, j=j, cs=cs, o=o, yk=ys[k], k=k: e.scalar_tensor_tensor(
                            out=o.t[:, cs], in0=yk.t[:, cs], scalar=WT.t[:, j, k:k + 1], in1=o.t[:, cs], op0=ALU.mult, op1=ALU.add),
                            reads=[ys[k], WT, o], writes=[o])
                    P.op(eng, lambda e, cs=cs, o=o: e.tensor_tensor(out=o.t[:, cs], in0=o.t[:, cs], in1=G2B.t[:, cs], op=ALU.mult),
                         reads=[o, G2B], writes=[o])
                    P.op(eng, lambda e, cs=cs, o=o, xb=xb: e.tensor_tensor(out=o.t[:, cs], in0=o.t[:, cs], in1=xb.t[:, cs], op=ALU.add),
                         reads=[o, xb], writes=[o])
                dma("sp", YO, y_out[j * 128:(j + 1) * 128, :], o, o.t[:], sem_tl=o)
            P.barrier()
            P.emit()


def _feat_major(v, nch):
    return np.ascontiguousarray(np.asarray(v, np.float32).reshape(nch, 128).T)


def _rope_tables(cfg, pos):
    half = 32
    inv_freq = (10000.0 ** (-np.arange(0, half, 2, dtype=np.float32) / np.float32(half))).astype(np.float32)
    row = (pos // cfg.GRID_W).astype(np.float32)
    col = (pos % cfg.GRID_W).astype(np.float32)
    out_c = np.zeros((len(pos), 64), np.float32)
    out_s = np.zeros((len(pos), 64), np.float32)
    for hi, base in enumerate((row, col)):
        ang = base[:, None] * inv_freq[None, :]
        cs, sn = np.cos(ang).astype(np.float32), np.sin(ang).astype(np.float32)
        o = hi * 32
        out_c[:, o:o + 16] = cs
        out_c[:, o + 16:o + 32] = cs
        out_s[:, o:o + 16] = -sn
        out_s[:, o + 16:o + 32] = sn
    return out_c, out_s


def _pool_mats(cfg, g0, n):
    out = np.zeros((cfg.NPG, 3, 128, 128), np.float32)
    for gi, w in enumerate((2, 4, 8, 16)):
        for t in range(128):
            gt = g0 + t
            lo = min(max(gt - w // 2, 0), n)
            hi = min(max(gt + w // 2, 0), n)
            cnt = hi - lo
            for gs in range(lo, hi):
                s = gs - g0
                kd = 0 if s < 0 else (1 if s < 128 else 2)
                out[gi, kd, s - (kd - 1) * 128, t] += 1.0 / cnt
            out[gi, 1, t, t] -= 1.0
    return out


def make_in_maps(cfg, inputs):
    c = cfg
    f = lambda k: np.asarray(inputs[k], np.float32)
    x, cc, ctx, c_ctx = f("x"), f("c"), f("ctx"), f("c_ctx")
    swap = np.concatenate([np.arange(16, 32), np.arange(0, 16), np.arange(48, 64), np.arange(32, 48)])
    gq, gk = f("q_norm_g")[0], f("k_norm_g")[0]
    gvec = np.concatenate([gq, gq[swap], gk, gk[swap]]).reshape(256).astype(np.float32)
    FK = c.FF // 128
    common = {
        "w_ada": f("w_ada")[0], "b_adaT": _feat_major(f("b_ada")[0], c.NMODC),
        "n1T": _feat_major(f("norm1_g")[0], c.KC), "n2T": _feat_major(f("norm2_g")[0], c.KC),
        "w_in": f("w_in")[0], "gvec": gvec, "sinks": np.ascontiguousarray(f("sinks")[0].reshape(-1)),
        "w_pool": f("w_pool")[0], "pscT": _feat_major(f("pool_scale")[0], c.PW // 128),
        "w_out": f("w_out")[0], "w_router": f("w_router")[0], "b_router": np.ascontiguousarray(f("b_router")[0].reshape(-1)),
        "w_gate": f("w_gate")[0], "w_up": f("w_up")[0], "w_down": f("w_down")[0],
        "bgT": np.ascontiguousarray(f("b_gate")[0].reshape(c.NE, FK, 128).transpose(2, 0, 1).reshape(128, c.NE * FK)),
        "buT": np.ascontiguousarray(f("b_up")[0].reshape(c.NE, FK, 128).transpose(2, 0, 1).reshape(128, c.NE * FK)),
        "b_down": f("b_down")[0],
        "ecap": (np.arange(c.NE, dtype=np.float32) * c.T),
    }
    cm = np.zeros((128, 3, 128), np.float32)
    cm[:, 0, :] = np.eye(128, dtype=np.float32)
    cm[:, 1, :] = np.triu(np.ones((128, 128), np.float32), 1)
    cm[:, 2, :] = 1.0
    common["cmat"] = cm.reshape(128, 384)
    kk = np.arange(128)[:, None]
    qq = np.arange(128)[None, :]
    m_prev = (qq <= kk).astype(np.float32)
    m_next = (kk <= qq).astype(np.float32)
    in_maps = []
    for core in range(NCORES):
        b = core // c.CPB
        t0 = (core % c.CPB) * c.T
        first = (core % c.CPB) == 0
        last = (core % c.CPB) == c.CPB - 1
        xe = np.zeros(((c.NT + 2) * 128, c.D), np.float32)
        lo, hi = t0 - 128, t0 + c.T + 128
        slo, shi = max(lo, 0), min(hi, c.SEQ)
        xe[slo - lo:shi - lo] = x[b, slo:shi]
        pos = np.arange(lo, hi)
        rc, rs = _rope_tables(c, np.clip(pos, 0, c.SEQ - 1))
        rc = np.concatenate([rc, np.ones((c.CTX, 64), np.float32)])
        rs = np.concatenate([rs, np.zeros((c.CTX, 64), np.float32)])
        mk = np.zeros((128, 4, 128), np.float32)
        mk[:, 0] = 0.0 if first else m_prev
        mk[:, 1] = m_prev
        mk[:, 2] = m_next
        mk[:, 3] = 0.0 if last else m_next
        pb = np.zeros((c.NPG, 3, 3, 128, 128), np.float32)
        pb[:, 0] = _pool_mats(c, t0, c.SEQ)
        pb[:, 1] = _pool_mats(c, t0 + 128, c.SEQ)
        pb[:, 2] = _pool_mats(c, t0 + c.T - 128, c.SEQ)
        pbl = np.ascontiguousarray(pb.reshape(c.NPG * 9, 128, 128).transpose(1, 0, 2)).reshape(128, c.NPG * 9 * 128)
        cT = np.stack([_feat_major(cc[b], c.KC), _feat_major(c_ctx, c.KC)], axis=2).reshape(128, c.KC * 2)
        m = dict(common)
        m.update({"x_ext": xe, "ctxb": np.ascontiguousarray(ctx[b]), "cT": np.ascontiguousarray(cT),
                  "rope_c": rc, "rope_s": rs, "masks": mk.reshape(128, 512), "poolB": pbl})
        in_maps.append(m)
    return in_maps


_CACHE = {}


def kernel(**inputs):
    cfg = Cfg()
    if "nc" not in _CACHE:
        _CACHE["nc"] = build_program(cfg)
    nc = _CACHE["nc"]
    in_maps = make_in_maps(cfg, inputs)
    res = run_bass_kernel_spmd(nc, in_maps, core_ids=list(range(NCORES)))
    out = np.concatenate([np.asarray(r["y"]) for r in res.results], axis=0)
    return out.reshape(cfg.BATCH, cfg.SEQ, cfg.D).astype(np.float32, copy=False)
```

```python
import numpy as np
from contextlib import ExitStack
import concourse.bass as bass
import concourse.mybir as mybir
from concourse.bass_utils import run_bass_kernel_spmd

F32 = mybir.dt.float32
BF16 = mybir.dt.bfloat16
I32 = mybir.dt.int32
ALU = mybir.AluOpType
AF = mybir.ActivationFunctionType
AX = mybir.AxisListType

NCORES = 8
NORM_EPS = 1e-6
BIGSLOT = 1.0e6


class Cfg:
    def __init__(self, D=4096, BATCH=2, SEQ=8192, CTX=256, FF=1024, NE=32, CAP=1024, GRID_W=64):
        self.D = D
        self.BATCH = BATCH
        self.SEQ = SEQ
        self.CTX = CTX
        self.FF = FF
        self.NE = NE
        self.CAP = CAP
        self.GRID_W = GRID_W
        self.HD = 64
        self.AW = D // 2
        self.NQ = self.AW // 64
        self.NKV = max(1, self.NQ // 8)
        self.GQ = self.NQ // self.NKV
        self.KVW = self.NKV * 64
        self.PW = D - self.AW
        self.NPG = 4
        self.PGW = self.PW // 4
        self.INW = self.AW + 2 * self.KVW + self.PW
        self.T = BATCH * SEQ // NCORES
        self.NT = self.T // 128
        self.KC = D // 128
        self.CPB = NCORES // BATCH
        self.NMODC = 6 * D // 128
        self.WCH = 256
        self.PIECE = max(self.KC * 256, (FF // 128) * 512)
        assert self.NT % 2 == 0 and self.NT >= 4
        assert CAP % 128 == 0


class Sem:
    def __init__(self, h):
        self.h = h
        self.count = 0
        self.sw = False


class Buf:
    __slots__ = ("name", "last_w", "readers")

    def __init__(self, name):
        self.name = name
        self.last_w = None
        self.readers = []


class Tl:
    def __init__(self, P, t, name, is_dram=False):
        self.t = t
        self.b = Buf(name)
        self.P = P
        self._ds = None
        self._dsw = None
        self.name = name
        self.is_dram = is_dram

    @property
    def ds(self):
        if self._ds is None:
            self._ds = self.P.sem("d_" + self.name)
        return self._ds

    @property
    def dsw(self):
        if self._dsw is None:
            self._dsw = self.P.sem("w_" + self.name)
            self._dsw.sw = True
        return self._dsw


class Prog:
    ENGS = ("pe", "act", "dve", "pool", "sp")

    def __init__(self, nc, stack):
        self.nc = nc
        self.stack = stack
        self.scope = stack
        self.q = {k: [] for k in self.ENGS}
        self.all_sems = []
        self.esem = {k: self.sem("e_" + k) for k in ("pe", "act", "dve", "pool")}
        self.seen = {k: {} for k in self.ENGS}
        self.n = 0
        self.bc_val = None
        self.bc_reg = None
        self.nblk = 0
        self.cond = None
        self.dummy = None

    def sem(self, name):
        s = Sem(self.stack.enter_context(self.nc.semaphore(name)))
        self.all_sems.append(s)
        return s

    def sb(self, name, shape, dtype):
        return Tl(self, self.scope.enter_context(self.nc.sbuf_tensor("s_" + name, list(shape), dtype)), name)

    def ps(self, name, shape, dtype):
        return Tl(self, self.scope.enter_context(self.nc.psum_tensor("p_" + name, list(shape), dtype)), name)

    def dram(self, t, name):
        return Tl(self, t, name, is_dram=True)

    def op(self, eng, fn, reads=(), writes=(), dsem=None):
        deps = []
        for tl in reads:
            b = tl.b
            if b.last_w is not None:
                deps.append(b.last_w)
        for tl in writes:
            b = tl.b
            if b.last_w is not None:
                deps.append(b.last_w)
            deps.extend(b.readers)
        waits = {}
        mysem = self.esem.get(eng)
        seen = self.seen[eng]
        for (s, v) in deps:
            if v is None:
                v = s.count
            if s is mysem and eng == "pe":
                continue
            if v <= seen.get(id(s), 0):
                continue
            if id(s) in waits and waits[id(s)][1] >= v:
                continue
            waits[id(s)] = (s, v)
        for s, v in waits.values():
            seen[id(s)] = v
        if dsem is not None:
            dsem.count += 16
            tok = (dsem, None)
            inc = (dsem, 16)
        else:
            s = self.esem[eng]
            s.count += 1
            tok = (s, s.count)
            inc = (s, 1)
        self.q[eng].append((list(waits.values()), fn, inc))
        if self.cond is not None:
            d = self.cond["comp"][eng]
            prev = d.get(id(inc[0]), (inc[0], 0))[1]
            d[id(inc[0])] = (inc[0], prev + inc[1])
        self.n += 1
        for tl in reads:
            tl.b.readers.append(tok)
        for tl in writes:
            tl.b.last_w = tok
            tl.b.readers = []

    def wait_only(self, eng, reads):
        waits = {}
        seen = self.seen[eng]
        for tl in reads:
            lw = tl.b.last_w
            if lw is None:
                continue
            s, v = lw
            if v is None:
                v = s.count
            if v <= seen.get(id(s), 0):
                continue
            waits[id(s)] = (s, v)
            seen[id(s)] = v
        self.q[eng].append((list(waits.values()), None, None))

    def cond_begin(self, flag_tl, flag_ap):
        assert self.cond is None
        for k in self.ENGS:
            self.wait_only(k, [flag_tl])
        self.cond = dict(flag_ap=flag_ap, outer_q=self.q, seen={k: dict(v) for k, v in self.seen.items()},
                         start={id(s): s.count for s in self.all_sems}, comp={k: {} for k in self.ENGS})
        self.q = {k: [] for k in self.ENGS}

    def cond_end(self):
        c = self.cond
        sub = self.q
        self.q = c["outer_q"]
        for k in self.ENGS:
            if sub[k]:
                comp = [(sm, c["start"].get(id(sm), 0), d) for (sm, d) in c["comp"][k].values()]
                self.q[k].append(("cond", c["flag_ap"], sub[k], comp))
        self.seen = c["seen"]
        self.cond = None

    def barrier(self):
        for k in self.ENGS:
            waits = []
            for s in self.all_sems:
                if s.count > self.seen[k].get(id(s), 0):
                    waits.append((s, s.count))
                    self.seen[k][id(s)] = s.count
            self.q[k].append((waits, None, None))

    def emit(self):
        nc = self.nc
        q = self.q
        if not any(q.values()):
            return

        self.nblk += 1
        nb = self.nblk

        def run_list(items, e, reg):
            for it in items:
                if it[0] == "cond":
                    _, flag_ap, sub, comp = it
                    e.reg_load(reg, flag_ap)
                    with e.If_ne(reg, 0):
                        run_list(sub, e, reg)
                    with e.Else():
                        prev = None
                        for sm, start, d in comp:
                            e.wait_ge(sm.h, start)
                            if sm.sw:
                                if prev is not None:
                                    e.wait_ge(prev[0].h, prev[1])
                                e.dma_start(out=self.dummy[0], in_=self.dummy[1]).then_inc(sm.h, d)
                                prev = (sm, start + d)
                            else:
                                e.sem_inc(sm.h, d)
                        if prev is not None:
                            e.wait_ge(prev[0].h, prev[1])
                    continue
                waits, fn, inc = it
                for sm, v in waits:
                    e.wait_ge(sm.h, v)
                if fn is None:
                    continue
                ins = fn(e)
                ins.then_inc(inc[0].h, inc[1])

        def run(k, e):
            with e.register(f"fl_{k}_{nb}") as reg:
                run_list(q[k], e, reg)

        with nc.Block() as block:
            @block.tensor
            def _(e):
                run("pe", e)

            @block.scalar
            def _(e):
                run("act", e)

            @block.vector
            def _(e):
                run("dve", e)

            @block.gpsimd
            def _(e):
                if self.bc_val is not None:
                    with e.register(f"bc{nb}") as bc:
                        e.reg_mov(bc, self.bc_val)
                        self.bc_reg = bc
                        run("pool", e)
                else:
                    run("pool", e)

            @block.sync
            def _(e):
                run("sp", e)
        self.q = {k: [] for k in self.ENGS}


STOP = None


def build_program(cfg, debug=False):
    c = cfg
    D, T, NT, KC, NE, FF, CAP = c.D, c.T, c.NT, c.KC, c.NE, c.FF, c.CAP
    NKV, AW, KVW, PW, PGW, NPG, INW = c.NKV, c.AW, c.KVW, c.PW, c.PGW, c.NPG, c.INW
    WCH = c.WCH
    NXT = NT + 2
    nc = bass.Bass("TRN2", target_bir_lowering=False)

    def din(name, shape, dt=F32):
        return nc.dram_tensor(name, list(shape), dt, kind="ExternalInput").ap()

    x_ext = din("x_ext", [NXT * 128, D])
    ctxb = din("ctxb", [c.CTX, D])
    cT_d = din("cT", [128, KC * 2])
    w_ada = din("w_ada", [D, 6 * D])
    b_adaT = din("b_adaT", [128, c.NMODC])
    n1T_d = din("n1T", [128, KC])
    n2T_d = din("n2T", [128, KC])
    w_in = din("w_in", [D, INW])
    gvec_d = din("gvec", [4 * 64])
    sinks_d = din("sinks", [c.NQ])
    w_pool = din("w_pool", [NPG, PGW, PGW])
    pscT_d = din("pscT", [128, PW // 128])
    w_out = din("w_out", [D, D])
    w_router = din("w_router", [D, NE])
    b_router = din("b_router", [NE])
    w_gate = din("w_gate", [NE, D, FF])
    w_up = din("w_up", [NE, D, FF])
    w_down = din("w_down", [NE, FF, D])
    bgT_d = din("bgT", [128, NE * (FF // 128)])
    buT_d = din("buT", [128, NE * (FF // 128)])
    b_down = din("b_down", [NE, D])
    rope_c = din("rope_c", [NXT * 128 + c.CTX, 64])
    rope_s = din("rope_s", [NXT * 128 + c.CTX, 64])
    masks_d = din("masks", [128, 4 * 128])
    poolB_d = din("poolB", [128, NPG * 9 * 128])
    cmat_d = din("cmat", [128, 3 * 128])
    ecap_d = din("ecap", [NE])
    y_out = nc.dram_tensor("y", [T, D], F32, kind="ExternalOutput").ap()
    x1kind = dict(kind="ExternalOutput") if debug else {}
    X1d = nc.dram_tensor("x1s", [T, D], F32, **x1kind).ap()
    HROWS = NE * T
    YW = max(512, D // 4)
    NYC = D // YW
    XEh = [nc.dram_tensor(f"xe_s{i}", [HROWS, D // 2], BF16).ap() for i in range(2)]
    YEq = [nc.dram_tensor(f"ye_s{i}", [HROWS, YW], F32).ap() for i in range(NYC)]
    G1d = nc.dram_tensor("g1b_s", [128, D], F32).ap()
    G2d = nc.dram_tensor("g2b_s", [128, D], F32).ap()
    if debug:
        LGd = nc.dram_tensor("lg_s", [T, NE], F32, kind="ExternalOutput").ap()

    top = ExitStack()
    with top:
        P = Prog(nc, top)
        X1 = P.dram(X1d, "X1")
        XE = P.dram(None, "XE")
        YE = P.dram(None, "YE")
        G1 = P.dram(G1d, "G1")
        G2 = P.dram(G2d, "G2")
        YO = P.dram(y_out, "YO")
        LGo = P.dram(None, "LGo")

        def ckpt(name):
            if STOP == name:
                P.barrier()
                P.emit()
                return True
            return False

        def dma(q, out_tl, out_ap, in_tl, in_ap, sem_tl=None, track_w=True, **kw):
            if sem_tl is None:
                sem_tl = out_tl if (out_tl is not None and not out_tl.is_dram) else in_tl
            reads = [in_tl] if in_tl is not None else []
            writes = [out_tl] if (out_tl is not None and track_w) else []
            sem = sem_tl.dsw if q == "pool" else sem_tl.ds
            P.op(q, lambda e: e.dma_start(out=out_ap, in_=in_ap, **kw), reads=reads, writes=writes,
                 dsem=sem)
            if out_tl is not None and not track_w:
                out_tl.b.last_w = (sem, None)

        A2 = P.sb("A2", [128, KC], F32)
        S2 = P.sb("S2", [128, KC], F32)
        with ExitStack() as scA:
            P.scope = scA
            NG = NT // 2
            cmat = P.sb("cmat", [128, 3, 128], BF16)
            identf = P.sb("identf", [128, 128], F32)
            onesf = P.sb("onesf", [128, 128], F32)
            masks = P.sb("masks", [128, 4, 128], BF16)
            poolB = P.sb("poolB", [128, NPG * 9, 128], BF16)
            gvec = P.sb("gvec", [128, 4, 64], F32)
            esink = P.sb("esink", [128, c.NQ], F32)
            cT = P.sb("cT", [128, KC, 2], F32)
            sig = P.sb("sig", [128, KC, 2], F32)
            scb = P.sb("scb", [128, KC, 2], BF16)
            badaT = P.sb("badaT", [128, c.NMODC], F32)
            MOD = P.sb("MOD", [128, c.NMODC, 2], F32)
            n1T = P.sb("n1T", [128, KC], F32)
            n2T = P.sb("n2T", [128, KC], F32)
            A1 = P.sb("A1", [128, KC], F32)
            A1c = P.sb("A1c", [128, KC], F32)
            pscT = P.sb("pscT", [128, PW // 128], F32)
            cst = P.sb("cst", [128, 1], F32)
            ident = cmat.t[:, 0, :]

            dma("pool", cmat, cmat.t[:], None, cmat_d.rearrange("p (a b) -> p a b", a=3), sem_tl=cst)
            dma("sp", identf, identf.t[:], None, cmat_d[:, 0:128], sem_tl=cst)
            dma("pool", masks, masks.t[:], None, masks_d.rearrange("p (a b) -> p a b", a=4), sem_tl=cst)
            dma("pool", poolB, poolB.t[:], None, poolB_d.rearrange("p (a b) -> p a b", b=128), sem_tl=cst)
            dma("sp", gvec, gvec.t[:], None,
                gvec_d.partition_broadcast(128).rearrange("p (a b) -> p a b", a=4), sem_tl=cst)
            dma("sp", esink, esink.t[:], None, sinks_d.partition_broadcast(128), sem_tl=cst)
            dma("sp", cT, cT.t[:], None, cT_d.rearrange("p (a b) -> p a b", b=2), sem_tl=cst)
            dma("sp", badaT, badaT.t[:], None, b_adaT, sem_tl=cst)
            dma("sp", n1T, n1T.t[:], None, n1T_d, sem_tl=cst)
            dma("sp", n2T, n2T.t[:], None, n2T_d, sem_tl=cst)
            dma("sp", pscT, pscT.t[:], None, pscT_d, sem_tl=cst)
            P.op("dve", lambda e: e.memset(onesf.t[:], 1.0), writes=[onesf])
            P.op("act", lambda e: e.activation(out=esink.t[:], in_=esink.t[:], func=AF.Exp), reads=[esink], writes=[esink])
            P.op("act", lambda e: e.activation(out=sig.t[:], in_=cT.t[:], func=AF.Sigmoid), reads=[cT], writes=[sig])
            P.op("dve", lambda e: e.tensor_tensor(out=scb.t[:], in0=cT.t[:], in1=sig.t[:], op=ALU.mult),
                 reads=[cT, sig], writes=[scb])

            TP = [P.ps(f"TP{i}", [128, 512], BF16) for i in range(2)]
            ACC = [P.ps(f"ACC{i}", [128, 512], F32) for i in range(2)]
            STp = [P.ps(f"ST{i}", [128, 512], F32) for i in range(2)]
            OAC = [P.ps(f"OAC{i}", [128, 4, 128], F32) for i in range(2)]
            rr = {"tp": 0, "acc": 0, "st": 0, "oac": 0, "w": 0, "ev": 0, "z": 0, "q4": 0}

            def nxt(key, n):
                v = rr[key]
                rr[key] = (v + 1) % n
                return v

            NW = 3
            wring = [P.sb(f"wr{i}", [128, KC * WCH], BF16) for i in range(NW)]

            def load_w(src_ap, ncols):
                i = nxt("w", NW)
                sl = wring[i]
                view = sl.t[:, 0:KC * ncols].rearrange("p (k n) -> p k n", n=ncols)
                dma("pool", sl, view, None, src_ap.rearrange("(k p) n -> p k n", p=128))
                return sl, view

            modps = ACC[0]
            npc = WCH // 128
            for pi in range(6 * D // WCH):
                sl, wv = load_w(w_ada[:, pi * WCH:(pi + 1) * WCH], WCH)

                def f(e, wv=wv, pi=pi):
                    ins = None
                    for cc in range(npc):
                        j = pi * npc + cc
                        for k in range(KC):
                            ins = e.matmul(modps.t[:, 2 * j:2 * j + 2], lhsT=wv[:, k, cc * 128:(cc + 1) * 128],
                                           rhs=scb.t[:, k, :], start=(k == 0), stop=(k == KC - 1))
                    return ins
                P.op("pe", f, reads=[sl, scb], writes=[modps])
            P.op("dve", lambda e: e.tensor_tensor(
                out=MOD.t[:], in0=modps.t[:, 0:2 * c.NMODC].rearrange("p (j r) -> p j r", r=2),
                in1=badaT.t[:, :].unsqueeze(2).to_broadcast([128, c.NMODC, 2]), op=ALU.add),
                reads=[modps, badaT], writes=[MOD])

            def modv(idx, r=0):
                return MOD.t[:, idx * KC:(idx + 1) * KC, r]
            P.op("dve", lambda e: e.scalar_tensor_tensor(out=A1.t[:], in0=modv(1, 0), scalar=1.0, in1=n1T.t[:],
                                                         op0=ALU.add, op1=ALU.mult), reads=[MOD, n1T], writes=[A1])
            P.op("dve", lambda e: e.scalar_tensor_tensor(out=A1c.t[:], in0=modv(1, 1), scalar=1.0, in1=n1T.t[:],
                                                         op0=ALU.add, op1=ALU.mult), reads=[MOD, n1T], writes=[A1c])

            diag = [P.sb(f"diag{i}", [128, 128], F32) for i in range(2)]
            gpc = [P.sb(f"gpc{i}", [128, 512], F32) for i in range(2)]
            for gi, (midx, Gd, Gt) in enumerate(((2, G1d, G1), (5, G2d, G2))):
                for n4 in range(D // 512):
                    acc = ACC[1]
                    for q4 in range(4):
                        ch = n4 * 4 + q4
                        dg = diag[ch % 2]
                        P.op("dve", lambda e, dg=dg, ch=ch, midx=midx: e.tensor_scalar(
                            out=dg.t[:], in0=identf.t[:], scalar1=MOD.t[:, midx * KC + ch, 0:1], scalar2=0.0,
                            op0=ALU.mult, op1=ALU.add), reads=[identf, MOD], writes=[dg])
                        P.op("pe", lambda e, dg=dg, q4=q4, acc=acc: e.matmul(
                            acc.t[:, q4 * 128:(q4 + 1) * 128], lhsT=onesf.t[:], rhs=dg.t[:], start=True, stop=True),
                            reads=[onesf, dg], writes=[acc])
                    gp = gpc[n4 % 2]
                    P.op("act", lambda e, gp=gp, acc=acc: e.activation(out=gp.t[:], in_=acc.t[:], func=AF.Copy),
                         reads=[acc], writes=[gp])
                    dma("sp", Gt, Gd[:, n4 * 512:(n4 + 1) * 512], gp, gp.t[:], sem_tl=Gt)

            if ckpt("setup"):
                return nc
            NL = 4
            xt = P.sb("xt", [128, D], F32)
            xs = P.sb("xs", [128, D], BF16)
            ss = P.sb("ss", [128, 2], F32)
            hT = P.sb("hT", [128, NL, KC, 128], BF16)
            mixT = P.sb("mixT", [128, KC, 256], BF16)
            kT = P.sb("kT", [64, NL + 2, NKV, 128], BF16)
            Vg = P.sb("Vg", [128, NL + 2, NKV, 65], BF16)
            rc = P.sb("rc", [128, NL, 64], F32)
            rs = P.sb("rs", [128, NL, 64], F32)
            tabs = P.sb("tabs", [128, 4, NL, 64], F32)
            zs = [P.sb(f"zs{i}", [128, 256], F32) for i in range(2)]
            sq = P.sb("sq", [128, 256], F32)
            t1 = P.sb("t1", [128, 256], F32)
            t2 = P.sb("t2", [128, 256], F32)
            hs = P.sb("hs", [128, 8], F32)
            qrot = [P.sb(f"qrot{i}", [128, 256], BF16) for i in range(2)]
            qT4 = [P.sb(f"qT4{i}", [64, 4, 128], BF16) for i in range(4)]
            Pt = [P.sb(f"Pt{i}", [128, 512], BF16) for i in range(3)]
            den = P.sb("den", [128, 8], F32)
            attn = [P.sb(f"attn{i}", [128, 256], BF16) for i in range(2)]
            Ubuf = P.sb("Ubuf", [128, NL, PGW], BF16)
            dT = P.sb("dT", [128, PGW // 128, 256], BF16)
            wpl = [P.sb(f"wpl{i}", [128, PGW // 128, PGW], BF16) for i in range(2)]
            xp = [P.sb(f"xp{i}", [128, 2, WCH], F32) for i in range(2)]
            g1p = [P.sb(f"g1p{i}", [128, WCH], F32) for i in range(2)]
            ytmp = [P.sb(f"ytmp{i}", [128, WCH], F32) for i in range(2)]
            x1p = [P.sb(f"x1p{i}", [128, 2, WCH], F32) for i in range(2)]
            P.op("dve", lambda e: e.memset(Vg.t[:], 1.0), writes=[Vg])
            P.op("dve", lambda e: e.memset(xs.t[:], 0.0), writes=[xs])
            if debug:
                P.op("dve", lambda e: e.memset(xt.t[:], 0.0), writes=[xt])
                for r in range(HROWS // 128):
                    for hh in range(2):
                        dma("sp", XE, XEh[hh][r * 128:(r + 1) * 128, :], xs, xs.t[:, 0:D // 2], sem_tl=xs, track_w=False)
                    for hc in range(NYC):
                        dma("sp", YE, YEq[hc][r * 128:(r + 1) * 128, :], xt, xt.t[:, 0:YW], sem_tl=xt, track_w=False)

            def evac_engine():
                return ("act", "dve")[nxt("ev", 2)]

            def copy_op(eng, out_ap, in_ap, reads, writes):
                if eng == "act":
                    P.op("act", lambda e: e.activation(out=out_ap, in_=in_ap, func=AF.Copy), reads=reads, writes=writes)
                else:
                    P.op(eng, lambda e: e.tensor_copy(out_ap, in_ap), reads=reads, writes=writes)

            def affine_op(eng, out_ap, in_ap, sc_ap, bi_ap, reads, writes):
                if eng == "act":
                    P.op("act", lambda e: e.activation(out=out_ap, in_=in_ap, func=AF.Identity, bias=bi_ap, scale=sc_ap),
                         reads=reads, writes=writes)
                else:
                    P.op(eng, lambda e: e.tensor_scalar(out=out_ap, in0=in_ap, scalar1=sc_ap, scalar2=bi_ap,
                                                        op0=ALU.mult, op1=ALU.add), reads=reads, writes=writes)

            def norm_tile(src_tl, dst_tl, ss_col):
                P.op("dve", lambda e: e.memset(ss.t[:, ss_col:ss_col + 1], 0.0), writes=[ss])
                P.op("act", lambda e: e.activation(out=dst_tl.t[:], in_=src_tl.t[:], func=AF.Square,
                                                   accum_out=ss.t[:, ss_col:ss_col + 1]),
                     reads=[src_tl], writes=[dst_tl, ss])
                P.op("act", lambda e: e.activation(out=ss.t[:, ss_col:ss_col + 1], in_=ss.t[:, ss_col:ss_col + 1],
                                                   func=AF.Sqrt, bias=cst_eps.t[:, 0:1], scale=1.0 / D),
                     reads=[ss, cst_eps], writes=[ss])
                P.op("dve", lambda e: e.reciprocal(out=ss.t[:, ss_col:ss_col + 1], in_=ss.t[:, ss_col:ss_col + 1]),
                     reads=[ss], writes=[ss])
                P.op("dve", lambda e: e.tensor_scalar(out=dst_tl.t[:], in0=src_tl.t[:], scalar1=ss.t[:, ss_col:ss_col + 1],
                                                      scalar2=0.0, op0=ALU.mult, op1=ALU.add),
                     reads=[src_tl, ss], writes=[dst_tl])

            cst_eps = P.sb("cst_eps", [128, 2], F32)
            P.op("dve", lambda e: e.memset(cst_eps.t[:, 0:1], NORM_EPS), writes=[cst_eps])
            P.op("dve", lambda e: e.memset(cst_eps.t[:, 1:2], NORM_EPS), writes=[cst_eps])

            def transposes_to(src_tl, nchunks, dst_fn, sc_fn, bi_fn, dst_tl, extra_reads=()):
                for c0 in range(0, nchunks, 4):
                    tp = TP[nxt("tp", 2)]
                    nn = min(4, nchunks - c0)

                    def f(e, c0=c0, nn=nn, tp=tp):
                        ins = None
                        for i in range(nn):
                            ins = e.transpose(tp.t[:, i * 128:(i + 1) * 128], src_tl.t[:, (c0 + i) * 128:(c0 + i + 1) * 128], ident)
                        return ins
                    P.op("pe", f, reads=[src_tl, cmat], writes=[tp])
                    for i in range(nn):
                        cc = c0 + i
                        eng = evac_engine()
                        if sc_fn is None:
                            copy_op(eng, dst_fn(cc), tp.t[:, i * 128:(i + 1) * 128], [tp], [dst_tl])
                        else:
                            affine_op(eng, dst_fn(cc), tp.t[:, i * 128:(i + 1) * 128], sc_fn(cc), bi_fn(cc),
                                      [tp, MOD, *extra_reads], [dst_tl])

            def stage1(row0, l, Avec, shift_r):
                src = x_ext if row0 >= 0 else ctxb
                r0 = row0 if row0 >= 0 else (-row0 - 1)
                dma("sp", xt, xt.t[:], None, src[r0:r0 + 128, :])
                norm_tile(xt, xs, 0)
                transposes_to(xs, KC, lambda cc: hT.t[:, l, cc, :],
                              lambda cc: Avec.t[:, cc:cc + 1], lambda cc: MOD.t[:, 0 * KC + cc, shift_r:shift_r + 1],
                              hT, extra_reads=[Avec])

            def project(l, wsl, wv, ncols):
                acc = ACC[nxt("acc", 2)]

                def f(e):
                    ins = None
                    for k in range(KC):
                        ins = e.matmul(acc.t[:, 0:ncols], lhsT=hT.t[:, l, k, :], rhs=wv[:, k, :],
                                       start=(k == 0), stop=(k == KC - 1))
                    return ins
                P.op("pe", f, reads=[hT, wsl], writes=[acc])
                return acc

            def rms_rope(acc, nh, l, tA, tB, out_aps):
                w = nh * 64
                z = zs[nxt("z", 2)]
                P.op("act", lambda e: e.activation(out=z.t[:, 0:w], in_=acc.t[:, 0:w], func=AF.Copy), reads=[acc], writes=[z])
                P.op("act", lambda e: e.activation(out=sq.t[:, 0:w], in_=z.t[:, 0:w], func=AF.Square), reads=[z], writes=[sq])
                P.op("dve", lambda e: e.tensor_reduce(out=hs.t[:, 0:nh], in_=sq.t[:, 0:w].rearrange("p (h d) -> p h d", d=64),
                                                      axis=AX.X, op=ALU.add), reads=[sq], writes=[hs])
                P.op("act", lambda e: e.activation(out=hs.t[:, 0:nh], in_=hs.t[:, 0:nh], func=AF.Sqrt,
                                                   bias=cst_eps.t[:, 1:2], scale=1.0 / 64), reads=[hs, cst_eps], writes=[hs])
                P.op("dve", lambda e: e.reciprocal(out=hs.t[:, 0:nh], in_=hs.t[:, 0:nh]), reads=[hs], writes=[hs])
                z3 = z.t[:, 0:w].rearrange("p (h d) -> p h d", d=64)
                z5 = z.t[:, 0:w].rearrange("p (h a b d) -> p h a b d", a=2, b=2, d=16)
                t13 = t1.t[:, 0:w].rearrange("p (h d) -> p h d", d=64)
                t25 = t2.t[:, 0:w].rearrange("p (h a b d) -> p h a b d", a=2, b=2, d=16)
                A_b = tabs.t[:, tA, l, :].unsqueeze(1).to_broadcast([128, nh, 64])
                B5 = tabs.t[:, tB, l, :].rearrange("p (a b d) -> p a b d", a=2, b=2)
                P.op("dve", lambda e: e.tensor_tensor(out=t13, in0=z3, in1=A_b, op=ALU.mult), reads=[z, tabs], writes=[t1])
                for b in range(2):
                    P.op("dve", lambda e, b=b: e.tensor_tensor(
                        out=t25[:, :, :, b, :], in0=z5[:, :, :, 1 - b, :],
                        in1=B5[:, :, b, :].unsqueeze(1).to_broadcast([128, nh, 2, 16]), op=ALU.mult),
                        reads=[z, tabs], writes=[t2])
                P.op("dve", lambda e: e.tensor_tensor(out=t1.t[:, 0:w], in0=t1.t[:, 0:w], in1=t2.t[:, 0:w], op=ALU.add),
                     reads=[t1, t2], writes=[t1])
                for (otl, oap) in out_aps:
                    P.op("dve", lambda e, oap=oap: e.tensor_tensor(
                        out=oap, in0=t13, in1=hs.t[:, 0:nh].unsqueeze(2).to_broadcast([128, nh, 64]), op=ALU.mult),
                        reads=[t1, hs], writes=[otl])

            def load_tables(row0, nl, l0):
                dma("sp", rc, rc.t[:, 0:nl, :], None, rope_c[row0:row0 + nl * 128, :].rearrange("(l p) d -> p l d", p=128))
                dma("sp", rs, rs.t[:, 0:nl, :], None, rope_s[row0:row0 + nl * 128, :].rearrange("(l p) d -> p l d", p=128))
                for ti, (src, gi) in enumerate(((rc, 0), (rs, 1), (rc, 2), (rs, 3))):
                    P.op("dve", lambda e, ti=ti, src=src, gi=gi: e.tensor_tensor(
                        out=tabs.t[:, ti, l0:l0 + nl, :], in0=src.t[:, 0:nl, :],
                        in1=gvec.t[:, gi, :].unsqueeze(1).to_broadcast([128, nl, 64]), op=ALU.mult),
                        reads=[src, gvec], writes=[tabs])

            def do_k(acc, l, slot):
                kr = qrot[nxt("oac", 2)]
                rms_rope(acc, NKV, l, 2, 3, [(kr, kr.t[:, 0:NKV * 64].rearrange("p (h d) -> p h d", d=64))])
                tp = TP[nxt("tp", 2)]

                def f(e):
                    ins = None
                    for h in range(NKV):
                        ins = e.transpose(tp.t[0:64, h * 128:(h + 1) * 128], kr.t[:, h * 64:(h + 1) * 64], ident)
                    return ins
                P.op("pe", f, reads=[kr, cmat], writes=[tp])
                copy_op(evac_engine(), kT.t[0:64, slot, :, :], tp.t[0:64, 0:NKV * 128].rearrange("p (h t) -> p h t", t=128),
                        [tp], [kT])

            def do_v(acc, slot):
                copy_op(evac_engine(), Vg.t[:, slot, :, 0:64], acc.t[:, 0:NKV * 64].rearrange("p (h d) -> p h d", d=64),
                        [acc], [Vg])

            load_tables(NXT * 128, 2, 0)
            kwid = min(WCH, KVW)
            for ci in range(c.CTX // 128):
                stage1(-(ci * 128) - 1, ci, A1c, 1)
            for kc0 in range(0, KVW, kwid):
                assert kwid == KVW, "k/v chunking assumes KVW <= 256"
            wsl, wv = load_w(w_in[:, AW:AW + KVW], KVW)
            for ci in range(c.CTX // 128):
                acc = project(ci, wsl, wv, KVW)
                do_k(acc, ci, NL + ci)
            wsl, wv = load_w(w_in[:, AW + KVW:AW + 2 * KVW], KVW)
            for ci in range(c.CTX // 128):
                acc = project(ci, wsl, wv, KVW)
                do_v(acc, NL + ci)

            if ckpt("ctx"):
                return nc
            scale = float(c.HD) ** -0.5
            nqc = AW // WCH
            nuc = max(1, PGW // WCH)
            ucw = min(WCH, PGW)
            for g in range(NG):
                own0 = 2 * g
                load_tables((own0) * 128, NL, 0)
                for l in range(NL):
                    stage1((own0 + l) * 128, l, A1, 0)
                wsl, wv = load_w(w_in[:, AW:AW + KVW], KVW)
                for l in range(NL):
                    acc = project(l, wsl, wv, KVW)
                    do_k(acc, l, l)
                wsl, wv = load_w(w_in[:, AW + KVW:AW + 2 * KVW], KVW)
                for l in range(NL):
                    acc = project(l, wsl, wv, KVW)
                    do_v(acc, l)
                if g == 0 and ckpt("g0kv"):
                    return nc
                for pg in range(NPG):
                    wp = wpl[pg % 2]
                    dma("pool", wp, wp.t[:], None, w_pool[pg].rearrange("(k p) n -> p k n", p=128))
                    for uc in range(nuc):
                        col0 = AW + 2 * KVW + pg * PGW + uc * ucw
                        wsl, wv = load_w(w_in[:, col0:col0 + ucw], ucw)
                        for l in range(NL):
                            acc = project(l, wsl, wv, ucw)
                            copy_op(evac_engine(), Ubuf.t[:, l, uc * ucw:(uc + 1) * ucw], acc.t[:, 0:ucw], [acc], [Ubuf])
                    for lo in range(2):
                        l = 1 + lo
                        own = own0 + lo
                        var = 0 if own == 0 else (2 if own == NT - 1 else 1)
                        for cc in range(PGW // 128):
                            acc = ACC[nxt("acc", 2)]

                            def f(e, acc=acc, l=l, cc=cc, var=var, pg=pg):
                                ins = None
                                for kd in range(3):
                                    ins = e.matmul(acc.t[:, 0:128], lhsT=Ubuf.t[:, l - 1 + kd, cc * 128:(cc + 1) * 128],
                                                   rhs=poolB.t[:, pg * 9 + var * 3 + kd, :], start=(kd == 0), stop=(kd == 2))
                                return ins
                            P.op("pe", f, reads=[Ubuf, poolB], writes=[acc])
                            copy_op(evac_engine(), dT.t[:, cc, lo * 128:(lo + 1) * 128], acc.t[:, 0:128], [acc], [dT])
                    for co in range(PGW // 128):
                        acc = ACC[nxt("acc", 2)]

                        def f(e, acc=acc, co=co, wp=wp):
                            ins = None
                            nk = PGW // 128
                            for ci in range(nk):
                                ins = e.matmul(acc.t[:, 0:256], lhsT=wp.t[:, ci, co * 128:(co + 1) * 128], rhs=dT.t[:, ci, :],
                                               start=(ci == 0), stop=(ci == nk - 1))
                            return ins
                        P.op("pe", f, reads=[wp, dT], writes=[acc])
                        pj = pg * (PGW // 128) + co
                        P.op("act", lambda e, acc=acc, pj=pj: e.activation(
                            out=mixT.t[:, AW // 128 + pj, :], in_=acc.t[:, 0:256], func=AF.Identity, scale=pscT.t[:, pj:pj + 1]),
                            reads=[acc, pscT], writes=[mixT])
                if g == 0 and ckpt("g0pool"):
                    return nc
                def q_stage(qc):
                    wsl, wv = load_w(w_in[:, qc * WCH:(qc + 1) * WCH], WCH)
                    res = []
                    for lo in range(2):
                        l = 1 + lo
                        acc = project(l, wsl, wv, WCH)
                        qr = qrot[nxt("oac", 2)]
                        rms_rope(acc, 4, l, 0, 1, [(qr, qr.t[:].rearrange("p (h d) -> p h d", d=64))])
                        tp = TP[nxt("tp", 2)]

                        def f(e, tp=tp, qr=qr):
                            ins = None
                            for h in range(4):
                                ins = e.transpose(tp.t[0:64, h * 128:(h + 1) * 128], qr.t[:, h * 64:(h + 1) * 64], ident)
                            return ins
                        P.op("pe", f, reads=[qr, cmat], writes=[tp])
                        q4 = qT4[nxt("q4", 4)]
                        copy_op(evac_engine(), q4.t[:], tp.t[0:64, :].rearrange("p (h t) -> p h t", t=128), [tp], [q4])
                        res.append((lo, q4))
                    return res

                pend = q_stage(0)
                for qc in range(nqc):
                    nxt_units = q_stage(qc + 1) if qc + 1 < nqc else None
                    kvh = (qc * 4) // c.GQ
                    for (lo, q4) in pend:
                        l = 1 + lo
                        own = own0 + lo
                        oac = OAC[lo]
                        blocks = [(l - 1, 0 if own == 0 else 1), (l, None), (l + 1, 3 if own == NT - 1 else 2),
                                  (NL, None), (NL + 1, None)][:3 + c.CTX // 128]
                        nb = len(blocks)
                        for bi, (slot, mk) in enumerate(blocks):
                            st = STp[bi % 2]
                            P.op("pe", lambda e, st=st, slot=slot, q4=q4, kvh=kvh: e.matmul(
                                st.t[:], lhsT=kT.t[0:64, slot, kvh, :], rhs=q4.t[:].rearrange("p h t -> p (h t)"),
                                start=True, stop=True), reads=[kT, q4], writes=[st])
                            pt = Pt[bi % 3]
                            P.op("act", lambda e, st=st, pt=pt: e.activation(out=pt.t[:], in_=st.t[:], func=AF.Exp, scale=scale),
                                 reads=[st], writes=[pt])
                            if mk is not None:
                                P.op("dve", lambda e, pt=pt, mk=mk: e.tensor_tensor(
                                    out=pt.t[:].rearrange("p (h t) -> p h t", t=128),
                                    in0=pt.t[:].rearrange("p (h t) -> p h t", t=128),
                                    in1=masks.t[:, mk, :].unsqueeze(1).to_broadcast([128, 4, 128]), op=ALU.mult),
                                    reads=[pt, masks], writes=[pt])

                            def f(e, pt=pt, slot=slot, bi=bi, oac=oac, kvh=kvh, nb=nb):
                                ins = None
                                for h in range(4):
                                    ins = e.matmul(oac.t[:, h, 0:65], lhsT=pt.t[:, h * 128:(h + 1) * 128],
                                                   rhs=Vg.t[:, slot, kvh, :], start=(bi == 0 and h == 0),
                                                   stop=(bi == nb - 1 and h == 3), skip_group_check=True)
                                return ins
                            P.op("pe", f, reads=[pt, Vg], writes=[oac])
                        P.op("dve", lambda e, oac=oac, qc=qc: e.tensor_tensor(
                            out=den.t[:, 0:4], in0=oac.t[:, :, 64], in1=esink.t[:, qc * 4:qc * 4 + 4], op=ALU.add),
                            reads=[oac, esink], writes=[den])
                        P.op("dve", lambda e: e.reciprocal(out=den.t[:, 0:4], in_=den.t[:, 0:4]), reads=[den], writes=[den])
                        at = attn[lo]
                        P.op("dve", lambda e, oac=oac, at=at: e.tensor_tensor(
                            out=at.t[:].rearrange("p (h d) -> p h d", d=64), in0=oac.t[:, :, 0:64],
                            in1=den.t[:, 0:4].unsqueeze(2).to_broadcast([128, 4, 64]), op=ALU.mult),
                            reads=[oac, den], writes=[at])
                        tp = TP[nxt("tp", 2)]

                        def f(e, tp=tp, at=at):
                            ins = None
                            for i in range(2):
                                ins = e.transpose(tp.t[:, i * 128:(i + 1) * 128], at.t[:, i * 128:(i + 1) * 128], ident)
                            return ins
                        P.op("pe", f, reads=[at, cmat], writes=[tp])
                        copy_op(evac_engine(), mixT.t[:, 2 * qc:2 * qc + 2, lo * 128:(lo + 1) * 128],
                                tp.t[:, 0:256].rearrange("p (c t) -> p c t", t=128), [tp], [mixT])
                    pend = nxt_units
                if g == 0 and ckpt("g0attn"):
                    return nc
                for n in range(D // WCH):
                    wsl, wv = load_w(w_out[:, n * WCH:(n + 1) * WCH], WCH)
                    xq = xp[n % 2]
                    gq = g1p[n % 2]
                    xo = x1p[n % 2]
                    r0 = (own0 + 1) * 128
                    dma("sp", xq, xq.t[:], None, x_ext[r0:r0 + 256, n * WCH:(n + 1) * WCH].rearrange("(l p) n -> p l n", p=128))
                    dma("sp", gq, gq.t[:], G1, G1d[:, n * WCH:(n + 1) * WCH])
                    for lo in range(2):
                        acc = ACC[nxt("acc", 2)]

                        def f(e, acc=acc, lo=lo, wv=wv):
                            ins = None
                            for k in range(KC):
                                ins = e.matmul(acc.t[:, 0:WCH], lhsT=mixT.t[:, k, lo * 128:(lo + 1) * 128], rhs=wv[:, k, :],
                                               start=(k == 0), stop=(k == KC - 1))
                            return ins
                        P.op("pe", f, reads=[mixT, wsl], writes=[acc])
                        yt = ytmp[lo]
                        P.op("dve", lambda e, acc=acc, yt=yt, gq=gq: e.tensor_tensor(out=yt.t[:], in0=acc.t[:, 0:WCH], in1=gq.t[:], op=ALU.mult),
                             reads=[acc, gq], writes=[yt])
                        P.op("dve", lambda e, yt=yt, xq=xq, xo=xo, lo=lo: e.tensor_tensor(out=xo.t[:, lo, :], in0=yt.t[:], in1=xq.t[:, lo, :], op=ALU.add),
                             reads=[yt, xq], writes=[xo])
                    dma("sp", X1, X1d[own0 * 128:own0 * 128 + 256, n * WCH:(n + 1) * WCH].rearrange("(l p) n -> p l n", p=128),
                        xo, xo.t[:], sem_tl=xo, track_w=False)

            P.op("dve", lambda e: e.scalar_tensor_tensor(out=A2.t[:], in0=modv(4, 0), scalar=1.0, in1=n2T.t[:],
                                                         op0=ALU.add, op1=ALU.mult), reads=[MOD, n2T], writes=[A2])
            P.op("dve", lambda e: e.tensor_copy(S2.t[:], modv(3, 0)), reads=[MOD], writes=[S2])
            P.barrier()
            P.emit()

        if STOP != "phaseA":
            phaseB(P, c, nc, locals())
        P.emit()
    return nc


def phaseB(P, c, nc, L):
    D, T, NT, KC, NE, FF, CAP = c.D, c.T, c.NT, c.KC, c.NE, c.FF, c.CAP
    X1, XE, YE, G2, YO = L["X1"], L["XE"], L["YE"], L["G2"], L["YO"]
    X1d, XEh, YEq, G2d, y_out = L["X1d"], L["XEh"], L["YEq"], L["G2d"], L["y_out"]
    HROWS, YW, NYC = L["HROWS"], L["YW"], L["NYC"]
    CH = T // 4
    NS = CH // 128
    A2, S2 = L["A2"], L["S2"]
    dma = L["dma"]
    debug = L["debug"]
    FK = FF // 128
    P.bc_val = HROWS - 1
    with ExitStack() as scP:
        P.scope = scP
        SLi = P.sb("SLi", [128, NT, 4], I32)
        SLi1 = P.sb("SLi1", [128, NT, 4], I32)
        FLi = P.sb("FLi", [1, 3 * NE], I32)
        dmy = P.sb("dmy", [1, NE], F32)
        P.dummy = (dmy.t[0:1, :], L["ecap_d"].rearrange("(a n) -> a n", a=1))
        WT = P.sb("WT", [128, NT, 4], F32)
        combT = P.sb("combT", [32, NT, 128], BF16)
        cmat = P.sb("cmatB", [128, 3, 128], BF16)
        cstB = P.sb("cstB", [128, 2], F32)
        ident = cmat.t[:, 0, :]
        dma("pool", cmat, cmat.t[:], None, L["cmat_d"].rearrange("p (a b) -> p a b", a=3), sem_tl=cstB)
        P.op("dve", lambda e: e.memset(cstB.t[:, 0:1], NORM_EPS), writes=[cstB])

        with ExitStack() as scB:
            P.scope = scB
            TP = [P.ps(f"TPb{i}", [128, 512], BF16) for i in range(2)]
            GU = [P.ps(f"GU{i}", [128, 512], F32) for i in range(4)]
            DN = [P.ps(f"DN{i}", [128, 512], F32) for i in range(2)]
            RT = DN[0]
            rr = {"tp": 0, "ev": 0, "w": 0, "dn": 0, "ys": 0, "gu": 0}

            def nxt(key, n):
                v = rr[key]
                rr[key] = (v + 1) % n
                return v

            def evac_engine():
                return ("act", "dve")[nxt("ev", 2)]

            wr_bf = P.sb("wr_bf", [128, KC, NE], BF16)
            brt = P.sb("brt", [128, NE], F32)
            ecap = P.sb("ecap", [128, NE], F32)
            bgT = P.sb("bgT", [128, NE * FK], F32)
            buT = P.sb("buT", [128, NE * FK], F32)
            Rf = P.sb("Rf", [128, NE], F32)
            Rb = P.sb("Rb", [128, NE], BF16)
            dma("pool", wr_bf, wr_bf.t[:], None, L["w_router"].rearrange("(k p) n -> p k n", p=128), sem_tl=cstB)
            dma("sp", brt, brt.t[:], None, L["b_router"].partition_broadcast(128), sem_tl=cstB)
            dma("sp", ecap, ecap.t[:], None, L["ecap_d"].partition_broadcast(128), sem_tl=cstB)
            dma("sp", bgT, bgT.t[:], None, L["bgT_d"], sem_tl=cstB)
            dma("sp", buT, buT.t[:], None, L["buT_d"], sem_tl=cstB)
            P.op("dve", lambda e: e.memset(Rf.t[:], 0.0), writes=[Rf])

            scB1 = ExitStack()
            P.scope = scB1
            x1t = P.sb("x1t", [128, D], F32)
            xs2 = [P.sb(f"xs2{i}", [128, D], BF16) for i in range(2)]
            ss = P.sb("ssB", [128, 1], F32)
            h2T = P.sb("h2T", [128, KC, 128], BF16)
            lg = P.sb("lg", [128, NE], F32)
            top8 = P.sb("top8", [128, 8], F32)
            mask = P.sb("mask", [128, NE], F32)
            maskb = P.sb("maskb", [128, NE], BF16)
            slotf = P.sb("slotf", [128, NE], F32)
            ovf = P.sb("ovf", [128, NE], F32)
            eq = P.sb("eq", [128, NE], F32)
            SLf = P.sb("SLf", [128, 4], F32)
            wex = P.sb("wex", [128, 4], F32)
            wsum = P.sb("wsum", [128, 1], F32)
            ntop = P.sb("ntop", [128, 1], F32)
            comb = P.sb("comb", [128, NE], F32)
            combb = P.sb("combb", [128, NE], BF16)

            for j in range(NT):
                xs_ = xs2[j % 2]
                dma("sp", x1t, x1t.t[:], X1, X1d[j * 128:(j + 1) * 128, :])
                P.op("dve", lambda e: e.memset(ss.t[:], 0.0), writes=[ss])
                P.op("act", lambda e, xs_=xs_: e.activation(out=xs_.t[:], in_=x1t.t[:], func=AF.Square, accum_out=ss.t[:, 0:1]),
                     reads=[x1t], writes=[xs_, ss])
                P.op("act", lambda e: e.activation(out=ss.t[:], in_=ss.t[:], func=AF.Sqrt, bias=cstB.t[:, 0:1], scale=1.0 / D),
                     reads=[ss, cstB], writes=[ss])
                P.op("dve", lambda e: e.reciprocal(out=ss.t[:], in_=ss.t[:]), reads=[ss], writes=[ss])
                P.op("dve", lambda e, xs_=xs_: e.tensor_scalar(out=xs_.t[:], in0=x1t.t[:], scalar1=ss.t[:, 0:1], scalar2=0.0,
                                                              op0=ALU.mult, op1=ALU.add), reads=[x1t, ss], writes=[xs_])
                for c0 in range(0, KC, 4):
                    tp = TP[nxt("tp", 2)]

                    def f(e, c0=c0, tp=tp, xs_=xs_):
                        ins = None
                        for i in range(4):
                            ins = e.transpose(tp.t[:, i * 128:(i + 1) * 128], xs_.t[:, (c0 + i) * 128:(c0 + i + 1) * 128], ident)
                        return ins
                    P.op("pe", f, reads=[xs_, cmat], writes=[tp])
                    for i in range(4):
                        cc = c0 + i
                        if evac_engine() == "act":
                            P.op("act", lambda e, tp=tp, i=i, cc=cc: e.activation(
                                out=h2T.t[:, cc, :], in_=tp.t[:, i * 128:(i + 1) * 128], func=AF.Identity,
                                bias=S2.t[:, cc:cc + 1], scale=A2.t[:, cc:cc + 1]), reads=[tp, A2, S2], writes=[h2T])
                        else:
                            P.op("dve", lambda e, tp=tp, i=i, cc=cc: e.tensor_scalar(
                                out=h2T.t[:, cc, :], in0=tp.t[:, i * 128:(i + 1) * 128], scalar1=A2.t[:, cc:cc + 1],
                                scalar2=S2.t[:, cc:cc + 1], op0=ALU.mult, op1=ALU.add), reads=[tp, A2, S2], writes=[h2T])

                def f(e):
                    ins = None
                    for k in range(KC):
                        ins = e.matmul(RT.t[:, 0:NE], lhsT=h2T.t[:, k, :], rhs=wr_bf.t[:, k, :], start=(k == 0), stop=(k == KC - 1))
                    return ins
                P.op("pe", f, reads=[h2T, wr_bf], writes=[RT])
                P.op("dve", lambda e: e.tensor_tensor(out=lg.t[:], in0=RT.t[:, 0:NE], in1=brt.t[:], op=ALU.add),
                     reads=[RT, brt], writes=[lg])
                if debug:
                    dma("sp", L["LGo"], L["LGd"][j * 128:(j + 1) * 128, :], lg, lg.t[:], sem_tl=lg)
                P.op("dve", lambda e: e.max(out=top8.t[:], in_=lg.t[:]), reads=[lg], writes=[top8])
                P.op("dve", lambda e: e.tensor_scalar(out=mask.t[:], in0=lg.t[:], scalar1=top8.t[:, 3:4], scalar2=0.0,
                                                      op0=ALU.is_ge, op1=ALU.add), reads=[lg, top8], writes=[mask])
                P.op("dve", lambda e: e.tensor_copy(maskb.t[:], mask.t[:]), reads=[mask], writes=[maskb])
                P.op("dve", lambda e: e.tensor_copy(Rb.t[:], Rf.t[:]), reads=[Rf], writes=[Rb])

                def f(e):
                    e.matmul(RT.t[:, 64:64 + NE], lhsT=cmat.t[:, 1, :], rhs=maskb.t[:], start=True, stop=False)
                    return e.matmul(RT.t[:, 64:64 + NE], lhsT=cmat.t[:, 2, :], rhs=Rb.t[:], start=False, stop=True)
                P.op("pe", f, reads=[cmat, maskb, Rb], writes=[RT])
                P.op("dve", lambda e: e.tensor_tensor(out=Rf.t[:], in0=Rf.t[:], in1=mask.t[:], op=ALU.add),
                     reads=[Rf, mask], writes=[Rf])
                P.op("dve", lambda e: e.tensor_tensor(out=slotf.t[:], in0=RT.t[:, 64:64 + NE], in1=ecap.t[:], op=ALU.add),
                     reads=[RT, ecap], writes=[slotf])
                for k in range(4):
                    P.op("dve", lambda e, k=k: e.tensor_scalar(out=eq.t[:], in0=lg.t[:], scalar1=top8.t[:, k:k + 1], scalar2=0.0,
                                                               op0=ALU.is_equal, op1=ALU.add), reads=[lg, top8], writes=[eq])
                    P.op("dve", lambda e: e.tensor_tensor(out=eq.t[:], in0=eq.t[:], in1=slotf.t[:], op=ALU.mult),
                         reads=[eq, slotf], writes=[eq])
                    P.op("dve", lambda e, k=k: e.tensor_reduce(out=SLf.t[:, k:k + 1], in_=eq.t[:], axis=AX.X, op=ALU.add),
                         reads=[eq], writes=[SLf])
                P.op("dve", lambda e, j=j: e.tensor_copy(SLi.t[:, j, :], SLf.t[:]), reads=[SLf], writes=[SLi])
                P.op("dve", lambda e: e.tensor_scalar(out=ntop.t[:], in0=top8.t[:, 0:1], scalar1=-1.0, scalar2=0.0,
                                                      op0=ALU.mult, op1=ALU.add), reads=[top8], writes=[ntop])
                P.op("act", lambda e: e.activation(out=wex.t[:], in_=top8.t[:, 0:4], func=AF.Exp, bias=ntop.t[:, 0:1], scale=1.0),
                     reads=[top8, ntop], writes=[wex])
                P.op("dve", lambda e: e.tensor_reduce(out=wsum.t[:], in_=wex.t[:], axis=AX.X, op=ALU.add), reads=[wex], writes=[wsum])
                P.op("dve", lambda e: e.reciprocal(out=wsum.t[:], in_=wsum.t[:]), reads=[wsum], writes=[wsum])
                P.op("dve", lambda e, j=j: e.tensor_scalar(out=WT.t[:, j, :], in0=wex.t[:], scalar1=wsum.t[:, 0:1], scalar2=0.0,
                                                           op0=ALU.mult, op1=ALU.add), reads=[wex, wsum], writes=[WT])
                P.op("act", lambda e: e.activation(out=comb.t[:], in_=lg.t[:], func=AF.Exp, bias=ntop.t[:, 0:1], scale=1.0),
                     reads=[lg, ntop], writes=[comb])
                P.op("dve", lambda e: e.tensor_tensor(out=comb.t[:], in0=comb.t[:], in1=mask.t[:], op=ALU.mult),
                     reads=[comb, mask], writes=[comb])
                P.op("dve", lambda e: e.tensor_scalar(out=combb.t[:], in0=comb.t[:], scalar1=wsum.t[:, 0:1], scalar2=0.0,
                                                      op0=ALU.mult, op1=ALU.add), reads=[comb, wsum], writes=[combb])
                tp = TP[nxt("tp", 2)]
                P.op("pe", lambda e, tp=tp: e.transpose(tp.t[0:NE, 0:128], combb.t[:, :], ident), reads=[combb, cmat], writes=[tp])
                P.op("act", lambda e, tp=tp, j=j: e.activation(out=combT.t[0:NE, j, :], in_=tp.t[0:NE, 0:128], func=AF.Copy),
                     reads=[tp], writes=[combT])
                for k in range(4):
                    for hh in range(2):
                        P.op("pool", lambda e, j=j, k=k, xs_=xs_, hh=hh: e.indirect_dma_start(
                            out=XEh[hh][:, :], out_offset=bass.IndirectOffsetOnAxis(ap=SLi.t[:, j, k:k + 1], axis=0),
                            in_=xs_.t[:, hh * (D // 2):(hh + 1) * (D // 2)], in_offset=None, bounds_check=P.bc_reg, oob_is_err=False),
                            reads=[xs_, SLi], writes=[], dsem=XE.dsw)
                        XE.b.last_w = (XE.dsw, None)
            P.op("dve", lambda e: e.tensor_copy(Rb.t[:], Rf.t[:]), reads=[Rf], writes=[Rb])
            P.op("pe", lambda e: e.matmul(RT.t[:, 0:NE], lhsT=cmat.t[:, 2, :], rhs=Rb.t[:], start=True, stop=True),
                 reads=[cmat, Rb], writes=[RT])
            for cp in range(1, 4):
                P.op("dve", lambda e, cp=cp: e.tensor_scalar(out=comb.t[0:1, :], in0=RT.t[0:1, 0:NE], scalar1=float(cp * CH) + 0.5, scalar2=0.0,
                                                             op0=ALU.is_gt, op1=ALU.add), reads=[RT], writes=[comb])
                P.op("dve", lambda e, cp=cp: e.tensor_copy(FLi.t[0:1, (cp - 1) * NE:cp * NE], comb.t[0:1, :]), reads=[comb], writes=[FLi])

            P.barrier()
            P.emit()
            scB1.close()
            P.scope = scB
            if STOP == "B1":
                return
            NWB = 6
            wring = [P.sb(f"wrb{i}", [128, KC * 128], BF16) for i in range(NWB)]
            wringc = [P.sb(f"wrc{i}", [128, KC * 128], BF16) for i in range(2)]
            xet = [P.sb(f"xet{i}", [128, D], BF16) for i in range(2)]
            XeT = [P.sb(f"XeT{i}", [128, KC, CH], BF16) for i in range(2)]
            actT = [P.sb(f"actT{i}", [128, FK, CH], BF16) for i in range(2)]
            gs = [P.sb(f"gs{i}", [128, CH], F32) for i in range(2)]
            sg = [P.sb(f"sg{i}", [128, CH], F32) for i in range(2)]
            us = [P.sb(f"us{i}", [128, CH], F32) for i in range(2)]
            ga = [P.sb(f"ga{i}", [128, CH], F32) for i in range(2)]
            ysb = [P.sb(f"ysb{i}", [128, 512], F32) for i in range(4)]
            assert KC * 128 == FK * 512, "weight ring slot size assumption (D == 4*FF)"
            rr.update({"xe": 0, "xt": 0, "at": 0, "wc": 0})

            def load_piece(view_fn, src_ap, cond=False):
                sl = wringc[nxt("wc", 2)] if cond else wring[nxt("w", NWB)]
                v = view_fn(sl.t)
                dma("pool", sl, v, None, src_ap)
                return sl, v

            def load_xe(e_, c_, xT):
                r0 = e_ * T + c_ * CH
                for s_i in range(NS):
                    xb = xet[nxt("xe", 2)]
                    for hh in range(2):
                        P.op("sp", lambda e, xb=xb, s_i=s_i, hh=hh, r0=r0: e.dma_start(
                            out=xb.t[:, hh * (D // 2):(hh + 1) * (D // 2)], in_=XEh[hh][r0 + s_i * 128:r0 + (s_i + 1) * 128, :]),
                            reads=[XE], writes=([xb] if hh == 0 else []), dsem=xb.ds)
                        xb.b.last_w = (xb.ds, None)
                    for c0 in range(0, KC, 4):
                        tp = TP[nxt("tp", 2)]

                        def f(e, c0=c0, tp=tp, xb=xb):
                            ins = None
                            for i in range(4):
                                ins = e.transpose(tp.t[:, i * 128:(i + 1) * 128], xb.t[:, (c0 + i) * 128:(c0 + i + 1) * 128], ident)
                            return ins
                        P.op("pe", f, reads=[xb, cmat], writes=[tp])
                        for i in range(4):
                            cc = c0 + i
                            if evac_engine() == "act":
                                P.op("act", lambda e, tp=tp, i=i, cc=cc, s_i=s_i, xT=xT: e.activation(
                                    out=xT.t[:, cc, s_i * 128:(s_i + 1) * 128], in_=tp.t[:, i * 128:(i + 1) * 128], func=AF.Identity,
                                    bias=S2.t[:, cc:cc + 1], scale=A2.t[:, cc:cc + 1]), reads=[tp, A2, S2], writes=[xT])
                            else:
                                P.op("dve", lambda e, tp=tp, i=i, cc=cc, s_i=s_i, xT=xT: e.tensor_scalar(
                                    out=xT.t[:, cc, s_i * 128:(s_i + 1) * 128], in0=tp.t[:, i * 128:(i + 1) * 128],
                                    scalar1=A2.t[:, cc:cc + 1], scalar2=S2.t[:, cc:cc + 1], op0=ALU.mult, op1=ALU.add),
                                    reads=[tp, A2, S2], writes=[xT])

            def expert_pass(e_, c_, xT, prefetch=None):
                cnd = c_ > 0
                aT = actT[nxt("at", 2)]
                r0 = e_ * T + c_ * CH
                for m in range(FK):
                    slg, vg = load_piece(lambda t: t[:, 0:KC * 128].rearrange("p (k n) -> p k n", n=128),
                                         L["w_gate"][e_, :, m * 128:(m + 1) * 128].rearrange("(k p) n -> p k n", p=128), cond=cnd)
                    slu, vu = load_piece(lambda t: t[:, 0:KC * 128].rearrange("p (k n) -> p k n", n=128),
                                         L["w_up"][e_, :, m * 128:(m + 1) * 128].rearrange("(k p) n -> p k n", p=128), cond=cnd)
                    bj = e_ * FK + m
                    pi = nxt("gu", 2)
                    G = GU[2 * pi]
                    U = GU[2 * pi + 1]
                    for (acc, sl, v) in ((G, slg, vg), (U, slu, vu)):
                        def f(e, acc=acc, v=v, xT=xT):
                            ins = None
                            for k in range(KC):
                                ins = e.matmul(acc.t[:, 0:CH], lhsT=v[:, k, :], rhs=xT.t[:, k, :], start=(k == 0), stop=(k == KC - 1))
                            return ins
                        P.op("pe", f, reads=[sl, xT], writes=[acc])
                    g_, s_, u_, a_ = gs[pi], sg[pi], us[pi], ga[pi]
                    P.op("dve", lambda e, G=G, bj=bj, g_=g_: e.tensor_scalar(out=g_.t[:], in0=G.t[:, 0:CH], scalar1=bgT.t[:, bj:bj + 1], scalar2=7.0,
                                                                         op0=ALU.add, op1=ALU.min), reads=[G, bgT], writes=[g_])
                    P.op("act", lambda e, g_=g_, s_=s_: e.activation(out=s_.t[:], in_=g_.t[:], func=AF.Sigmoid, scale=1.702), reads=[g_], writes=[s_])
                    P.op("dve", lambda e, U=U, bj=bj, u_=u_: e.tensor_scalar(out=u_.t[:], in0=U.t[:, 0:CH], scalar1=buT.t[:, bj:bj + 1], scalar2=7.0,
                                                                         op0=ALU.add, op1=ALU.min), reads=[U, buT], writes=[u_])
                    P.op("dve", lambda e, u_=u_: e.tensor_scalar(out=u_.t[:], in0=u_.t[:], scalar1=-7.0, scalar2=1.0, op0=ALU.max, op1=ALU.add),
                         reads=[u_], writes=[u_])
                    P.op("dve", lambda e, g_=g_, s_=s_, a_=a_: e.tensor_tensor(out=a_.t[:], in0=g_.t[:], in1=s_.t[:], op=ALU.mult), reads=[g_, s_], writes=[a_])
                    P.op("dve", lambda e, aT=aT, m=m, a_=a_, u_=u_: e.tensor_tensor(out=aT.t[:, m, :], in0=a_.t[:], in1=u_.t[:], op=ALU.mult),
                         reads=[a_, u_], writes=[aT])
                if prefetch is not None:
                    prefetch()
                for n in range(D // 512):
                    sld, vd = load_piece(lambda t: t[:, 0:FK * 512].rearrange("p (k n) -> p k n", n=512),
                                         L["w_down"][e_, :, n * 512:(n + 1) * 512].rearrange("(k p) n -> p k n", p=128), cond=cnd)
                    for s_i in range(NS):
                        dn = DN[nxt("dn", 2)]

                        def f(e, dn=dn, s_i=s_i, vd=vd, aT=aT):
                            ins = None
                            for k in range(FK):
                                ins = e.matmul(dn.t[:], lhsT=aT.t[:, k, s_i * 128:(s_i + 1) * 128], rhs=vd[:, k, :], start=(k == 0), stop=(k == FK - 1))
                            return ins
                        P.op("pe", f, reads=[aT, sld], writes=[dn])
                        yb = ysb[nxt("ys", 4)]
                        if evac_engine() == "act":
                            P.op("act", lambda e, dn=dn, yb=yb: e.activation(out=yb.t[:], in_=dn.t[:], func=AF.Copy), reads=[dn], writes=[yb])
                        else:
                            P.op("dve", lambda e, dn=dn, yb=yb: e.tensor_copy(yb.t[:], dn.t[:]), reads=[dn], writes=[yb])
                        hc = (n * 512) // YW
                        c0_ = n * 512 - hc * YW
                        dma("sp", YE, YEq[hc][r0 + s_i * 128:r0 + (s_i + 1) * 128, c0_:c0_ + 512], yb, yb.t[:], sem_tl=yb, track_w=False)

            load_xe(0, 0, XeT[0])
            for e_ in range(NE):
                cur = XeT[e_ % 2]
                nxt_ = XeT[(e_ + 1) % 2]
                pf = (lambda e_=e_, nxt_=nxt_: load_xe(e_ + 1, 0, nxt_)) if e_ + 1 < NE else None
                expert_pass(e_, 0, cur, prefetch=pf)
                for c_ in range(1, 4):
                    fi = (c_ - 1) * NE + e_
                    P.cond_begin(FLi, FLi.t[0:1, fi:fi + 1])
                    load_xe(e_, c_, cur)
                    expert_pass(e_, c_, cur)
                    P.cond_end()
            P.barrier()
            P.emit()
        if STOP == "B2":
            return

        with ExitStack() as scC:
            P.scope = scC
            BT = [P.ps(f"BT{i}", [128, 512], F32) for i in range(2)]
            NY = 6
            Yk = [P.sb(f"Yk{i}", [128, D], F32) for i in range(NY)]
            x1b = [P.sb(f"x1b{i}", [128, D], F32) for i in range(2)]
            ob = [P.sb(f"ob{i}", [128, D], F32) for i in range(2)]
            G2B = P.sb("G2B", [128, D], F32)
            bdn = P.sb("bdn", [NE, D], BF16)
            dma("sp", G2B, G2B.t[:], G2, G2d)
            dma("pool", bdn, bdn.t[:], None, L["b_down"])
            for i in range(NY):
                P.op("dve", lambda e, i=i: e.memset(Yk[i].t[:], 0.0), writes=[Yk[i]])
            yi = 0
            for j in range(NT):
                ys = []
                for k in range(4):
                    yt = Yk[yi % NY]
                    yi += 1
                    for hc in range(NYC):
                        P.op("pool", lambda e, yt=yt, j=j, k=k, hc=hc: e.indirect_dma_start(
                            out=yt.t[:, hc * YW:(hc + 1) * YW], out_offset=None, in_=YEq[hc][:, :],
                            in_offset=bass.IndirectOffsetOnAxis(ap=SLi.t[:, j, k:k + 1], axis=0),
                            bounds_check=P.bc_reg, oob_is_err=False), reads=[YE, SLi], writes=([yt] if hc == 0 else []), dsem=yt.dsw)
                        yt.b.last_w = (yt.dsw, None)
                    ys.append(yt)
                xb = x1b[j % 2]
                o = ob[j % 2]
                dma("sp", xb, xb.t[:], X1, X1d[j * 128:(j + 1) * 128, :])
                for n in range(D // 512):
                    bt = BT[n % 2]
                    cs = slice(n * 512, (n + 1) * 512)
                    P.op("pe", lambda e, bt=bt, j=j, cs=cs: e.matmul(bt.t[:], lhsT=combT.t[0:NE, j, :], rhs=bdn.t[0:NE, cs], start=True, stop=True),
                         reads=[combT, bdn], writes=[bt])
                    eng = "dve"
                    P.op("dve", lambda e, bt=bt, j=j, cs=cs, o=o, y0=ys[0]: e.scalar_tensor_tensor(
                        out=o.t[:, cs], in0=y0.t[:, cs], scalar=WT.t[:, j, 0:1], in1=bt.t[:], op0=ALU.mult, op1=ALU.add),
                        reads=[ys[0], WT, bt], writes=[o])
                    for k in range(1, 4):
                        P.op("dve", lambda e, j=j, cs=cs, o=o, yk=ys[k], k=k: e.scalar_tensor_tensor(
                            out=o.t[:, cs], in0=yk.t[:, cs], scalar=WT.t[:, j, k:k + 1], in1=o.t[:, cs], op0=ALU.mult, op1=ALU.add),
                            reads=[ys[k], WT, o], writes=[o])
                    P.op(eng, lambda e, cs=cs, o=o: e.tensor_tensor(out=o.t[:, cs], in0=o.t[:, cs], in1=G2B.t[:, cs], op=ALU.mult),
                         reads=[o, G2B], writes=[o])
                    P.op(eng, lambda e, cs=cs, o=o, xb=xb: e.tensor_tensor(out=o.t[:, cs], in0=o.t[:, cs], in1=xb.t[:, cs], op=ALU.add),
                         reads=[o, xb], writes=[o])
                dma("sp", YO, y_out[j * 128:(j + 1) * 128, :], o, o.t[:], sem_tl=o)
            P.barrier()
            P.emit()


def _feat_major(v, nch):
    return np.ascontiguousarray(np.asarray(v, np.float32).reshape(nch, 128).T)


def _rope_tables(cfg, pos):
    half = 32
    inv_freq = (10000.0 ** (-np.arange(0, half, 2, dtype=np.float32) / np.float32(half))).astype(np.float32)
    row = (pos // cfg.GRID_W).astype(np.float32)
    col = (pos % cfg.GRID_W).astype(np.float32)
    out_c = np.zeros((len(pos), 64), np.float32)
    out_s = np.zeros((len(pos), 64), np.float32)
    for hi, base in enumerate((row, col)):
        ang = base[:, None] * inv_freq[None, :]
        cs, sn = np.cos(ang).astype(np.float32), np.sin(ang).astype(np.float32)
        o = hi * 32
        out_c[:, o:o + 16] = cs
        out_c[:, o + 16:o + 32] = cs
        out_s[:, o:o + 16] = -sn
        out_s[:, o + 16:o + 32] = sn
    return out_c, out_s


def _pool_mats(cfg, g0, n):
    out = np.zeros((cfg.NPG, 3, 128, 128), np.float32)
    for gi, w in enumerate((2, 4, 8, 16)):
        for t in range(128):
            gt = g0 + t
            lo = min(max(gt - w // 2, 0), n)
            hi = min(max(gt + w // 2, 0), n)
            cnt = hi - lo
            for gs in range(lo, hi):
                s = gs - g0
                kd = 0 if s < 0 else (1 if s < 128 else 2)
                out[gi, kd, s - (kd - 1) * 128, t] += 1.0 / cnt
            out[gi, 1, t, t] -= 1.0
    return out


def make_in_maps(cfg, inputs):
    c = cfg
    f = lambda k: np.asarray(inputs[k], np.float32)
    x, cc, ctx, c_ctx = f("x"), f("c"), f("ctx"), f("c_ctx")
    swap = np.concatenate([np.arange(16, 32), np.arange(0, 16), np.arange(48, 64), np.arange(32, 48)])
    gq, gk = f("q_norm_g")[0], f("k_norm_g")[0]
    gvec = np.concatenate([gq, gq[swap], gk, gk[swap]]).reshape(256).astype(np.float32)
    FK = c.FF // 128
    common = {
        "w_ada": f("w_ada")[0], "b_adaT": _feat_major(f("b_ada")[0], c.NMODC),
        "n1T": _feat_major(f("norm1_g")[0], c.KC), "n2T": _feat_major(f("norm2_g")[0], c.KC),
        "w_in": f("w_in")[0], "gvec": gvec, "sinks": np.ascontiguousarray(f("sinks")[0].reshape(-1)),
        "w_pool": f("w_pool")[0], "pscT": _feat_major(f("pool_scale")[0], c.PW // 128),
        "w_out": f("w_out")[0], "w_router": f("w_router")[0], "b_router": np.ascontiguousarray(f("b_router")[0].reshape(-1)),
        "w_gate": f("w_gate")[0], "w_up": f("w_up")[0], "w_down": f("w_down")[0],
        "bgT": np.ascontiguousarray(f("b_gate")[0].reshape(c.NE, FK, 128).transpose(2, 0, 1).reshape(128, c.NE * FK)),
        "buT": np.ascontiguousarray(f("b_up")[0].reshape(c.NE, FK, 128).transpose(2, 0, 1).reshape(128, c.NE * FK)),
        "b_down": f("b_down")[0],
        "ecap": (np.arange(c.NE, dtype=np.float32) * c.T),
    }
    cm = np.zeros((128, 3, 128), np.float32)
    cm[:, 0, :] = np.eye(128, dtype=np.float32)
    cm[:, 1, :] = np.triu(np.ones((128, 128), np.float32), 1)
    cm[:, 2, :] = 1.0
    common["cmat"] = cm.reshape(128, 384)
    kk = np.arange(128)[:, None]
    qq = np.arange(128)[None, :]
    m_prev = (qq <= kk).astype(np.float32)
    m_next = (kk <= qq).astype(np.float32)
    in_maps = []
    for core in range(NCORES):
        b = core // c.CPB
        t0 = (core % c.CPB) * c.T
        first = (core % c.CPB) == 0
        last = (core % c.CPB) == c.CPB - 1
        xe = np.zeros(((c.NT + 2) * 128, c.D), np.float32)
        lo, hi = t0 - 128, t0 + c.T + 128
        slo, shi = max(lo, 0), min(hi, c.SEQ)
        xe[slo - lo:shi - lo] = x[b, slo:shi]
        pos = np.arange(lo, hi)
        rc, rs = _rope_tables(c, np.clip(pos, 0, c.SEQ - 1))
        rc = np.concatenate([rc, np.ones((c.CTX, 64), np.float32)])
        rs = np.concatenate([rs, np.zeros((c.CTX, 64), np.float32)])
        mk = np.zeros((128, 4, 128), np.float32)
        mk[:, 0] = 0.0 if first else m_prev
        mk[:, 1] = m_prev
        mk[:, 2] = m_next
        mk[:, 3] = 0.0 if last else m_next
        pb = np.zeros((c.NPG, 3, 3, 128, 128), np.float32)
        pb[:, 0] = _pool_mats(c, t0, c.SEQ)
        pb[:, 1] = _pool_mats(c, t0 + 128, c.SEQ)
        pb[:, 2] = _pool_mats(c, t0 + c.T - 128, c.SEQ)
        pbl = np.ascontiguousarray(pb.reshape(c.NPG * 9, 128, 128).transpose(1, 0, 2)).reshape(128, c.NPG * 9 * 128)
        cT = np.stack([_feat_major(cc[b], c.KC), _feat_major(c_ctx, c.KC)], axis=2).reshape(128, c.KC * 2)
        m = dict(common)
        m.update({"x_ext": xe, "ctxb": np.ascontiguousarray(ctx[b]), "cT": np.ascontiguousarray(cT),
                  "rope_c": rc, "rope_s": rs, "masks": mk.reshape(128, 512), "poolB": pbl})
        in_maps.append(m)
    return in_maps


_CACHE = {}


def kernel(**inputs):
    cfg = Cfg()
    if "nc" not in _CACHE:
        _CACHE["nc"] = build_program(cfg)
    nc = _CACHE["nc"]
    in_maps = make_in_maps(cfg, inputs)
    res = run_bass_kernel_spmd(nc, in_maps, core_ids=list(range(NCORES)))
    out = np.concatenate([np.asarray(r["y"]) for r in res.results], axis=0)
    return out.reshape(cfg.BATCH, cfg.SEQ, cfg.D).astype(np.float32, copy=False)
```

```python
import numpy as np
from contextlib import ExitStack
import concourse.bass as bass
import concourse.mybir as mybir
from concourse.bass_utils import run_bass_kernel_spmd

F32 = mybir.dt.float32
BF16 = mybir.dt.bfloat16
I32 = mybir.dt.int32
ALU = mybir.AluOpType
AF = mybir.ActivationFunctionType
AX = mybir.AxisListType

NCORES = 8
NORM_EPS = 1e-6
BIGSLOT = 1.0e6


class Cfg:
    def __init__(self, D=4096, BATCH=2, SEQ=8192, CTX=256, FF=1024, NE=32, CAP=1024, GRID_W=64):
        self.D = D
        self.BATCH = BATCH
        self.SEQ = SEQ
        self.CTX = CTX
        self.FF = FF
        self.NE = NE
        self.CAP = CAP
        self.GRID_W = GRID_W
        self.HD = 64
        self.AW = D // 2
        self.NQ = self.AW // 64
        self.NKV = max(1, self.NQ // 8)
        self.GQ = self.NQ // self.NKV
        self.KVW = self.NKV * 64
        self.PW = D - self.AW
        self.NPG = 4
        self.PGW = self.PW // 4
        self.INW = self.AW + 2 * self.KVW + self.PW
        self.T = BATCH * SEQ // NCORES
        self.NT = self.T // 128
        self.KC = D // 128
        self.CPB = NCORES // BATCH
        self.NMODC = 6 * D // 128
        self.WCH = 256
        self.PIECE = max(self.KC * 256, (FF // 128) * 512)
        assert self.NT % 2 == 0 and self.NT >= 4
        assert CAP % 128 == 0


class Sem:
    def __init__(self, h):
        self.h = h
        self.count = 0
        self.sw = False


class Buf:
    __slots__ = ("name", "last_w", "readers")

    def __init__(self, name):
        self.name = name
        self.last_w = None
        self.readers = []


class Tl:
    def __init__(self, P, t, name, is_dram=False):
        self.t = t
        self.b = Buf(name)
        self.P = P
        self._ds = None
        self._dsw = None
        self.name = name
        self.is_dram = is_dram

    @property
    def ds(self):
        if self._ds is None:
            self._ds = self.P.sem("d_" + self.name)
        return self._ds

    @property
    def dsw(self):
        if self._dsw is None:
            self._dsw = self.P.sem("w_" + self.name)
            self._dsw.sw = True
        return self._dsw


class Prog:
    ENGS = ("pe", "act", "dve", "pool", "sp")

    def __init__(self, nc, stack):
        self.nc = nc
        self.stack = stack
        self.scope = stack
        self.q = {k: [] for k in self.ENGS}
        self.all_sems = []
        self.esem = {k: self.sem("e_" + k) for k in ("pe", "act", "dve", "pool")}
        self.seen = {k: {} for k in self.ENGS}
        self.n = 0
        self.bc_val = None
        self.bc_reg = None
        self.nblk = 0
        self.cond = None
        self.dummy = None

    def sem(self, name):
        s = Sem(self.stack.enter_context(self.nc.semaphore(name)))
        self.all_sems.append(s)
        return s

    def sb(self, name, shape, dtype):
        return Tl(self, self.scope.enter_context(self.nc.sbuf_tensor("s_" + name, list(shape), dtype)), name)

    def ps(self, name, shape, dtype):
        return Tl(self, self.scope.enter_context(self.nc.psum_tensor("p_" + name, list(shape), dtype)), name)

    def dram(self, t, name):
        return Tl(self, t, name, is_dram=True)

    def op(self, eng, fn, reads=(), writes=(), dsem=None):
        deps = []
        for tl in reads:
            b = tl.b
            if b.last_w is not None:
                deps.append(b.last_w)
        for tl in writes:
            b = tl.b
            if b.last_w is not None:
                deps.append(b.last_w)
            deps.extend(b.readers)
        waits = {}
        mysem = self.esem.get(eng)
        seen = self.seen[eng]
        for (s, v) in deps:
            if v is None:
                v = s.count
            if s is mysem and eng == "pe":
                continue
            if v <= seen.get(id(s), 0):
                continue
            if id(s) in waits and waits[id(s)][1] >= v:
                continue
            waits[id(s)] = (s, v)
        for s, v in waits.values():
            seen[id(s)] = v
        if dsem is not None:
            dsem.count += 16
            tok = (dsem, None)
            inc = (dsem, 16)
        else:
            s = self.esem[eng]
            s.count += 1
            tok = (s, s.count)
            inc = (s, 1)
        self.q[eng].append((list(waits.values()), fn, inc))
        if self.cond is not None:
            d = self.cond["comp"][eng]
            prev = d.get(id(inc[0]), (inc[0], 0))[1]
            d[id(inc[0])] = (inc[0], prev + inc[1])
        self.n += 1
        for tl in reads:
            tl.b.readers.append(tok)
        for tl in writes:
            tl.b.last_w = tok
            tl.b.readers = []

    def wait_only(self, eng, reads):
        waits = {}
        seen = self.seen[eng]
        for tl in reads:
            lw = tl.b.last_w
            if lw is None:
                continue
            s, v = lw
            if v is None:
                v = s.count
            if v <= seen.get(id(s), 0):
                continue
            waits[id(s)] = (s, v)
            seen[id(s)] = v
        self.q[eng].append((list(waits.values()), None, None))

    def cond_begin(self, flag_tl, flag_ap):
        assert self.cond is None
        for k in self.ENGS:
            self.wait_only(k, [flag_tl])
        self.cond = dict(flag_ap=flag_ap, outer_q=self.q, seen={k: dict(v) for k, v in self.seen.items()},
                         start={id(s): s.count for s in self.all_sems}, comp={k: {} for k in self.ENGS})
        self.q = {k: [] for k in self.ENGS}

    def cond_end(self):
        c = self.cond
        sub = self.q
        self.q = c["outer_q"]
        for k in self.ENGS:
            if sub[k]:
                comp = [(sm, c["start"].get(id(sm), 0), d) for (sm, d) in c["comp"][k].values()]
                self.q[k].append(("cond", c["flag_ap"], sub[k], comp))
        self.seen = c["seen"]
        self.cond = None

    def barrier(self):
        for k in self.ENGS:
            waits = []
            for s in self.all_sems:
                if s.count > self.seen[k].get(id(s), 0):
                    waits.append((s, s.count))
                    self.seen[k][id(s)] = s.count
            self.q[k].append((waits, None, None))

    def emit(self):
        nc = self.nc
        q = self.q
        if not any(q.values()):
            return

        self.nblk += 1
        nb = self.nblk

        def run_list(items, e, reg):
            for it in items:
                if it[0] == "cond":
                    _, flag_ap, sub, comp = it
                    e.reg_load(reg, flag_ap)
                    with e.If_ne(reg, 0):
                        run_list(sub, e, reg)
                    with e.Else():
                        prev = None
                        for sm, start, d in comp:
                            e.wait_ge(sm.h, start)
                            if sm.sw:
                                if prev is not None:
                                    e.wait_ge(prev[0].h, prev[1])
                                e.dma_start(out=self.dummy[0], in_=self.dummy[1]).then_inc(sm.h, d)
                                prev = (sm, start + d)
                            else:
                                e.sem_inc(sm.h, d)
                        if prev is not None:
                            e.wait_ge(prev[0].h, prev[1])
                    continue
                waits, fn, inc = it
                for sm, v in waits:
                    e.wait_ge(sm.h, v)
                if fn is None:
                    continue
                ins = fn(e)
                ins.then_inc(inc[0].h, inc[1])

        def run(k, e):
            with e.register(f"fl_{k}_{nb}") as reg:
                run_list(q[k], e, reg)

        with nc.Block() as block:
            @block.tensor
            def _(e):
                run("pe", e)

            @block.scalar
            def _(e):
                run("act", e)

            @block.vector
            def _(e):
                run("dve", e)

            @block.gpsimd
            def _(e):
                if self.bc_val is not None:
                    with e.register(f"bc{nb}") as bc:
                        e.reg_mov(bc, self.bc_val)
                        self.bc_reg = bc
                        run("pool", e)
                else:
                    run("pool", e)

            @block.sync
            def _(e):
                run("sp", e)
        self.q = {k: [] for k in self.ENGS}


STOP = None


def build_program(cfg, debug=False):
    c = cfg
    D, T, NT, KC, NE, FF, CAP = c.D, c.T, c.NT, c.KC, c.NE, c.FF, c.CAP
    NKV, AW, KVW, PW, PGW, NPG, INW = c.NKV, c.AW, c.KVW, c.PW, c.PGW, c.NPG, c.INW
    WCH = c.WCH
    NXT = NT + 2
    nc = bass.Bass("TRN2", target_bir_lowering=False)

    def din(name, shape, dt=F32):
        return nc.dram_tensor(name, list(shape), dt, kind="ExternalInput").ap()

    x_ext = din("x_ext", [NXT * 128, D])
    ctxb = din("ctxb", [c.CTX, D])
    cT_d = din("cT", [128, KC * 2])
    w_ada = din("w_ada", [D, 6 * D])
    b_adaT = din("b_adaT", [128, c.NMODC])
    n1T_d = din("n1T", [128, KC])
    n2T_d = din("n2T", [128, KC])
    w_in = din("w_in", [D, INW])
    gvec_d = din("gvec", [4 * 64])
    sinks_d = din("sinks", [c.NQ])
    w_pool = din("w_pool", [NPG, PGW, PGW])
    pscT_d = din("pscT", [128, PW // 128])
    w_out = din("w_out", [D, D])
    w_router = din("w_router", [D, NE])
    b_router = din("b_router", [NE])
    w_gate = din("w_gate", [NE, FF // 128, 128, KC * 128])
    w_up = din("w_up", [NE, FF // 128, 128, KC * 128])
    w_down = din("w_down", [NE, FF, D])
    bgT_d = din("bgT", [128, NE * (FF // 128)])
    buT_d = din("buT", [128, NE * (FF // 128)])
    b_down = din("b_down", [NE, D])
    rope_c = din("rope_c", [NXT * 128 + c.CTX, 64])
    rope_s = din("rope_s", [NXT * 128 + c.CTX, 64])
    masks_d = din("masks", [128, 4 * 128])
    poolB_d = din("poolB", [128, NPG * 9 * 128])
    cmat_d = din("cmat", [128, 3 * 128])
    ecap_d = din("ecap", [NE])
    y_out = nc.dram_tensor("y", [T, D], F32, kind="ExternalOutput").ap()
    x1kind = dict(kind="ExternalOutput") if debug else {}
    X1d = nc.dram_tensor("x1s", [T, D], F32, **x1kind).ap()
    HROWS = NE * T
    YW = max(512, D // 4)
    NYC = D // YW
    XEh = [nc.dram_tensor(f"xe_s{i}", [HROWS, D // 2], BF16).ap() for i in range(2)]
    YEq = [nc.dram_tensor(f"ye_s{i}", [HROWS, YW], F32).ap() for i in range(NYC)]
    G1d = nc.dram_tensor("g1b_s", [128, D], F32).ap()
    G2d = nc.dram_tensor("g2b_s", [128, D], F32).ap()
    if debug:
        LGd = nc.dram_tensor("lg_s", [T, NE], F32, kind="ExternalOutput").ap()

    top = ExitStack()
    with top:
        P = Prog(nc, top)
        X1 = P.dram(X1d, "X1")
        XE = P.dram(None, "XE")
        YE = P.dram(None, "YE")
        G1 = P.dram(G1d, "G1")
        G2 = P.dram(G2d, "G2")
        YO = P.dram(y_out, "YO")
        LGo = P.dram(None, "LGo")

        def ckpt(name):
            if STOP == name:
                P.barrier()
                P.emit()
                return True
            return False

        def dma(q, out_tl, out_ap, in_tl, in_ap, sem_tl=None, track_w=True, **kw):
            if sem_tl is None:
                sem_tl = out_tl if (out_tl is not None and not out_tl.is_dram) else in_tl
            reads = [in_tl] if in_tl is not None else []
            writes = [out_tl] if (out_tl is not None and track_w) else []
            sem = sem_tl.dsw if q == "pool" else sem_tl.ds
            P.op(q, lambda e: e.dma_start(out=out_ap, in_=in_ap, **kw), reads=reads, writes=writes,
                 dsem=sem)
            if out_tl is not None and not track_w:
                out_tl.b.last_w = (sem, None)

        A2 = P.sb("A2", [128, KC], F32)
        S2 = P.sb("S2", [128, KC], F32)
        with ExitStack() as scA:
            P.scope = scA
            NG = NT // 2
            cmat = P.sb("cmat", [128, 3, 128], BF16)
            identf = P.sb("identf", [128, 128], F32)
            onesf = P.sb("onesf", [128, 128], F32)
            masks = P.sb("masks", [128, 4, 128], BF16)
            poolB = P.sb("poolB", [128, NPG * 9, 128], BF16)
            gvec = P.sb("gvec", [128, 4, 64], F32)
            esink = P.sb("esink", [128, c.NQ], F32)
            cT = P.sb("cT", [128, KC, 2], F32)
            sig = P.sb("sig", [128, KC, 2], F32)
            scb = P.sb("scb", [128, KC, 2], BF16)
            badaT = P.sb("badaT", [128, c.NMODC], F32)
            MOD = P.sb("MOD", [128, c.NMODC, 2], F32)
            n1T = P.sb("n1T", [128, KC], F32)
            n2T = P.sb("n2T", [128, KC], F32)
            A1 = P.sb("A1", [128, KC], F32)
            A1c = P.sb("A1c", [128, KC], F32)
            pscT = P.sb("pscT", [128, PW // 128], F32)
            cst = P.sb("cst", [128, 1], F32)
            ident = cmat.t[:, 0, :]

            dma("pool", cmat, cmat.t[:], None, cmat_d.rearrange("p (a b) -> p a b", a=3), sem_tl=cst)
            dma("sp", identf, identf.t[:], None, cmat_d[:, 0:128], sem_tl=cst)
            dma("pool", masks, masks.t[:], None, masks_d.rearrange("p (a b) -> p a b", a=4), sem_tl=cst)
            dma("pool", poolB, poolB.t[:], None, poolB_d.rearrange("p (a b) -> p a b", b=128), sem_tl=cst)
            dma("sp", gvec, gvec.t[:], None,
                gvec_d.partition_broadcast(128).rearrange("p (a b) -> p a b", a=4), sem_tl=cst)
            dma("sp", esink, esink.t[:], None, sinks_d.partition_broadcast(128), sem_tl=cst)
            dma("sp", cT, cT.t[:], None, cT_d.rearrange("p (a b) -> p a b", b=2), sem_tl=cst)
            dma("sp", badaT, badaT.t[:], None, b_adaT, sem_tl=cst)
            dma("sp", n1T, n1T.t[:], None, n1T_d, sem_tl=cst)
            dma("sp", n2T, n2T.t[:], None, n2T_d, sem_tl=cst)
            dma("sp", pscT, pscT.t[:], None, pscT_d, sem_tl=cst)
            P.op("dve", lambda e: e.memset(onesf.t[:], 1.0), writes=[onesf])
            P.op("act", lambda e: e.activation(out=esink.t[:], in_=esink.t[:], func=AF.Exp), reads=[esink], writes=[esink])
            P.op("act", lambda e: e.activation(out=sig.t[:], in_=cT.t[:], func=AF.Sigmoid), reads=[cT], writes=[sig])
            P.op("dve", lambda e: e.tensor_tensor(out=scb.t[:], in0=cT.t[:], in1=sig.t[:], op=ALU.mult),
                 reads=[cT, sig], writes=[scb])

            TP = [P.ps(f"TP{i}", [128, 512], BF16) for i in range(2)]
            ACC = [P.ps(f"ACC{i}", [128, 512], F32) for i in range(2)]
            STp = [P.ps(f"ST{i}", [128, 512], F32) for i in range(2)]
            OAC = [P.ps(f"OAC{i}", [128, 4, 128], F32) for i in range(2)]
            rr = {"tp": 0, "acc": 0, "st": 0, "oac": 0, "w": 0, "ev": 0, "z": 0, "q4": 0}

            def nxt(key, n):
                v = rr[key]
                rr[key] = (v + 1) % n
                return v

            NW = 3
            wring = [P.sb(f"wr{i}", [128, KC * WCH], BF16) for i in range(NW)]

            def load_w(src_ap, ncols):
                i = nxt("w", NW)
                sl = wring[i]
                view = sl.t[:, 0:KC * ncols].rearrange("p (k n) -> p k n", n=ncols)
                dma("pool", sl, view, None, src_ap.rearrange("(k p) n -> p k n", p=128))
                return sl, view

            modps = ACC[0]
            npc = WCH // 128
            for pi in range(6 * D // WCH):
                sl, wv = load_w(w_ada[:, pi * WCH:(pi + 1) * WCH], WCH)

                def f(e, wv=wv, pi=pi):
                    ins = None
                    for cc in range(npc):
                        j = pi * npc + cc
                        for k in range(KC):
                            ins = e.matmul(modps.t[:, 2 * j:2 * j + 2], lhsT=wv[:, k, cc * 128:(cc + 1) * 128],
                                           rhs=scb.t[:, k, :], start=(k == 0), stop=(k == KC - 1))
                    return ins
                P.op("pe", f, reads=[sl, scb], writes=[modps])
            P.op("dve", lambda e: e.tensor_tensor(
                out=MOD.t[:], in0=modps.t[:, 0:2 * c.NMODC].rearrange("p (j r) -> p j r", r=2),
                in1=badaT.t[:, :].unsqueeze(2).to_broadcast([128, c.NMODC, 2]), op=ALU.add),
                reads=[modps, badaT], writes=[MOD])

            def modv(idx, r=0):
                return MOD.t[:, idx * KC:(idx + 1) * KC, r]
            P.op("dve", lambda e: e.scalar_tensor_tensor(out=A1.t[:], in0=modv(1, 0), scalar=1.0, in1=n1T.t[:],
                                                         op0=ALU.add, op1=ALU.mult), reads=[MOD, n1T], writes=[A1])
            P.op("dve", lambda e: e.scalar_tensor_tensor(out=A1c.t[:], in0=modv(1, 1), scalar=1.0, in1=n1T.t[:],
                                                         op0=ALU.add, op1=ALU.mult), reads=[MOD, n1T], writes=[A1c])

            diag = [P.sb(f"diag{i}", [128, 128], F32) for i in range(2)]
            gpc = [P.sb(f"gpc{i}", [128, 512], F32) for i in range(2)]
            for gi, (midx, Gd, Gt) in enumerate(((2, G1d, G1), (5, G2d, G2))):
                for n4 in range(D // 512):
                    acc = ACC[1]
                    for q4 in range(4):
                        ch = n4 * 4 + q4
                        dg = diag[ch % 2]
                        P.op("dve", lambda e, dg=dg, ch=ch, midx=midx: e.tensor_scalar(
                            out=dg.t[:], in0=identf.t[:], scalar1=MOD.t[:, midx * KC + ch, 0:1], scalar2=0.0,
                            op0=ALU.mult, op1=ALU.add), reads=[identf, MOD], writes=[dg])
                        P.op("pe", lambda e, dg=dg, q4=q4, acc=acc: e.matmul(
                            acc.t[:, q4 * 128:(q4 + 1) * 128], lhsT=onesf.t[:], rhs=dg.t[:], start=True, stop=True),
                            reads=[onesf, dg], writes=[acc])
                    gp = gpc[n4 % 2]
                    P.op("act", lambda e, gp=gp, acc=acc: e.activation(out=gp.t[:], in_=acc.t[:], func=AF.Copy),
                         reads=[acc], writes=[gp])
                    dma("sp", Gt, Gd[:, n4 * 512:(n4 + 1) * 512], gp, gp.t[:], sem_tl=Gt)

            if ckpt("setup"):
                return nc
            NL = 4
            xt = P.sb("xt", [128, D], F32)
            xs = P.sb("xs", [128, D], BF16)
            ss = P.sb("ss", [128, 2], F32)
            hT = P.sb("hT", [128, NL, KC, 128], BF16)
            mixT = P.sb("mixT", [128, KC, 256], BF16)
            kT = P.sb("kT", [64, NL + 2, NKV, 128], BF16)
            Vg = P.sb("Vg", [128, NL + 2, NKV, 65], BF16)
            rc = P.sb("rc", [128, NL, 64], F32)
            rs = P.sb("rs", [128, NL, 64], F32)
            tabs = P.sb("tabs", [128, 4, NL, 64], F32)
            zs = [P.sb(f"zs{i}", [128, 256], F32) for i in range(2)]
            sq = P.sb("sq", [128, 256], F32)
            t1 = P.sb("t1", [128, 256], F32)
            t2 = P.sb("t2", [128, 256], F32)
            hs = P.sb("hs", [128, 8], F32)
            qrot = [P.sb(f"qrot{i}", [128, 256], BF16) for i in range(2)]
            qT4 = [P.sb(f"qT4{i}", [64, 4, 128], BF16) for i in range(4)]
            Pt = [P.sb(f"Pt{i}", [128, 512], BF16) for i in range(3)]
            den = P.sb("den", [128, 8], F32)
            attn = [P.sb(f"attn{i}", [128, 256], BF16) for i in range(2)]
            Ubuf = P.sb("Ubuf", [128, NL, PGW], BF16)
            dT = P.sb("dT", [128, PGW // 128, 256], BF16)
            wpl = [P.sb(f"wpl{i}", [128, PGW // 128, PGW], BF16) for i in range(2)]
            xp = [P.sb(f"xp{i}", [128, 2, WCH], F32) for i in range(2)]
            g1p = [P.sb(f"g1p{i}", [128, WCH], F32) for i in range(2)]
            ytmp = [P.sb(f"ytmp{i}", [128, WCH], F32) for i in range(2)]
            x1p = [P.sb(f"x1p{i}", [128, 2, WCH], F32) for i in range(2)]
            P.op("dve", lambda e: e.memset(Vg.t[:], 1.0), writes=[Vg])
            P.op("dve", lambda e: e.memset(xs.t[:], 0.0), writes=[xs])
            if debug:
                P.op("dve", lambda e: e.memset(xt.t[:], 0.0), writes=[xt])
                for r in range(HROWS // 128):
                    for hh in range(2):
                        dma("sp", XE, XEh[hh][r * 128:(r + 1) * 128, :], xs, xs.t[:, 0:D // 2], sem_tl=xs, track_w=False)
                    for hc in range(NYC):
                        dma("sp", YE, YEq[hc][r * 128:(r + 1) * 128, :], xt, xt.t[:, 0:YW], sem_tl=xt, track_w=False)

            def evac_engine():
                return ("act", "dve")[nxt("ev", 2)]

            def copy_op(eng, out_ap, in_ap, reads, writes):
                if eng == "act":
                    P.op("act", lambda e: e.activation(out=out_ap, in_=in_ap, func=AF.Copy), reads=reads, writes=writes)
                else:
                    P.op(eng, lambda e: e.tensor_copy(out_ap, in_ap), reads=reads, writes=writes)

            def affine_op(eng, out_ap, in_ap, sc_ap, bi_ap, reads, writes):
                if eng == "act":
                    P.op("act", lambda e: e.activation(out=out_ap, in_=in_ap, func=AF.Identity, bias=bi_ap, scale=sc_ap),
                         reads=reads, writes=writes)
                else:
                    P.op(eng, lambda e: e.tensor_scalar(out=out_ap, in0=in_ap, scalar1=sc_ap, scalar2=bi_ap,
                                                        op0=ALU.mult, op1=ALU.add), reads=reads, writes=writes)

            def norm_tile(src_tl, dst_tl, ss_col):
                P.op("dve", lambda e: e.memset(ss.t[:, ss_col:ss_col + 1], 0.0), writes=[ss])
                P.op("act", lambda e: e.activation(out=dst_tl.t[:], in_=src_tl.t[:], func=AF.Square,
                                                   accum_out=ss.t[:, ss_col:ss_col + 1]),
                     reads=[src_tl], writes=[dst_tl, ss])
                P.op("act", lambda e: e.activation(out=ss.t[:, ss_col:ss_col + 1], in_=ss.t[:, ss_col:ss_col + 1],
                                                   func=AF.Sqrt, bias=cst_eps.t[:, 0:1], scale=1.0 / D),
                     reads=[ss, cst_eps], writes=[ss])
                P.op("dve", lambda e: e.reciprocal(out=ss.t[:, ss_col:ss_col + 1], in_=ss.t[:, ss_col:ss_col + 1]),
                     reads=[ss], writes=[ss])
                P.op("dve", lambda e: e.tensor_scalar(out=dst_tl.t[:], in0=src_tl.t[:], scalar1=ss.t[:, ss_col:ss_col + 1],
                                                      scalar2=0.0, op0=ALU.mult, op1=ALU.add),
                     reads=[src_tl, ss], writes=[dst_tl])

            cst_eps = P.sb("cst_eps", [128, 2], F32)
            P.op("dve", lambda e: e.memset(cst_eps.t[:, 0:1], NORM_EPS), writes=[cst_eps])
            P.op("dve", lambda e: e.memset(cst_eps.t[:, 1:2], NORM_EPS), writes=[cst_eps])

            def transposes_to(src_tl, nchunks, dst_fn, sc_fn, bi_fn, dst_tl, extra_reads=()):
                for c0 in range(0, nchunks, 4):
                    tp = TP[nxt("tp", 2)]
                    nn = min(4, nchunks - c0)

                    def f(e, c0=c0, nn=nn, tp=tp):
                        ins = None
                        for i in range(nn):
                            ins = e.transpose(tp.t[:, i * 128:(i + 1) * 128], src_tl.t[:, (c0 + i) * 128:(c0 + i + 1) * 128], ident)
                        return ins
                    P.op("pe", f, reads=[src_tl, cmat], writes=[tp])
                    for i in range(nn):
                        cc = c0 + i
                        eng = evac_engine()
                        if sc_fn is None:
                            copy_op(eng, dst_fn(cc), tp.t[:, i * 128:(i + 1) * 128], [tp], [dst_tl])
                        else:
                            affine_op(eng, dst_fn(cc), tp.t[:, i * 128:(i + 1) * 128], sc_fn(cc), bi_fn(cc),
                                      [tp, MOD, *extra_reads], [dst_tl])

            def stage1(row0, l, Avec, shift_r):
                src = x_ext if row0 >= 0 else ctxb
                r0 = row0 if row0 >= 0 else (-row0 - 1)
                dma("sp", xt, xt.t[:], None, src[r0:r0 + 128, :])
                norm_tile(xt, xs, 0)
                transposes_to(xs, KC, lambda cc: hT.t[:, l, cc, :],
                              lambda cc: Avec.t[:, cc:cc + 1], lambda cc: MOD.t[:, 0 * KC + cc, shift_r:shift_r + 1],
                              hT, extra_reads=[Avec])

            def project(l, wsl, wv, ncols):
                acc = ACC[nxt("acc", 2)]

                def f(e):
                    ins = None
                    for k in range(KC):
                        ins = e.matmul(acc.t[:, 0:ncols], lhsT=hT.t[:, l, k, :], rhs=wv[:, k, :],
                                       start=(k == 0), stop=(k == KC - 1))
                    return ins
                P.op("pe", f, reads=[hT, wsl], writes=[acc])
                return acc

            def rms_rope(acc, nh, l, tA, tB, out_aps):
                w = nh * 64
                z = zs[nxt("z", 2)]
                P.op("act", lambda e: e.activation(out=z.t[:, 0:w], in_=acc.t[:, 0:w], func=AF.Copy), reads=[acc], writes=[z])
                P.op("act", lambda e: e.activation(out=sq.t[:, 0:w], in_=z.t[:, 0:w], func=AF.Square), reads=[z], writes=[sq])
                P.op("dve", lambda e: e.tensor_reduce(out=hs.t[:, 0:nh], in_=sq.t[:, 0:w].rearrange("p (h d) -> p h d", d=64),
                                                      axis=AX.X, op=ALU.add), reads=[sq], writes=[hs])
                P.op("act", lambda e: e.activation(out=hs.t[:, 0:nh], in_=hs.t[:, 0:nh], func=AF.Sqrt,
                                                   bias=cst_eps.t[:, 1:2], scale=1.0 / 64), reads=[hs, cst_eps], writes=[hs])
                P.op("dve", lambda e: e.reciprocal(out=hs.t[:, 0:nh], in_=hs.t[:, 0:nh]), reads=[hs], writes=[hs])
                z3 = z.t[:, 0:w].rearrange("p (h d) -> p h d", d=64)
                z5 = z.t[:, 0:w].rearrange("p (h a b d) -> p h a b d", a=2, b=2, d=16)
                t13 = t1.t[:, 0:w].rearrange("p (h d) -> p h d", d=64)
                t25 = t2.t[:, 0:w].rearrange("p (h a b d) -> p h a b d", a=2, b=2, d=16)
                A_b = tabs.t[:, tA, l, :].unsqueeze(1).to_broadcast([128, nh, 64])
                B5 = tabs.t[:, tB, l, :].rearrange("p (a b d) -> p a b d", a=2, b=2)
                P.op("dve", lambda e: e.tensor_tensor(out=t13, in0=z3, in1=A_b, op=ALU.mult), reads=[z, tabs], writes=[t1])
                for b in range(2):
                    P.op("dve", lambda e, b=b: e.tensor_tensor(
                        out=t25[:, :, :, b, :], in0=z5[:, :, :, 1 - b, :],
                        in1=B5[:, :, b, :].unsqueeze(1).to_broadcast([128, nh, 2, 16]), op=ALU.mult),
                        reads=[z, tabs], writes=[t2])
                P.op("dve", lambda e: e.tensor_tensor(out=t1.t[:, 0:w], in0=t1.t[:, 0:w], in1=t2.t[:, 0:w], op=ALU.add),
                     reads=[t1, t2], writes=[t1])
                for (otl, oap) in out_aps:
                    P.op("dve", lambda e, oap=oap: e.tensor_tensor(
                        out=oap, in0=t13, in1=hs.t[:, 0:nh].unsqueeze(2).to_broadcast([128, nh, 64]), op=ALU.mult),
                        reads=[t1, hs], writes=[otl])

            def load_tables(row0, nl, l0):
                dma("sp", rc, rc.t[:, 0:nl, :], None, rope_c[row0:row0 + nl * 128, :].rearrange("(l p) d -> p l d", p=128))
                dma("sp", rs, rs.t[:, 0:nl, :], None, rope_s[row0:row0 + nl * 128, :].rearrange("(l p) d -> p l d", p=128))
                for ti, (src, gi) in enumerate(((rc, 0), (rs, 1), (rc, 2), (rs, 3))):
                    P.op("dve", lambda e, ti=ti, src=src, gi=gi: e.tensor_tensor(
                        out=tabs.t[:, ti, l0:l0 + nl, :], in0=src.t[:, 0:nl, :],
                        in1=gvec.t[:, gi, :].unsqueeze(1).to_broadcast([128, nl, 64]), op=ALU.mult),
                        reads=[src, gvec], writes=[tabs])

            def do_k(acc, l, slot):
                kr = qrot[nxt("oac", 2)]
                rms_rope(acc, NKV, l, 2, 3, [(kr, kr.t[:, 0:NKV * 64].rearrange("p (h d) -> p h d", d=64))])
                tp = TP[nxt("tp", 2)]

                def f(e):
                    ins = None
                    for h in range(NKV):
                        ins = e.transpose(tp.t[0:64, h * 128:(h + 1) * 128], kr.t[:, h * 64:(h + 1) * 64], ident)
                    return ins
                P.op("pe", f, reads=[kr, cmat], writes=[tp])
                copy_op(evac_engine(), kT.t[0:64, slot, :, :], tp.t[0:64, 0:NKV * 128].rearrange("p (h t) -> p h t", t=128),
                        [tp], [kT])

            def do_v(acc, slot):
                copy_op(evac_engine(), Vg.t[:, slot, :, 0:64], acc.t[:, 0:NKV * 64].rearrange("p (h d) -> p h d", d=64),
                        [acc], [Vg])

            load_tables(NXT * 128, 2, 0)
            kwid = min(WCH, KVW)
            for ci in range(c.CTX // 128):
                stage1(-(ci * 128) - 1, ci, A1c, 1)
            for kc0 in range(0, KVW, kwid):
                assert kwid == KVW, "k/v chunking assumes KVW <= 256"
            wsl, wv = load_w(w_in[:, AW:AW + KVW], KVW)
            for ci in range(c.CTX // 128):
                acc = project(ci, wsl, wv, KVW)
                do_k(acc, ci, NL + ci)
            wsl, wv = load_w(w_in[:, AW + KVW:AW + 2 * KVW], KVW)
            for ci in range(c.CTX // 128):
                acc = project(ci, wsl, wv, KVW)
                do_v(acc, NL + ci)

            if ckpt("ctx"):
                return nc
            scale = float(c.HD) ** -0.5
            nqc = AW // WCH
            nuc = max(1, PGW // WCH)
            ucw = min(WCH, PGW)
            for g in range(NG):
                own0 = 2 * g
                load_tables((own0) * 128, NL, 0)
                for l in range(NL):
                    stage1((own0 + l) * 128, l, A1, 0)
                wsl, wv = load_w(w_in[:, AW:AW + KVW], KVW)
                for l in range(NL):
                    acc = project(l, wsl, wv, KVW)
                    do_k(acc, l, l)
                wsl, wv = load_w(w_in[:, AW + KVW:AW + 2 * KVW], KVW)
                for l in range(NL):
                    acc = project(l, wsl, wv, KVW)
                    do_v(acc, l)
                if g == 0 and ckpt("g0kv"):
                    return nc
                for pg in range(NPG):
                    wp = wpl[pg % 2]
                    dma("pool", wp, wp.t[:], None, w_pool[pg].rearrange("(k p) n -> p k n", p=128))
                    for uc in range(nuc):
                        col0 = AW + 2 * KVW + pg * PGW + uc * ucw
                        wsl, wv = load_w(w_in[:, col0:col0 + ucw], ucw)
                        for l in range(NL):
                            acc = project(l, wsl, wv, ucw)
                            copy_op(evac_engine(), Ubuf.t[:, l, uc * ucw:(uc + 1) * ucw], acc.t[:, 0:ucw], [acc], [Ubuf])
                    for lo in range(2):
                        l = 1 + lo
                        own = own0 + lo
                        var = 0 if own == 0 else (2 if own == NT - 1 else 1)
                        for cc in range(PGW // 128):
                            acc = ACC[nxt("acc", 2)]

                            def f(e, acc=acc, l=l, cc=cc, var=var, pg=pg):
                                ins = None
                                for kd in range(3):
                                    ins = e.matmul(acc.t[:, 0:128], lhsT=Ubuf.t[:, l - 1 + kd, cc * 128:(cc + 1) * 128],
                                                   rhs=poolB.t[:, pg * 9 + var * 3 + kd, :], start=(kd == 0), stop=(kd == 2))
                                return ins
                            P.op("pe", f, reads=[Ubuf, poolB], writes=[acc])
                            copy_op(evac_engine(), dT.t[:, cc, lo * 128:(lo + 1) * 128], acc.t[:, 0:128], [acc], [dT])
                    for co in range(PGW // 128):
                        acc = ACC[nxt("acc", 2)]

                        def f(e, acc=acc, co=co, wp=wp):
                            ins = None
                            nk = PGW // 128
                            for ci in range(nk):
                                ins = e.matmul(acc.t[:, 0:256], lhsT=wp.t[:, ci, co * 128:(co + 1) * 128], rhs=dT.t[:, ci, :],
                                               start=(ci == 0), stop=(ci == nk - 1))
                            return ins
                        P.op("pe", f, reads=[wp, dT], writes=[acc])
                        pj = pg * (PGW // 128) + co
                        P.op("act", lambda e, acc=acc, pj=pj: e.activation(
                            out=mixT.t[:, AW // 128 + pj, :], in_=acc.t[:, 0:256], func=AF.Identity, scale=pscT.t[:, pj:pj + 1]),
                            reads=[acc, pscT], writes=[mixT])
                if g == 0 and ckpt("g0pool"):
                    return nc
                def q_stage(qc):
                    wsl, wv = load_w(w_in[:, qc * WCH:(qc + 1) * WCH], WCH)
                    res = []
                    for lo in range(2):
                        l = 1 + lo
                        acc = project(l, wsl, wv, WCH)
                        qr = qrot[nxt("oac", 2)]
                        rms_rope(acc, 4, l, 0, 1, [(qr, qr.t[:].rearrange("p (h d) -> p h d", d=64))])
                        tp = TP[nxt("tp", 2)]

                        def f(e, tp=tp, qr=qr):
                            ins = None
                            for h in range(4):
                                ins = e.transpose(tp.t[0:64, h * 128:(h + 1) * 128], qr.t[:, h * 64:(h + 1) * 64], ident)
                            return ins
                        P.op("pe", f, reads=[qr, cmat], writes=[tp])
                        q4 = qT4[nxt("q4", 4)]
                        copy_op(evac_engine(), q4.t[:], tp.t[0:64, :].rearrange("p (h t) -> p h t", t=128), [tp], [q4])
                        res.append((lo, q4))
                    return res

                pend = q_stage(0)
                for qc in range(nqc):
                    nxt_units = q_stage(qc + 1) if qc + 1 < nqc else None
                    kvh = (qc * 4) // c.GQ
                    for (lo, q4) in pend:
                        l = 1 + lo
                        own = own0 + lo
                        oac = OAC[lo]
                        blocks = [(l - 1, 0 if own == 0 else 1), (l, None), (l + 1, 3 if own == NT - 1 else 2),
                                  (NL, None), (NL + 1, None)][:3 + c.CTX // 128]
                        nb = len(blocks)
                        for bi, (slot, mk) in enumerate(blocks):
                            st = STp[bi % 2]
                            P.op("pe", lambda e, st=st, slot=slot, q4=q4, kvh=kvh: e.matmul(
                                st.t[:], lhsT=kT.t[0:64, slot, kvh, :], rhs=q4.t[:].rearrange("p h t -> p (h t)"),
                                start=True, stop=True), reads=[kT, q4], writes=[st])
                            pt = Pt[bi % 3]
                            P.op("act", lambda e, st=st, pt=pt: e.activation(out=pt.t[:], in_=st.t[:], func=AF.Exp, scale=scale),
                                 reads=[st], writes=[pt])
                            if mk is not None:
                                P.op("dve", lambda e, pt=pt, mk=mk: e.tensor_tensor(
                                    out=pt.t[:].rearrange("p (h t) -> p h t", t=128),
                                    in0=pt.t[:].rearrange("p (h t) -> p h t", t=128),
                                    in1=masks.t[:, mk, :].unsqueeze(1).to_broadcast([128, 4, 128]), op=ALU.mult),
                                    reads=[pt, masks], writes=[pt])

                            def f(e, pt=pt, slot=slot, bi=bi, oac=oac, kvh=kvh, nb=nb):
                                ins = None
                                for h in range(4):
                                    ins = e.matmul(oac.t[:, h, 0:65], lhsT=pt.t[:, h * 128:(h + 1) * 128],
                                                   rhs=Vg.t[:, slot, kvh, :], start=(bi == 0 and h == 0),
                                                   stop=(bi == nb - 1 and h == 3), skip_group_check=True)
                                return ins
                            P.op("pe", f, reads=[pt, Vg], writes=[oac])
                        P.op("dve", lambda e, oac=oac, qc=qc: e.tensor_tensor(
                            out=den.t[:, 0:4], in0=oac.t[:, :, 64], in1=esink.t[:, qc * 4:qc * 4 + 4], op=ALU.add),
                            reads=[oac, esink], writes=[den])
                        P.op("dve", lambda e: e.reciprocal(out=den.t[:, 0:4], in_=den.t[:, 0:4]), reads=[den], writes=[den])
                        at = attn[lo]
                        P.op("dve", lambda e, oac=oac, at=at: e.tensor_tensor(
                            out=at.t[:].rearrange("p (h d) -> p h d", d=64), in0=oac.t[:, :, 0:64],
                            in1=den.t[:, 0:4].unsqueeze(2).to_broadcast([128, 4, 64]), op=ALU.mult),
                            reads=[oac, den], writes=[at])
                        tp = TP[nxt("tp", 2)]

                        def f(e, tp=tp, at=at):
                            ins = None
                            for i in range(2):
                                ins = e.transpose(tp.t[:, i * 128:(i + 1) * 128], at.t[:, i * 128:(i + 1) * 128], ident)
                            return ins
                        P.op("pe", f, reads=[at, cmat], writes=[tp])
                        copy_op(evac_engine(), mixT.t[:, 2 * qc:2 * qc + 2, lo * 128:(lo + 1) * 128],
                                tp.t[:, 0:256].rearrange("p (c t) -> p c t", t=128), [tp], [mixT])
                    pend = nxt_units
                if g == 0 and ckpt("g0attn"):
                    return nc
                for n in range(D // WCH):
                    wsl, wv = load_w(w_out[:, n * WCH:(n + 1) * WCH], WCH)
                    xq = xp[n % 2]
                    gq = g1p[n % 2]
                    xo = x1p[n % 2]
                    r0 = (own0 + 1) * 128
                    dma("sp", xq, xq.t[:], None, x_ext[r0:r0 + 256, n * WCH:(n + 1) * WCH].rearrange("(l p) n -> p l n", p=128))
                    dma("sp", gq, gq.t[:], G1, G1d[:, n * WCH:(n + 1) * WCH])
                    for lo in range(2):
                        acc = ACC[nxt("acc", 2)]

                        def f(e, acc=acc, lo=lo, wv=wv):
                            ins = None
                            for k in range(KC):
                                ins = e.matmul(acc.t[:, 0:WCH], lhsT=mixT.t[:, k, lo * 128:(lo + 1) * 128], rhs=wv[:, k, :],
                                               start=(k == 0), stop=(k == KC - 1))
                            return ins
                        P.op("pe", f, reads=[mixT, wsl], writes=[acc])
                        yt = ytmp[lo]
                        P.op("dve", lambda e, acc=acc, yt=yt, gq=gq: e.tensor_tensor(out=yt.t[:], in0=acc.t[:, 0:WCH], in1=gq.t[:], op=ALU.mult),
                             reads=[acc, gq], writes=[yt])
                        P.op("dve", lambda e, yt=yt, xq=xq, xo=xo, lo=lo: e.tensor_tensor(out=xo.t[:, lo, :], in0=yt.t[:], in1=xq.t[:, lo, :], op=ALU.add),
                             reads=[yt, xq], writes=[xo])
                    dma("sp", X1, X1d[own0 * 128:own0 * 128 + 256, n * WCH:(n + 1) * WCH].rearrange("(l p) n -> p l n", p=128),
                        xo, xo.t[:], sem_tl=xo, track_w=False)

            P.op("dve", lambda e: e.scalar_tensor_tensor(out=A2.t[:], in0=modv(4, 0), scalar=1.0, in1=n2T.t[:],
                                                         op0=ALU.add, op1=ALU.mult), reads=[MOD, n2T], writes=[A2])
            P.op("dve", lambda e: e.tensor_copy(S2.t[:], modv(3, 0)), reads=[MOD], writes=[S2])
            P.barrier()
            P.emit()

        if STOP != "phaseA":
            phaseB(P, c, nc, locals())
        P.emit()
    return nc


def phaseB(P, c, nc, L):
    D, T, NT, KC, NE, FF, CAP = c.D, c.T, c.NT, c.KC, c.NE, c.FF, c.CAP
    X1, XE, YE, G2, YO = L["X1"], L["XE"], L["YE"], L["G2"], L["YO"]
    X1d, XEh, YEq, G2d, y_out = L["X1d"], L["XEh"], L["YEq"], L["G2d"], L["y_out"]
    HROWS, YW, NYC = L["HROWS"], L["YW"], L["NYC"]
    CH = T // 4
    NS = CH // 128
    A2, S2 = L["A2"], L["S2"]
    dma = L["dma"]
    debug = L["debug"]
    FK = FF // 128
    P.bc_val = HROWS - 1
    with ExitStack() as scP:
        P.scope = scP
        SLi = P.sb("SLi", [128, NT, 4], I32)
        SLi1 = P.sb("SLi1", [128, NT, 4], I32)
        FLi = P.sb("FLi", [1, 3 * NE], I32)
        dmy = P.sb("dmy", [1, NE], F32)
        P.dummy = (dmy.t[0:1, :], L["ecap_d"].rearrange("(a n) -> a n", a=1))
        WT = P.sb("WT", [128, NT, 4], F32)
        combT = P.sb("combT", [32, NT, 128], BF16)
        cmat = P.sb("cmatB", [128, 3, 128], BF16)
        cstB = P.sb("cstB", [128, 2], F32)
        ident = cmat.t[:, 0, :]
        dma("pool", cmat, cmat.t[:], None, L["cmat_d"].rearrange("p (a b) -> p a b", a=3), sem_tl=cstB)
        P.op("dve", lambda e: e.memset(cstB.t[:, 0:1], NORM_EPS), writes=[cstB])

        with ExitStack() as scB:
            P.scope = scB
            TP = [P.ps(f"TPb{i}", [128, 512], BF16) for i in range(2)]
            GU = [P.ps(f"GU{i}", [128, 512], F32) for i in range(4)]
            DN = [P.ps(f"DN{i}", [128, 512], F32) for i in range(2)]
            RT = DN[0]
            rr = {"tp": 0, "ev": 0, "w": 0, "dn": 0, "ys": 0, "gu": 0}

            def nxt(key, n):
                v = rr[key]
                rr[key] = (v + 1) % n
                return v

            def evac_engine():
                return ("act", "dve")[nxt("ev", 2)]

            wr_bf = P.sb("wr_bf", [128, KC, NE], BF16)
            brt = P.sb("brt", [128, NE], F32)
            ecap = P.sb("ecap", [128, NE], F32)
            bgT = P.sb("bgT", [128, NE * FK], F32)
            buT = P.sb("buT", [128, NE * FK], F32)
            Rf = P.sb("Rf", [128, NE], F32)
            Rb = P.sb("Rb", [128, NE], BF16)
            dma("pool", wr_bf, wr_bf.t[:], None, L["w_router"].rearrange("(k p) n -> p k n", p=128), sem_tl=cstB)
            dma("sp", brt, brt.t[:], None, L["b_router"].partition_broadcast(128), sem_tl=cstB)
            dma("sp", ecap, ecap.t[:], None, L["ecap_d"].partition_broadcast(128), sem_tl=cstB)
            dma("sp", bgT, bgT.t[:], None, L["bgT_d"], sem_tl=cstB)
            dma("sp", buT, buT.t[:], None, L["buT_d"], sem_tl=cstB)
            P.op("dve", lambda e: e.memset(Rf.t[:], 0.0), writes=[Rf])

            scB1 = ExitStack()
            P.scope = scB1
            x1t = P.sb("x1t", [128, D], F32)
            xs2 = [P.sb(f"xs2{i}", [128, D], BF16) for i in range(2)]
            ss = P.sb("ssB", [128, 1], F32)
            h2T = P.sb("h2T", [128, KC, 128], BF16)
            lg = P.sb("lg", [128, NE], F32)
            top8 = P.sb("top8", [128, 8], F32)
            mask = P.sb("mask", [128, NE], F32)
            maskb = P.sb("maskb", [128, NE], BF16)
            slotf = P.sb("slotf", [128, NE], F32)
            ovf = P.sb("ovf", [128, NE], F32)
            eq = P.sb("eq", [128, NE], F32)
            SLf = P.sb("SLf", [128, 4], F32)
            wex = P.sb("wex", [128, 4], F32)
            wsum = P.sb("wsum", [128, 1], F32)
            ntop = P.sb("ntop", [128, 1], F32)
            comb = P.sb("comb", [128, NE], F32)
            combb = P.sb("combb", [128, NE], BF16)

            for j in range(NT):
                xs_ = xs2[j % 2]
                dma("sp", x1t, x1t.t[:], X1, X1d[j * 128:(j + 1) * 128, :])
                P.op("dve", lambda e: e.memset(ss.t[:], 0.0), writes=[ss])
                P.op("act", lambda e, xs_=xs_: e.activation(out=xs_.t[:], in_=x1t.t[:], func=AF.Square, accum_out=ss.t[:, 0:1]),
                     reads=[x1t], writes=[xs_, ss])
                P.op("act", lambda e: e.activation(out=ss.t[:], in_=ss.t[:], func=AF.Sqrt, bias=cstB.t[:, 0:1], scale=1.0 / D),
                     reads=[ss, cstB], writes=[ss])
                P.op("dve", lambda e: e.reciprocal(out=ss.t[:], in_=ss.t[:]), reads=[ss], writes=[ss])
                P.op("dve", lambda e, xs_=xs_: e.tensor_scalar(out=xs_.t[:], in0=x1t.t[:], scalar1=ss.t[:, 0:1], scalar2=0.0,
                                                              op0=ALU.mult, op1=ALU.add), reads=[x1t, ss], writes=[xs_])
                for c0 in range(0, KC, 4):
                    tp = TP[nxt("tp", 2)]

                    def f(e, c0=c0, tp=tp, xs_=xs_):
                        ins = None
                        for i in range(4):
                            ins = e.transpose(tp.t[:, i * 128:(i + 1) * 128], xs_.t[:, (c0 + i) * 128:(c0 + i + 1) * 128], ident)
                        return ins
                    P.op("pe", f, reads=[xs_, cmat], writes=[tp])
                    for i in range(4):
                        cc = c0 + i
                        if evac_engine() == "act":
                            P.op("act", lambda e, tp=tp, i=i, cc=cc: e.activation(
                                out=h2T.t[:, cc, :], in_=tp.t[:, i * 128:(i + 1) * 128], func=AF.Identity,
                                bias=S2.t[:, cc:cc + 1], scale=A2.t[:, cc:cc + 1]), reads=[tp, A2, S2], writes=[h2T])
                        else:
                            P.op("dve", lambda e, tp=tp, i=i, cc=cc: e.tensor_scalar(
                                out=h2T.t[:, cc, :], in0=tp.t[:, i * 128:(i + 1) * 128], scalar1=A2.t[:, cc:cc + 1],
                                scalar2=S2.t[:, cc:cc + 1], op0=ALU.mult, op1=ALU.add), reads=[tp, A2, S2], writes=[h2T])

                def f(e):
                    ins = None
                    for k in range(KC):
                        ins = e.matmul(RT.t[:, 0:NE], lhsT=h2T.t[:, k, :], rhs=wr_bf.t[:, k, :], start=(k == 0), stop=(k == KC - 1))
                    return ins
                P.op("pe", f, reads=[h2T, wr_bf], writes=[RT])
                P.op("dve", lambda e: e.tensor_tensor(out=lg.t[:], in0=RT.t[:, 0:NE], in1=brt.t[:], op=ALU.add),
                     reads=[RT, brt], writes=[lg])
                if debug:
                    dma("sp", L["LGo"], L["LGd"][j * 128:(j + 1) * 128, :], lg, lg.t[:], sem_tl=lg)
                P.op("dve", lambda e: e.max(out=top8.t[:], in_=lg.t[:]), reads=[lg], writes=[top8])
                P.op("dve", lambda e: e.tensor_scalar(out=mask.t[:], in0=lg.t[:], scalar1=top8.t[:, 3:4], scalar2=0.0,
                                                      op0=ALU.is_ge, op1=ALU.add), reads=[lg, top8], writes=[mask])
                P.op("dve", lambda e: e.tensor_copy(maskb.t[:], mask.t[:]), reads=[mask], writes=[maskb])
                P.op("dve", lambda e: e.tensor_copy(Rb.t[:], Rf.t[:]), reads=[Rf], writes=[Rb])

                def f(e):
                    e.matmul(RT.t[:, 64:64 + NE], lhsT=cmat.t[:, 1, :], rhs=maskb.t[:], start=True, stop=False)
                    return e.matmul(RT.t[:, 64:64 + NE], lhsT=cmat.t[:, 2, :], rhs=Rb.t[:], start=False, stop=True)
                P.op("pe", f, reads=[cmat, maskb, Rb], writes=[RT])
                P.op("dve", lambda e: e.tensor_tensor(out=Rf.t[:], in0=Rf.t[:], in1=mask.t[:], op=ALU.add),
                     reads=[Rf, mask], writes=[Rf])
                P.op("dve", lambda e: e.tensor_tensor(out=slotf.t[:], in0=RT.t[:, 64:64 + NE], in1=ecap.t[:], op=ALU.add),
                     reads=[RT, ecap], writes=[slotf])
                for k in range(4):
                    P.op("dve", lambda e, k=k: e.tensor_scalar(out=eq.t[:], in0=lg.t[:], scalar1=top8.t[:, k:k + 1], scalar2=0.0,
                                                               op0=ALU.is_equal, op1=ALU.add), reads=[lg, top8], writes=[eq])
                    P.op("dve", lambda e: e.tensor_tensor(out=eq.t[:], in0=eq.t[:], in1=slotf.t[:], op=ALU.mult),
                         reads=[eq, slotf], writes=[eq])
                    P.op("dve", lambda e, k=k: e.tensor_reduce(out=SLf.t[:, k:k + 1], in_=eq.t[:], axis=AX.X, op=ALU.add),
                         reads=[eq], writes=[SLf])
                P.op("dve", lambda e, j=j: e.tensor_copy(SLi.t[:, j, :], SLf.t[:]), reads=[SLf], writes=[SLi])
                P.op("dve", lambda e: e.tensor_scalar(out=ntop.t[:], in0=top8.t[:, 0:1], scalar1=-1.0, scalar2=0.0,
                                                      op0=ALU.mult, op1=ALU.add), reads=[top8], writes=[ntop])
                P.op("act", lambda e: e.activation(out=wex.t[:], in_=top8.t[:, 0:4], func=AF.Exp, bias=ntop.t[:, 0:1], scale=1.0),
                     reads=[top8, ntop], writes=[wex])
                P.op("dve", lambda e: e.tensor_reduce(out=wsum.t[:], in_=wex.t[:], axis=AX.X, op=ALU.add), reads=[wex], writes=[wsum])
                P.op("dve", lambda e: e.reciprocal(out=wsum.t[:], in_=wsum.t[:]), reads=[wsum], writes=[wsum])
                P.op("dve", lambda e, j=j: e.tensor_scalar(out=WT.t[:, j, :], in0=wex.t[:], scalar1=wsum.t[:, 0:1], scalar2=0.0,
                                                           op0=ALU.mult, op1=ALU.add), reads=[wex, wsum], writes=[WT])
                P.op("act", lambda e: e.activation(out=comb.t[:], in_=lg.t[:], func=AF.Exp, bias=ntop.t[:, 0:1], scale=1.0),
                     reads=[lg, ntop], writes=[comb])
                P.op("dve", lambda e: e.tensor_tensor(out=comb.t[:], in0=comb.t[:], in1=mask.t[:], op=ALU.mult),
                     reads=[comb, mask], writes=[comb])
                P.op("dve", lambda e: e.tensor_scalar(out=combb.t[:], in0=comb.t[:], scalar1=wsum.t[:, 0:1], scalar2=0.0,
                                                      op0=ALU.mult, op1=ALU.add), reads=[comb, wsum], writes=[combb])
                tp = TP[nxt("tp", 2)]
                P.op("pe", lambda e, tp=tp: e.transpose(tp.t[0:NE, 0:128], combb.t[:, :], ident), reads=[combb, cmat], writes=[tp])
                P.op("act", lambda e, tp=tp, j=j: e.activation(out=combT.t[0:NE, j, :], in_=tp.t[0:NE, 0:128], func=AF.Copy),
                     reads=[tp], writes=[combT])
                for k in range(4):
                    for hh in range(2):
                        P.op("pool", lambda e, j=j, k=k, xs_=xs_, hh=hh: e.indirect_dma_start(
                            out=XEh[hh][:, :], out_offset=bass.IndirectOffsetOnAxis(ap=SLi.t[:, j, k:k + 1], axis=0),
                            in_=xs_.t[:, hh * (D // 2):(hh + 1) * (D // 2)], in_offset=None, bounds_check=P.bc_reg, oob_is_err=False),
                            reads=[xs_, SLi], writes=[], dsem=XE.dsw)
                        XE.b.last_w = (XE.dsw, None)
            P.op("dve", lambda e: e.tensor_copy(Rb.t[:], Rf.t[:]), reads=[Rf], writes=[Rb])
            P.op("pe", lambda e: e.matmul(RT.t[:, 0:NE], lhsT=cmat.t[:, 2, :], rhs=Rb.t[:], start=True, stop=True),
                 reads=[cmat, Rb], writes=[RT])
            for cp in range(1, 4):
                P.op("dve", lambda e, cp=cp: e.tensor_scalar(out=comb.t[0:1, :], in0=RT.t[0:1, 0:NE], scalar1=float(cp * CH) + 0.5, scalar2=0.0,
                                                             op0=ALU.is_gt, op1=ALU.add), reads=[RT], writes=[comb])
                P.op("dve", lambda e, cp=cp: e.tensor_copy(FLi.t[0:1, (cp - 1) * NE:cp * NE], comb.t[0:1, :]), reads=[comb], writes=[FLi])

            P.barrier()
            P.emit()
            scB1.close()
            P.scope = scB
            if STOP == "B1":
                return
            NWB = 6
            wring = [P.sb(f"wrb{i}", [128, KC * 128], BF16) for i in range(NWB)]
            wringc = [P.sb(f"wrc{i}", [128, KC * 128], BF16) for i in range(2)]
            xet = [P.sb(f"xet{i}", [128, D], BF16) for i in range(2)]
            XeT = [P.sb(f"XeT{i}", [128, KC, CH], BF16) for i in range(2)]
            actT = [P.sb(f"actT{i}", [128, FK, CH], BF16) for i in range(2)]
            gs = [P.sb(f"gs{i}", [128, CH], F32) for i in range(2)]
            sg = [P.sb(f"sg{i}", [128, CH], F32) for i in range(2)]
            us = [P.sb(f"us{i}", [128, CH], F32) for i in range(2)]
            ga = [P.sb(f"ga{i}", [128, CH], F32) for i in range(2)]
            ysb = [P.sb(f"ysb{i}", [128, 512], F32) for i in range(4)]
            assert KC * 128 == FK * 512, "weight ring slot size assumption (D == 4*FF)"
            rr.update({"xe": 0, "xt": 0, "at": 0, "wc": 0})

            def load_piece(view_fn, src_ap, cond=False):
                sl = wringc[nxt("wc", 2)] if cond else wring[nxt("w", NWB)]
                v = view_fn(sl.t)
                dma("pool", sl, v, None, src_ap)
                return sl, v

            def load_xe(e_, c_, xT):
                r0 = e_ * T + c_ * CH
                for s_i in range(NS):
                    xb = xet[nxt("xe", 2)]
                    for hh in range(2):
                        P.op("sp", lambda e, xb=xb, s_i=s_i, hh=hh, r0=r0: e.dma_start(
                            out=xb.t[:, hh * (D // 2):(hh + 1) * (D // 2)], in_=XEh[hh][r0 + s_i * 128:r0 + (s_i + 1) * 128, :]),
                            reads=[XE], writes=([xb] if hh == 0 else []), dsem=xb.ds)
                        xb.b.last_w = (xb.ds, None)
                    for c0 in range(0, KC, 4):
                        tp = TP[nxt("tp", 2)]

                        def f(e, c0=c0, tp=tp, xb=xb):
                            ins = None
                            for i in range(4):
                                ins = e.transpose(tp.t[:, i * 128:(i + 1) * 128], xb.t[:, (c0 + i) * 128:(c0 + i + 1) * 128], ident)
                            return ins
                        P.op("pe", f, reads=[xb, cmat], writes=[tp])
                        for i in range(4):
                            cc = c0 + i
                            if evac_engine() == "act":
                                P.op("act", lambda e, tp=tp, i=i, cc=cc, s_i=s_i, xT=xT: e.activation(
                                    out=xT.t[:, cc, s_i * 128:(s_i + 1) * 128], in_=tp.t[:, i * 128:(i + 1) * 128], func=AF.Identity,
                                    bias=S2.t[:, cc:cc + 1], scale=A2.t[:, cc:cc + 1]), reads=[tp, A2, S2], writes=[xT])
                            else:
                                P.op("dve", lambda e, tp=tp, i=i, cc=cc, s_i=s_i, xT=xT: e.tensor_scalar(
                                    out=xT.t[:, cc, s_i * 128:(s_i + 1) * 128], in0=tp.t[:, i * 128:(i + 1) * 128],
                                    scalar1=A2.t[:, cc:cc + 1], scalar2=S2.t[:, cc:cc + 1], op0=ALU.mult, op1=ALU.add),
                                    reads=[tp, A2, S2], writes=[xT])

            def expert_pass(e_, c_, xT, prefetch=None):
                cnd = c_ > 0
                aT = actT[nxt("at", 2)]
                r0 = e_ * T + c_ * CH
                for m in range(FK):
                    slg, vg = load_piece(lambda t: t[:, 0:KC * 128].rearrange("p (k n) -> p k n", n=128),
                                         L["w_gate"][e_, m].rearrange("p (k n) -> p k n", n=128), cond=cnd)
                    slu, vu = load_piece(lambda t: t[:, 0:KC * 128].rearrange("p (k n) -> p k n", n=128),
                                         L["w_up"][e_, m].rearrange("p (k n) -> p k n", n=128), cond=cnd)
                    bj = e_ * FK + m
                    pi = nxt("gu", 2)
                    G = GU[2 * pi]
                    U = GU[2 * pi + 1]
                    for (acc, sl, v) in ((G, slg, vg), (U, slu, vu)):
                        def f(e, acc=acc, v=v, xT=xT):
                            ins = None
                            for k in range(KC):
                                ins = e.matmul(acc.t[:, 0:CH], lhsT=v[:, k, :], rhs=xT.t[:, k, :], start=(k == 0), stop=(k == KC - 1))
                            return ins
                        P.op("pe", f, reads=[sl, xT], writes=[acc])
                    g_, s_, u_, a_ = gs[pi], sg[pi], us[pi], ga[pi]
                    P.op("dve", lambda e, G=G, bj=bj, g_=g_: e.tensor_scalar(out=g_.t[:], in0=G.t[:, 0:CH], scalar1=bgT.t[:, bj:bj + 1], scalar2=7.0,
                                                                         op0=ALU.add, op1=ALU.min), reads=[G, bgT], writes=[g_])
                    P.op("act", lambda e, g_=g_, s_=s_: e.activation(out=s_.t[:], in_=g_.t[:], func=AF.Sigmoid, scale=1.702), reads=[g_], writes=[s_])
                    P.op("dve", lambda e, U=U, bj=bj, u_=u_: e.tensor_scalar(out=u_.t[:], in0=U.t[:, 0:CH], scalar1=buT.t[:, bj:bj + 1], scalar2=7.0,
                                                                         op0=ALU.add, op1=ALU.min), reads=[U, buT], writes=[u_])
                    P.op("dve", lambda e, u_=u_: e.tensor_scalar(out=u_.t[:], in0=u_.t[:], scalar1=-7.0, scalar2=1.0, op0=ALU.max, op1=ALU.add),
                         reads=[u_], writes=[u_])
                    P.op("dve", lambda e, g_=g_, s_=s_, a_=a_: e.tensor_tensor(out=a_.t[:], in0=g_.t[:], in1=s_.t[:], op=ALU.mult), reads=[g_, s_], writes=[a_])
                    P.op("dve", lambda e, aT=aT, m=m, a_=a_, u_=u_: e.tensor_tensor(out=aT.t[:, m, :], in0=a_.t[:], in1=u_.t[:], op=ALU.mult),
                         reads=[a_, u_], writes=[aT])
                if prefetch is not None:
                    prefetch()
                for n in range(D // 512):
                    sld, vd = load_piece(lambda t: t[:, 0:FK * 512].rearrange("p (k n) -> p k n", n=512),
                                         L["w_down"][e_, :, n * 512:(n + 1) * 512].rearrange("(k p) n -> p k n", p=128), cond=cnd)
                    for s_i in range(NS):
                        dn = DN[nxt("dn", 2)]

                        def f(e, dn=dn, s_i=s_i, vd=vd, aT=aT):
                            ins = None
                            for k in range(FK):
                                ins = e.matmul(dn.t[:], lhsT=aT.t[:, k, s_i * 128:(s_i + 1) * 128], rhs=vd[:, k, :], start=(k == 0), stop=(k == FK - 1))
                            return ins
                        P.op("pe", f, reads=[aT, sld], writes=[dn])
                        yb = ysb[nxt("ys", 4)]
                        if evac_engine() == "act":
                            P.op("act", lambda e, dn=dn, yb=yb: e.activation(out=yb.t[:], in_=dn.t[:], func=AF.Copy), reads=[dn], writes=[yb])
                        else:
                            P.op("dve", lambda e, dn=dn, yb=yb: e.tensor_copy(yb.t[:], dn.t[:]), reads=[dn], writes=[yb])
                        hc = (n * 512) // YW
                        c0_ = n * 512 - hc * YW
                        dma("sp", YE, YEq[hc][r0 + s_i * 128:r0 + (s_i + 1) * 128, c0_:c0_ + 512], yb, yb.t[:], sem_tl=yb, track_w=False)

            load_xe(0, 0, XeT[0])
            for e_ in range(NE):
                cur = XeT[e_ % 2]
                nxt_ = XeT[(e_ + 1) % 2]
                pf = (lambda e_=e_, nxt_=nxt_: load_xe(e_ + 1, 0, nxt_)) if e_ + 1 < NE else None
                expert_pass(e_, 0, cur, prefetch=pf)
                for c_ in range(1, 4):
                    fi = (c_ - 1) * NE + e_
                    P.cond_begin(FLi, FLi.t[0:1, fi:fi + 1])
                    load_xe(e_, c_, cur)
                    expert_pass(e_, c_, cur)
                    P.cond_end()
            P.barrier()
            P.emit()
        if STOP == "B2":
            return

        with ExitStack() as scC:
            P.scope = scC
            BT = [P.ps(f"BT{i}", [128, 512], F32) for i in range(2)]
            NY = 6
            Yk = [P.sb(f"Yk{i}", [128, D], F32) for i in range(NY)]
            x1b = [P.sb(f"x1b{i}", [128, D], F32) for i in range(2)]
            ob = [P.sb(f"ob{i}", [128, D], F32) for i in range(2)]
            G2B = P.sb("G2B", [128, D], F32)
            bdn = P.sb("bdn", [NE, D], BF16)
            dma("sp", G2B, G2B.t[:], G2, G2d)
            dma("pool", bdn, bdn.t[:], None, L["b_down"])
            for i in range(NY):
                P.op("dve", lambda e, i=i: e.memset(Yk[i].t[:], 0.0), writes=[Yk[i]])
            yi = 0
            for j in range(NT):
                ys = []
                for k in range(4):
                    yt = Yk[yi % NY]
                    yi += 1
                    for hc in range(NYC):
                        P.op("pool", lambda e, yt=yt, j=j, k=k, hc=hc: e.indirect_dma_start(
                            out=yt.t[:, hc * YW:(hc + 1) * YW], out_offset=None, in_=YEq[hc][:, :],
                            in_offset=bass.IndirectOffsetOnAxis(ap=SLi.t[:, j, k:k + 1], axis=0),
                            bounds_check=P.bc_reg, oob_is_err=False), reads=[YE, SLi], writes=([yt] if hc == 0 else []), dsem=yt.dsw)
                        yt.b.last_w = (yt.dsw, None)
                    ys.append(yt)
                xb = x1b[j % 2]
                o = ob[j % 2]
                dma("sp", xb, xb.t[:], X1, X1d[j * 128:(j + 1) * 128, :])
                for n in range(D // 512):
                    bt = BT[n % 2]
                    cs = slice(n * 512, (n + 1) * 512)
                    P.op("pe", lambda e, bt=bt, j=j, cs=cs: e.matmul(bt.t[:], lhsT=combT.t[0:NE, j, :], rhs=bdn.t[0:NE, cs], start=True, stop=True),
                         reads=[combT, bdn], writes=[bt])
                    eng = "dve"
                    P.op("dve", lambda e, bt=bt, j=j, cs=cs, o=o, y0=ys[0]: e.scalar_tensor_tensor(
                        out=o.t[:, cs], in0=y0.t[:, cs], scalar=WT.t[:, j, 0:1], in1=bt.t[:], op0=ALU.mult, op1=ALU.add),
                        reads=[ys[0], WT, bt], writes=[o])
                    for k in range(1, 4):
                        P.op("dve", lambda e, j=j, cs=cs, o=o, yk=ys[k], k=k: e.scalar_tensor_tensor(
                            out=o.t[:, cs], in0=yk.t[:, cs], scalar=WT.t[:, j, k:k + 1], in1=o.t[:, cs], op0=ALU.mult, op1=ALU.add),
                            reads=[ys[k], WT, o], writes=[o])
                    P.op(eng, lambda e, cs=cs, o=o: e.tensor_tensor(out=o.t[:, cs], in0=o.t[:, cs], in1=G2B.t[:, cs], op=ALU.mult),
                         reads=[o, G2B], writes=[o])
                    P.op(eng, lambda e, cs=cs, o=o, xb=xb: e.tensor_tensor(out=o.t[:, cs], in0=o.t[:, cs], in1=xb.t[:, cs], op=ALU.add),
                         reads=[o, xb], writes=[o])
                dma("sp", YO, y_out[j * 128:(j + 1) * 128, :], o, o.t[:], sem_tl=o)
            P.barrier()
            P.emit()


def _feat_major(v, nch):
    return np.ascontiguousarray(np.asarray(v, np.float32).reshape(nch, 128).T)


def _piece_major(w, cfg):
    NE, KC, FK = cfg.NE, cfg.KC, cfg.FF // 128
    v = w.reshape(NE, KC, 128, FK, 128).transpose(0, 3, 2, 1, 4)
    return np.ascontiguousarray(v).reshape(NE, FK, 128, KC * 128)


def _rope_tables(cfg, pos):
    half = 32
    inv_freq = (10000.0 ** (-np.arange(0, half, 2, dtype=np.float32) / np.float32(half))).astype(np.float32)
    row = (pos // cfg.GRID_W).astype(np.float32)
    col = (pos % cfg.GRID_W).astype(np.float32)
    out_c = np.zeros((len(pos), 64), np.float32)
    out_s = np.zeros((len(pos), 64), np.float32)
    for hi, base in enumerate((row, col)):
        ang = base[:, None] * inv_freq[None, :]
        cs, sn = np.cos(ang).astype(np.float32), np.sin(ang).astype(np.float32)
        o = hi * 32
        out_c[:, o:o + 16] = cs
        out_c[:, o + 16:o + 32] = cs
        out_s[:, o:o + 16] = -sn
        out_s[:, o + 16:o + 32] = sn
    return out_c, out_s


def _pool_mats(cfg, g0, n):
    out = np.zeros((cfg.NPG, 3, 128, 128), np.float32)
    for gi, w in enumerate((2, 4, 8, 16)):
        for t in range(128):
            gt = g0 + t
            lo = min(max(gt - w // 2, 0), n)
            hi = min(max(gt + w // 2, 0), n)
            cnt = hi - lo
            for gs in range(lo, hi):
                s = gs - g0
                kd = 0 if s < 0 else (1 if s < 128 else 2)
                out[gi, kd, s - (kd - 1) * 128, t] += 1.0 / cnt
            out[gi, 1, t, t] -= 1.0
    return out


def make_in_maps(cfg, inputs):
    c = cfg
    f = lambda k: np.asarray(inputs[k], np.float32)
    x, cc, ctx, c_ctx = f("x"), f("c"), f("ctx"), f("c_ctx")
    swap = np.concatenate([np.arange(16, 32), np.arange(0, 16), np.arange(48, 64), np.arange(32, 48)])
    gq, gk = f("q_norm_g")[0], f("k_norm_g")[0]
    gvec = np.concatenate([gq, gq[swap], gk, gk[swap]]).reshape(256).astype(np.float32)
    FK = c.FF // 128
    common = {
        "w_ada": f("w_ada")[0], "b_adaT": _feat_major(f("b_ada")[0], c.NMODC),
        "n1T": _feat_major(f("norm1_g")[0], c.KC), "n2T": _feat_major(f("norm2_g")[0], c.KC),
        "w_in": f("w_in")[0], "gvec": gvec, "sinks": np.ascontiguousarray(f("sinks")[0].reshape(-1)),
        "w_pool": f("w_pool")[0], "pscT": _feat_major(f("pool_scale")[0], c.PW // 128),
        "w_out": f("w_out")[0], "w_router": f("w_router")[0], "b_router": np.ascontiguousarray(f("b_router")[0].reshape(-1)),
        "w_gate": _piece_major(f("w_gate")[0], c), "w_up": _piece_major(f("w_up")[0], c), "w_down": f("w_down")[0],
        "bgT": np.ascontiguousarray(f("b_gate")[0].reshape(c.NE, FK, 128).transpose(2, 0, 1).reshape(128, c.NE * FK)),
        "buT": np.ascontiguousarray(f("b_up")[0].reshape(c.NE, FK, 128).transpose(2, 0, 1).reshape(128, c.NE * FK)),
        "b_down": f("b_down")[0],
        "ecap": (np.arange(c.NE, dtype=np.float32) * c.T),
    }
    cm = np.zeros((128, 3, 128), np.float32)
    cm[:, 0, :] = np.eye(128, dtype=np.float32)
    cm[:, 1, :] = np.triu(np.ones((128, 128), np.float32), 1)
    cm[:, 2, :] = 1.0
    common["cmat"] = cm.reshape(128, 384)
    kk = np.arange(128)[:, None]
    qq = np.arange(128)[None, :]
    m_prev = (qq <= kk).astype(np.float32)
    m_next = (kk <= qq).astype(np.float32)
    in_maps = []
    for core in range(NCORES):
        b = core // c.CPB
        t0 = (core % c.CPB) * c.T
        first = (core % c.CPB) == 0
        last = (core % c.CPB) == c.CPB - 1
        xe = np.zeros(((c.NT + 2) * 128, c.D), np.float32)
        lo, hi = t0 - 128, t0 + c.T + 128
        slo, shi = max(lo, 0), min(hi, c.SEQ)
        xe[slo - lo:shi - lo] = x[b, slo:shi]
        pos = np.arange(lo, hi)
        rc, rs = _rope_tables(c, np.clip(pos, 0, c.SEQ - 1))
        rc = np.concatenate([rc, np.ones((c.CTX, 64), np.float32)])
        rs = np.concatenate([rs, np.zeros((c.CTX, 64), np.float32)])
        mk = np.zeros((128, 4, 128), np.float32)
        mk[:, 0] = 0.0 if first else m_prev
        mk[:, 1] = m_prev
        mk[:, 2] = m_next
        mk[:, 3] = 0.0 if last else m_next
        pb = np.zeros((c.NPG, 3, 3, 128, 128), np.float32)
        pb[:, 0] = _pool_mats(c, t0, c.SEQ)
        pb[:, 1] = _pool_mats(c, t0 + 128, c.SEQ)
        pb[:, 2] = _pool_mats(c, t0 + c.T - 128, c.SEQ)
        pbl = np.ascontiguousarray(pb.reshape(c.NPG * 9, 128, 128).transpose(1, 0, 2)).reshape(128, c.NPG * 9 * 128)
        cT = np.stack([_feat_major(cc[b], c.KC), _feat_major(c_ctx, c.KC)], axis=2).reshape(128, c.KC * 2)
        m = dict(common)
        m.update({"x_ext": xe, "ctxb": np.ascontiguousarray(ctx[b]), "cT": np.ascontiguousarray(cT),
                  "rope_c": rc, "rope_s": rs, "masks": mk.reshape(128, 512), "poolB": pbl})
        in_maps.append(m)
    return in_maps


_CACHE = {}


def kernel(**inputs):
    cfg = Cfg()
    if "nc" not in _CACHE:
        _CACHE["nc"] = build_program(cfg)
    nc = _CACHE["nc"]
    in_maps = make_in_maps(cfg, inputs)
    res = run_bass_kernel_spmd(nc, in_maps, core_ids=list(range(NCORES)))
    out = np.concatenate([np.asarray(r["y"]) for r in res.results], axis=0)
    return out.reshape(cfg.BATCH, cfg.SEQ, cfg.D).astype(np.float32, copy=False)
```
